# Optimizing a Trainium2 kernel written in Bass

```python
import math
import jax, jax.numpy as jnp
from jax import lax
import numpy as np

D_MODEL = 1024
BATCH = 16
SEQ = 4096
DEPTH = 4

N_MIXERS = 3
D_FF = 4 * D_MODEL
RMS_EPS = 1e-6
Q_BLOCK = 128

N_BUCKETS = 32
MAX_DISTANCE = 128
N_BIAS_HEADS = 16

A_HEADS = 16
A_HEAD_DIM = 64
IDX_HEADS = 8
IDX_DIM = 64
TOPK_MAX = 256
A_IN = A_HEADS * A_HEAD_DIM + 2 * A_HEAD_DIM + IDX_HEADS * IDX_DIM + IDX_DIM + IDX_HEADS

B_HEADS = 16
B_KV_HEADS = 4
B_HEAD_DIM = 64
WINDOW = 128
B_IN = (B_HEADS + 2 * B_KV_HEADS) * B_HEAD_DIM

C_HEADS = 16
C_Q_RANK = 256
C_KV_RANK = 128
C_NOPE = 64
C_ROPE = 32
C_V = 64
C_QK = C_NOPE + C_ROPE
C_IN = C_Q_RANK + C_KV_RANK + C_ROPE
ROPE_THETA = 10000.0

N_A = (DEPTH + 2) // 3
N_B = (DEPTH + 1) // 3
N_C = DEPTH // 3

kernel_name = "hybrid_dsa_swa_mla_trunk"


def rms_norm(x, g):
    xf = x.astype(jnp.float32)
    y = xf * lax.rsqrt(jnp.mean(xf * xf, axis=-1, keepdims=True) + RMS_EPS)
    return (y * g.astype(jnp.float32)).astype(x.dtype)


def split_cols(t, sizes):
    return jnp.split(t, np.cumsum(sizes)[:-1].tolist(), axis=-1)


def t5_bucket(rel):
    n = jnp.maximum(rel, 0)
    max_exact = N_BUCKETS // 2
    nf = jnp.maximum(n, 1).astype(jnp.float32)
    large = max_exact + (jnp.log(nf / max_exact) / math.log(MAX_DISTANCE / max_exact)
                         * (N_BUCKETS - max_exact)).astype(jnp.int32)
    large = jnp.minimum(large, N_BUCKETS - 1)
    return jnp.where(n < max_exact, n, large)


def to_blocks(t, nb):
    return jnp.moveaxis(t.reshape(t.shape[0], nb, Q_BLOCK, *t.shape[2:]), 1, 0)


def from_blocks(t):
    t = jnp.moveaxis(t, 0, 1)
    return t.reshape(t.shape[0], t.shape[1] * t.shape[2], *t.shape[3:])


def dsa_mixer(h, w_in, q_gain, k_gain, w_out, rel_bias):
    B, L, _ = h.shape
    nb = L // Q_BLOCK
    k_sel = min(TOPK_MAX, L // 4)
    q, k, v, q_idx, k_idx, w_idx = split_cols(
        h @ w_in, [A_HEADS * A_HEAD_DIM, A_HEAD_DIM, A_HEAD_DIM, IDX_HEADS * IDX_DIM, IDX_DIM, IDX_HEADS])
    q = rms_norm(q.reshape(B, L, A_HEADS, A_HEAD_DIM), q_gain)
    k = rms_norm(k, k_gain)
    q_idx = q_idx.reshape(B, L, IDX_HEADS, IDX_DIM)
    w_idx = w_idx * IDX_HEADS ** -0.5
    key_pos = jnp.arange(L, dtype=jnp.int32)
    q_pos = key_pos.reshape(nb, Q_BLOCK)
    scale = A_HEAD_DIM ** -0.5

    def block(args):
        qb, qib, wb, pos = args
        s_idx = jnp.einsum('bqhd,bsd->bqhs', qib, k_idx) * IDX_DIM ** -0.5
        score = jnp.einsum('bqh,bqhs->bqs', wb, jax.nn.relu(s_idx)).astype(jnp.float32)
        admissible = key_pos[None, :] <= pos[:, None]
        score = jnp.where(admissible[None], score, -jnp.inf)
        _, sel = lax.top_k(score, k_sel)
        kg = jax.vmap(lambda kk, ii: kk[ii])(k, sel)
        vg = jax.vmap(lambda vv, ii: vv[ii])(v, sel)
        rel = pos[None, :, None] - sel
        bias = jnp.moveaxis(rel_bias[t5_bucket(rel)], -1, 1)
        logits = jnp.einsum('bqhd,bqjd->bhqj', qb, kg).astype(jnp.float32) * scale + bias
        logits = jnp.where((rel >= 0)[:, None], logits, -jnp.inf)
        p = jax.nn.softmax(logits, axis=-1).astype(vg.dtype)
        return jnp.einsum('bhqj,bqjd->bqhd', p, vg)

    out = lax.map(block, (to_blocks(q, nb), to_blocks(q_idx, nb), to_blocks(w_idx, nb), q_pos))
    return from_blocks(out).reshape(B, L, A_HEADS * A_HEAD_DIM) @ w_out


def swa_mixer(h, w_in, q_gain, k_gain, sinks, w_out, rel_bias):
    B, L, _ = h.shape
    nb = L // WINDOW
    group = B_HEADS // B_KV_HEADS
    q, k, v = split_cols(h @ w_in, [B_HEADS * B_HEAD_DIM, B_KV_HEADS * B_HEAD_DIM, B_KV_HEADS * B_HEAD_DIM])
    q = rms_norm(q.reshape(B, L, B_HEADS, B_HEAD_DIM), q_gain).reshape(B, L, B_KV_HEADS, group, B_HEAD_DIM)
    k = rms_norm(k.reshape(B, L, B_KV_HEADS, B_HEAD_DIM), k_gain)
    v = v.reshape(B, L, B_KV_HEADS, B_HEAD_DIM)

    def band(t):
        tb = t.reshape(B, nb, WINDOW, B_KV_HEADS, B_HEAD_DIM)
        prev = jnp.pad(tb[:, :-1], ((0, 0), (1, 0), (0, 0), (0, 0), (0, 0)))
        return jnp.moveaxis(jnp.concatenate([prev, tb], axis=2), 1, 0)

    kb, vb = band(k), band(v)
    qi = jnp.arange(WINDOW, dtype=jnp.int32)[:, None] + WINDOW
    kj = jnp.arange(2 * WINDOW, dtype=jnp.int32)[None, :]
    rel = qi - kj
    in_band = (rel >= 0) & (rel < WINDOW)
    bias = jnp.transpose(rel_bias[t5_bucket(rel)], (2, 0, 1)).reshape(B_KV_HEADS, group, WINDOW, 2 * WINDOW)
    sink = sinks.astype(jnp.float32).reshape(B_KV_HEADS, group, 1, 1)
    scale = B_HEAD_DIM ** -0.5

    def block(args):
        qb, kbb, vbb, n = args
        logits = jnp.einsum('bqkgd,bjkd->bkgqj', qb, kbb).astype(jnp.float32) * scale + bias
        ok = in_band & ((n > 0) | (kj >= WINDOW))
        logits = jnp.where(ok, logits, -jnp.inf)
        m = jnp.maximum(jnp.max(logits, axis=-1, keepdims=True), sink)
        e = jnp.exp(logits - m)
        p = e / (jnp.sum(e, axis=-1, keepdims=True) + jnp.exp(sink - m))
        return jnp.einsum('bkgqj,bjkd->bqkgd', p.astype(vbb.dtype), vbb)

    out = lax.map(block, (to_blocks(q, nb), kb, vb, jnp.arange(nb, dtype=jnp.int32)))
    return from_blocks(out).reshape(B, L, B_HEADS * B_HEAD_DIM) @ w_out


def apply_rope(t, cos, sin):
    t1, t2 = jnp.split(t, 2, axis=-1)
    return jnp.concatenate([t1 * cos - t2 * sin, t2 * cos + t1 * sin], axis=-1).astype(t.dtype)


def mla_mixer(h, positions, w_in, q_a_gain, w_q_b, kv_a_gain, w_kv_b, q_gain, k_gain, w_out):
    B, L, _ = h.shape
    nb = L // Q_BLOCK
    q_lat, kv_lat, k_rope = split_cols(h @ w_in, [C_Q_RANK, C_KV_RANK, C_ROPE])
    q = (rms_norm(q_lat, q_a_gain) @ w_q_b).reshape(B, L, C_HEADS, C_QK)
    kv = (rms_norm(kv_lat, kv_a_gain) @ w_kv_b).reshape(B, L, C_HEADS, C_NOPE + C_V)
    k_nope, v = split_cols(kv, [C_NOPE, C_V])
    k = jnp.concatenate([k_nope, jnp.broadcast_to(k_rope[:, :, None, :], (B, L, C_HEADS, C_ROPE))], axis=-1)
    q = rms_norm(q, q_gain)
    k = rms_norm(k, k_gain)
    inv_freq = ROPE_THETA ** (-jnp.arange(0, C_ROPE, 2, dtype=jnp.float32) / C_ROPE)
    ang = positions.astype(jnp.float32)[..., None] * inv_freq
    cos, sin = jnp.cos(ang)[:, :, None, :], jnp.sin(ang)[:, :, None, :]
    q = jnp.concatenate([q[..., :C_NOPE], apply_rope(q[..., C_NOPE:], cos, sin)], axis=-1)
    k = jnp.concatenate([k[..., :C_NOPE], apply_rope(k[..., C_NOPE:], cos, sin)], axis=-1)
    key_pos = jnp.arange(L, dtype=jnp.int32)
    scale = C_QK ** -0.5

    def block(args):
        qb, pos = args
        logits = jnp.einsum('bqhd,bshd->bhqs', qb, k).astype(jnp.float32) * scale
        logits = jnp.where(key_pos[None, :] <= pos[:, None], logits, -jnp.inf)
        p = jax.nn.softmax(logits, axis=-1).astype(v.dtype)
        return jnp.einsum('bhqs,bshd->bqhd', p, v)

    out = lax.map(block, (to_blocks(q, nb), key_pos.reshape(nb, Q_BLOCK)))
    return from_blocks(out).reshape(B, L, C_HEADS * C_V) @ w_out


def sqrelu_mlp(h, w_up, w_down):
    return jnp.square(jax.nn.relu(h @ w_up)) @ w_down


def setup_inputs(seed: int = 0) -> dict:
    key = jax.random.key(seed)
    ks = jax.random.split(key, 24)
    f32 = jnp.float32

    def w(k, shape, fan_in):
        return jax.random.normal(k, shape, f32) * fan_in ** -0.5

    def gain(k, shape):
        return 1.0 + 0.02 * jax.random.normal(k, shape, f32)

    x = jax.random.normal(ks[0], (BATCH, SEQ, D_MODEL), f32)
    start = jax.random.randint(ks[1], (BATCH, 1), 0, 1024, dtype=jnp.int32)
    positions = start + jnp.arange(SEQ, dtype=jnp.int32)[None, :]
    return {
        'x': x,
        'positions': positions,
        'rel_bias': 0.5 * jax.random.normal(ks[2], (N_BUCKETS, N_BIAS_HEADS), f32),
        'norm_mix': gain(ks[3], (DEPTH, D_MODEL)),
        'norm_mlp': gain(ks[4], (DEPTH, D_MODEL)),
        'w_up': w(ks[5], (DEPTH, D_MODEL, D_FF), D_MODEL),
        'w_down': w(ks[6], (DEPTH, D_FF, D_MODEL), D_FF),
        'a_w_in': w(ks[7], (N_A, D_MODEL, A_IN), D_MODEL),
        'a_q_gain': gain(ks[8], (N_A, A_HEAD_DIM)),
        'a_k_gain': gain(ks[9], (N_A, A_HEAD_DIM)),
        'a_w_out': w(ks[10], (N_A, A_HEADS * A_HEAD_DIM, D_MODEL), A_HEADS * A_HEAD_DIM),
        'b_w_in': w(ks[11], (N_B, D_MODEL, B_IN), D_MODEL),
        'b_q_gain': gain(ks[12], (N_B, B_HEAD_DIM)),
        'b_k_gain': gain(ks[13], (N_B, B_HEAD_DIM)),
        'b_sinks': jax.random.normal(ks[14], (N_B, B_HEADS), f32),
        'b_w_out': w(ks[15], (N_B, B_HEADS * B_HEAD_DIM, D_MODEL), B_HEADS * B_HEAD_DIM),
        'c_w_in': w(ks[16], (N_C, D_MODEL, C_IN), D_MODEL),
        'c_q_a_gain': gain(ks[17], (N_C, C_Q_RANK)),
        'c_w_q_b': w(ks[18], (N_C, C_Q_RANK, C_HEADS * C_QK), C_Q_RANK),
        'c_kv_a_gain': gain(ks[19], (N_C, C_KV_RANK)),
        'c_w_kv_b': w(ks[20], (N_C, C_KV_RANK, C_HEADS * (C_NOPE + C_V)), C_KV_RANK),
        'c_q_gain': gain(ks[21], (N_C, C_QK)),
        'c_k_gain': gain(ks[22], (N_C, C_QK)),
        'c_w_out': w(ks[23], (N_C, C_HEADS * C_V, D_MODEL), C_HEADS * C_V),
    }


def reference(x, positions, rel_bias, norm_mix, norm_mlp, w_up, w_down,
              a_w_in, a_q_gain, a_k_gain, a_w_out,
              b_w_in, b_q_gain, b_k_gain, b_sinks, b_w_out,
              c_w_in, c_q_a_gain, c_w_q_b, c_kv_a_gain, c_w_kv_b, c_q_gain, c_k_gain, c_w_out):
    ia = ib = ic = 0
    for i in range(DEPTH):
        h = rms_norm(x, norm_mix[i])
        kind = i % N_MIXERS
        if kind == 0:
            y = dsa_mixer(h, a_w_in[ia], a_q_gain[ia], a_k_gain[ia], a_w_out[ia], rel_bias)
            ia += 1
        elif kind == 1:
            y = swa_mixer(h, b_w_in[ib], b_q_gain[ib], b_k_gain[ib], b_sinks[ib], b_w_out[ib], rel_bias)
            ib += 1
        else:
            y = mla_mixer(h, positions, c_w_in[ic], c_q_a_gain[ic], c_w_q_b[ic], c_kv_a_gain[ic],
                          c_w_kv_b[ic], c_q_gain[ic], c_k_gain[ic], c_w_out[ic])
            ic += 1
        x = x + y
        x = x + sqrelu_mlp(rms_norm(x, norm_mlp[i]), w_up[i], w_down[i])
    return x
```

```python
import math
from contextlib import ExitStack

import numpy as np
import concourse.bass as bass
import concourse.mybir as mybir
from concourse.bass_utils import run_bass_kernel_spmd

F32 = mybir.dt.float32
BF16 = mybir.dt.bfloat16
I32 = mybir.dt.int32
AF = mybir.ActivationFunctionType
ALU = mybir.AluOpType
AX = mybir.AxisListType

D = 1024
DFF = 4096
EPS = 1e-6
NEG = -30000.0
NCORES = 8


class Op:
    __slots__ = ("idx", "eng", "eidx", "fn", "cdeps", "dwaits", "is_dma", "dkey", "dh", "sig", "sigidx")


class Sched:
    ENGS = ("pe", "act", "dve", "pool", "sp")

    def __init__(self, nc):
        self.nc = nc
        self.ops = []
        self.eops = {e: [] for e in self.ENGS}
        self.kw = {}
        self.kr = {}
        self.dsem = {}
        self.excl = set()
        self.pending = {}
        self.free_dsems = {"sp": [], "pool": [], "act": []}

    def add(self, eng, fn, r=(), w=(), dma=None):
        op = Op()
        op.idx = len(self.ops)
        op.eng = eng
        op.eidx = len(self.eops[eng])
        op.fn = fn
        op.is_dma = dma is not None
        op.dkey = dma
        op.cdeps = {}
        op.dwaits = {}
        op.sig = False
        op.sigidx = 0

        def dep(p, kind):
            if p is None or p is op:
                return
            if p.is_dma:
                if op.is_dma and p.dkey == dma and kind == "waw":
                    return
                ent = self.dsem.get(p.dkey)
                if ent is None or ent[0] is not p.dh:
                    return
                op.dwaits[id(ent[0])] = (ent[0], ent[1])
                return
            if p.eng == eng and not op.is_dma:
                if eng == "pe":
                    return
            cur = op.cdeps.get(p.eng)
            if cur is None or p.eidx > cur.eidx:
                op.cdeps[p.eng] = p

        for k in r:
            dep(self.kw.get(k), "raw")
            rd = self.kr.setdefault(k, {})
            if k in self.excl:
                for q in rd.values():
                    if q.eng != eng:
                        dep(q, "war")
            rd[("d", dma) if op.is_dma else ("c", eng)] = op
        for k in w:
            dep(self.kw.get(k), "waw")
            for q in self.kr.get(k, {}).values():
                dep(q, "war")
            self.kw[k] = op
            self.kr[k] = {}
        pb = self.pending.pop(eng, None)
        if pb is not None:
            for p in pb[0]:
                if p.eng != eng:
                    dep(p, "raw")
            for h, c in pb[1]:
                old = op.dwaits.get(id(h))
                if old is None or old[1] < c:
                    op.dwaits[id(h)] = (h, c)
        if op.is_dma:
            if dma not in self.dsem:
                if self.free_dsems[eng]:
                    self.dsem[dma] = self.free_dsems[eng].pop()
                else:
                    self.nsem = getattr(self, "nsem", 0) + 1
                    self.dsem[dma] = [self.nc.alloc_semaphore("d_%d" % self.nsem), 0, eng]
            self.dsem[dma][1] += 16
            op.dh = self.dsem[dma][0]
        self.ops.append(op)
        self.eops[eng].append(op)
        return op

    def barrier(self):
        last = [l[-1] for l in self.eops.values() if l and not l[-1].is_dma]
        for l in self.eops.values():
            for o in reversed(l):
                if not o.is_dma:
                    if o not in last:
                        last.append(o)
                    break
        dw = [(v[0], v[1]) for v in self.dsem.values()]
        for e in self.ENGS:
            old = self.pending.get(e)
            if old is None:
                self.pending[e] = (list(last), list(dw))
            else:
                old[0].extend(last)
                old[1].extend(dw)

    def recycle(self, keep=()):
        for k in list(self.dsem.keys()):
            if k in keep:
                continue
            ent = self.dsem.pop(k)
            self.free_dsems[ent[2]].append(ent)

    def emit(self):
        nc = self.nc
        for op in self.ops:
            for p in op.cdeps.values():
                p.sig = True
        esem = {}
        for e in self.ENGS:
            n = 0
            for op in self.eops[e]:
                if op.sig:
                    n += 1
                    op.sigidx = n
            if n:
                esem[e] = nc.alloc_semaphore("e_" + e)
        sched = self

        def run(e, eng):
            waited = {}
            for op in sched.eops[e]:
                for pe_, p in op.cdeps.items():
                    if waited.get(pe_, 0) < p.sigidx:
                        eng.wait_ge(esem[pe_], p.sigidx)
                        waited[pe_] = p.sigidx
                for hid, (h, c) in op.dwaits.items():
                    if waited.get(hid, 0) < c:
                        eng.wait_ge(h, c)
                        waited[hid] = c
                ins = op.fn(eng)
                if op.is_dma:
                    ins.then_inc(op.dh, 16)
                elif op.sig:
                    ins.then_inc(esem[e], 1)

        with nc.Block() as block:
            @block.tensor
            def _(eng):
                run("pe", eng)

            @block.scalar
            def _(eng):
                run("act", eng)

            @block.vector
            def _(eng):
                run("dve", eng)

            @block.gpsimd
            def _(eng):
                run("pool", eng)

            @block.sync
            def _(eng):
                run("sp", eng)


def _t5_bucket(n):
    n = np.maximum(n, 0)
    nf = np.maximum(n, 1).astype(np.float32)
    large = 16 + (np.log(nf / np.float32(16)) / np.float32(math.log(128 / 16)) * np.float32(16)).astype(np.int32)
    large = np.minimum(large, 31)
    return np.where(n < 16, n, large)


TW = 384


def _host_consts():
    c = {}
    c["ident"] = np.eye(128, dtype=np.float32)
    rel = np.arange(TW) - 127
    b = _t5_bucket(rel)
    oh = np.zeros((2, 33, TW), np.float32)
    for v in range(2):
        for r in range(TW):
            if rel[r] >= 0:
                oh[v, b[r], r] += 1.0
                oh[v, 31, r] -= 1.0
            masked = rel[r] < 0 or (v == 1 and rel[r] >= 128)
            oh[v, 32, r] = NEG if masked else 0.0
    c["t5oh"] = oh
    t = np.arange(128)
    c["caus_ts"] = np.where(t[None, :] <= t[:, None], 0.0, -1e30).astype(np.float32)
    c["caus_st"] = np.where(t[:, None] <= t[None, :], 0.0, NEG).astype(np.float32)
    inv_freq = (10000.0 ** (-np.arange(0, 32, 2, dtype=np.float32) / np.float32(32))).astype(np.float32)
    c["invf"] = np.broadcast_to(inv_freq[None, :], (128, 16)).copy()
    return c


class Builder:
    def __init__(self, phases, SEQ, NSEQ, topk):
        self.phases = phases
        self.SEQ = SEQ
        self.NSEQ = NSEQ
        self.NT = SEQ // 128
        self.TOK = SEQ * NSEQ
        self.topk = topk
        self.nc = bass.Bass("TRN2", target_bir_lowering=False)
        self.S = Sched(self.nc)
        self.uid = 0
        self.dram_in = {}
        self.sb_off = 16512
        self.sb_peak = 0
        self.sb_cap = 229312

    def din(self, name, shape, dt=F32):
        if name not in self.dram_in:
            self.dram_in[name] = self.nc.dram_tensor(name, list(shape), dt, kind="ExternalInput").ap()
        return self.dram_in[name]

    def sb(self, es, name, shape, dt):
        self.uid += 1
        esz = 2 if dt == BF16 else 4
        n = 1
        for d in shape[1:]:
            n *= d
        nbytes = (n * esz + 63) // 64 * 64
        off = self.sb_off
        self.sb_off += nbytes
        assert self.sb_off <= self.sb_cap, "SBUF overflow: %d > %d (%s)" % (self.sb_off, self.sb_cap, name)
        self.sb_peak = max(self.sb_peak, self.sb_off)
        return self.nc.alloc_sbuf_tensor_at("%s_%d" % (name, self.uid), list(shape), dt, offset=off)

    def op(self, eng, fn, r=(), w=(), dma=None):
        return self.S.add(eng, fn, r, w, dma)

    def dma_in(self, dst_ap, src_ap, tkey, r=(), w=(), eng="sp"):
        return self.op(eng, lambda e: e.dma_start(out=dst_ap, in_=src_ap), r=r, w=tuple(w) + (tkey,), dma=tkey)

    def dma_out(self, dst_ap, src_ap, tkey, r=(), w=(), eng="sp"):
        return self.op(eng, lambda e: e.dma_start(out=dst_ap, in_=src_ap), r=tuple(r) + (tkey,), w=w, dma=tkey)

    def setup_psum(self, es):
        self.banks = []
        for i in range(8):
            t = self.nc.alloc_psum_tensor("bank%d" % i, [128, 512], F32)
            self.banks.append(t)
            self.S.excl.add("bank%d" % i)
        self.grot = 0
        self.ng = 3

    def gbank(self):
        i = self.grot % self.ng
        self.grot += 1
        return i

    def setup_consts(self, es):
        hc = _host_consts()
        self.host_consts = hc
        nc = self.nc
        ident_f = self.sb(es, "identf", [128, 128], F32)
        self.ident = self.sb(es, "ident", [128, 128], BF16)
        d = self.din("k_ident", [128, 128])
        self.dma_in(ident_f[:], d[:, :], "identf")
        self.op("dve", lambda e: e.tensor_copy(out=self.ident[:], in_=ident_f[:]), r=("identf",), w=("ident",))
        self.eps_t = self.sb(es, "eps", [128, 1], F32)
        self.op("dve", lambda e: e.memset(self.eps_t[:], EPS), w=("eps",))

    def transpose_to(self, src_tile, src_key, col_offs, width, dst3, dst_key, dst_blk0=0, evac_eng="act"):
        n = len(col_offs)
        i0 = 0
        while i0 < n:
            cnt = min(8, n - i0)
            b = self.gbank()
            bk = "bank%d" % b
            pb = self.banks[b].bitcast(BF16)
            for j in range(cnt):
                off = col_offs[i0 + j]
                self.op("pe", lambda e, j=j, off=off, pb=pb: e.transpose(
                    out=pb[0:width, j * 128:(j + 1) * 128], in_=src_tile[:, off:off + width], identity=self.ident[:]),
                    r=(src_key, "ident"), w=(bk,))
            dst = dst3[0:width, dst_blk0 + i0:dst_blk0 + i0 + cnt, :]
            src = pb[0:width, 0:cnt * 128].rearrange("p (a b) -> p a b", b=128)
            if evac_eng == "act":
                self.op("act", lambda e, dst=dst, src=src: e.copy(out=dst, in_=src), r=(bk,), w=(dst_key,))
            else:
                self.op("dve", lambda e, dst=dst, src=src: e.tensor_copy(out=dst, in_=src), r=(bk,), w=(dst_key,))
            i0 += cnt

    def transpose_full(self, src_tile, src_key, nblk, dstT, dst_key, evac_eng="act"):
        i0 = 0
        while i0 < nblk:
            cnt = min(8, nblk - i0)
            b = self.gbank()
            bk = "bank%d" % b
            pb = self.banks[b].bitcast(BF16)
            for j in range(cnt):
                off = (i0 + j) * 128
                self.op("pe", lambda e, j=j, off=off, pb=pb: e.transpose(
                    out=pb[:, j * 128:(j + 1) * 128], in_=src_tile[:, off:off + 128], identity=self.ident[:]),
                    r=(src_key, "ident"), w=(bk,))
            dst = dstT[:, i0:i0 + cnt, :]
            src = pb[:, 0:cnt * 128].rearrange("p (a b) -> p a b", b=128)
            if evac_eng == "act":
                self.op("act", lambda e, dst=dst, src=src: e.copy(out=dst, in_=src), r=(bk,), w=(dst_key,))
            else:
                self.op("dve", lambda e, dst=dst, src=src: e.tensor_copy(out=dst, in_=src), r=(bk,), w=(dst_key,))
            i0 += cnt

    def linear(self, xT, xT_key, nk, W, W_key, N, evac):
        n0 = 0
        while n0 < N:
            wd = min(512, N - n0)
            b = self.gbank()
            bk = "bank%d" % b
            ps = self.banks[b]
            for k in range(nk):
                self.op("pe", lambda e, k=k, n0=n0, wd=wd, ps=ps: e.matmul(
                    ps[:, 0:wd], lhsT=xT[:, k, :], rhs=W[:, k, n0:n0 + wd], start=(k == 0), stop=(k == nk - 1)),
                    r=(xT_key, W_key), w=(bk,))
            evac(b, bk, n0, wd)
            n0 += wd

    def load_weight(self, es, name, dram_ap2d, K, N):
        nk = max(1, K // 128)
        kp = min(K, 128)
        t = self.sb(es, name, [kp, nk, N], BF16)
        key = name + "_%d" % self.uid
        src = dram_ap2d.rearrange("(k p) n -> p k n", p=kp)
        step = max(1, min(nk, 8192 // N if N <= 8192 else 1))
        k0 = 0
        while k0 < nk:
            k1 = min(nk, k0 + step)
            self.dma_in(t[:, k0:k1, :], src[:, k0:k1, :], key, eng="pool")
            k0 = k1
        return t, key

    def load_bcast_row(self, es, name, dram_row_ap, n):
        t = self.sb(es, name, [128, n], F32)
        key = name + "_%d" % self.uid
        src = bass.AP(tensor=dram_row_ap.tensor, offset=dram_row_ap.offset, ap=[[0, 128], [1, n]])
        self.dma_in(t[:], src, key)
        return t, key

    def rms_scale(self, es_tiles, ssq_ap, ssq_key, out_ap, out_key, n, shape_cols):
        self.op("act", lambda e: e.activation(out=out_ap, in_=ssq_ap, func=AF.Ln, bias=self.eps_t[:, 0:1], scale=1.0 / n),
                r=(ssq_key, "eps"), w=(out_key,))
        self.op("act", lambda e: e.activation(out=out_ap, in_=out_ap, func=AF.Exp, scale=-0.5),
                r=(out_key,), w=(out_key,))

    def rot(self, es, name, shape, dt, n=2):
        tiles = []
        for i in range(n):
            t = self.sb(es, name + str(i), shape, dt)
            tiles.append((t, "%s%d_%d" % (name, i, self.uid)))
        return Rot(tiles)

    def tile_norm(self, xt, xk, st, sk, col, g, gk, h, hk, junk):
        ssq = st[:, col:col + 1]
        rstd = st[:, col + 1:col + 2]
        self.op("act", lambda e: e.activation(out=junk[:], in_=xt[:], func=AF.Square, accum_out=ssq), r=(xk, sk), w=(sk,))
        self.rms_scale(None, ssq, sk, rstd, sk, D, 1)
        self.op("dve", lambda e: e.scalar_tensor_tensor(out=h[:], in0=xt[:], scalar=rstd, in1=g[:], op0=ALU.mult,
                                                        op1=ALU.mult), r=(xk, sk, gk), w=(hk,))

    def phase_mlp(self, li, x_src, src_id, x_dst, dst_id):
        S = self.S
        ntile = self.TOK // 128
        self.ng = 8
        w_up = self.din("w_up", [4, D, DFF])
        w_down = self.din("w_down", [4, DFF, D])
        norm_mlp = self.din("norm_mlp", [4, D])
        mark = self.sb_off
        with ExitStack() as es:
            g, gk = self.load_bcast_row(es, "gml", norm_mlp[li, :], D)
            wup, wupk = self.load_weight(es, "wup", w_up[li], D, DFF)
            wdn, wdnk = self.load_weight(es, "wdn", w_down[li], DFF, D)
            xts = self.rot(es, "xt", [128, D], F32, 2)
            hs = self.rot(es, "h", [128, D], BF16, 2)
            hTs = self.rot(es, "hT", [128, 8, 128], BF16, 2)
            sts = self.rot(es, "st", [128, 4], F32, 2)
            rls = self.rot(es, "rl", [128, 512], F32, 3)
            hids = self.rot(es, "hid", [128, DFF], BF16, 2)
            hidTs = self.rot(es, "hidT", [128, 32, 128], BF16, 2)
            xos = self.rot(es, "xo", [128, D], F32, 2)
            junk = self.sb(es, "junk", [128, D], BF16)

            def load(ti):
                xt, xk = xts.get(ti)
                self.dma_in(xt[:], x_src[ti * 128:(ti + 1) * 128, :], xk, r=(("xd", src_id, ti),))

            load(0)
            for ti in range(ntile):
                if ti + 1 < ntile:
                    load(ti + 1)
                xt, xk = xts.get(ti)
                h, hk = hs.get(ti)
                hT, hTk = hTs.get(ti)
                st, sk = sts.get(ti)
                hid, hidk = hids.get(ti)
                hidT, hidTk = hidTs.get(ti)
                xo, xok = xos.get(ti)
                self.tile_norm(xt, xk, st, sk, 0, g, gk, h, hk, junk)
                self.transpose_full(h, hk, 8, hT, hTk)

                def evac_up(b, bk, n0, wd, hid=hid, hidk=hidk):
                    rl, rlk = rls.next()
                    ps = self.banks[b]
                    self.op("act", lambda e: e.activation(out=rl[:, 0:wd], in_=ps[:, 0:wd], func=AF.Relu), r=(bk,), w=(rlk,))
                    self.op("dve", lambda e: e.tensor_tensor(out=hid[:, n0:n0 + wd], in0=rl[:, 0:wd], in1=rl[:, 0:wd],
                                                             op=ALU.mult), r=(rlk,), w=(hidk,))

                self.linear(hT, hTk, 8, wup, wupk, DFF, evac_up)
                self.transpose_full(hid, hidk, 32, hidT, hidTk)

                def evac_dn(b, bk, n0, wd, xo=xo, xok=xok, xt=xt, xk=xk):
                    ps = self.banks[b]
                    self.op("dve", lambda e: e.tensor_tensor(out=xo[:, n0:n0 + wd], in0=ps[:, 0:wd], in1=xt[:, n0:n0 + wd],
                                                             op=ALU.add), r=(bk, xk), w=(xok,))

                self.linear(hidT, hidTk, 32, wdn, wdnk, D, evac_dn)
                self.dma_out(x_dst[ti * 128:(ti + 1) * 128, :], xo[:], xok, w=(("xd", dst_id, ti),))
        S.barrier()
        S.recycle()
        self.sb_off = mark


class Rot:
    def __init__(self, tiles):
        self.tiles = tiles
        self.i = 0

    def get(self, i):
        return self.tiles[i % len(self.tiles)]

    def next(self):
        t = self.tiles[self.i % len(self.tiles)]
        self.i += 1
        return t


def build_program(phases, SEQ, NSEQ, topk):
    B = Builder(phases, SEQ, NSEQ, topk)
    nc = B.nc
    TOK = B.TOK
    B.setup_psum(None)
    B.setup_consts(None)
    x_in = B.din("x", [TOK, D])
    out = nc.dram_tensor("out", [TOK, D], F32, kind="ExternalOutput").ap()
    scr = [nc.dram_tensor("xs%d" % i, [TOK, D], F32, kind="Internal").ap() for i in range(2)] if len(phases) > 1 else []
    cur, cur_id = x_in, "in"
    for pi, (kind, li) in enumerate(phases):
        last = pi == len(phases) - 1
        dst, dst_id = (out, "out") if last else (scr[pi % 2], "s%d_%d" % (pi % 2, pi))
        if kind == "mlp":
            B.phase_mlp(li, cur, cur_id, dst, dst_id)
        elif kind == "a":
            B.phase_a(li, li // 3, cur, cur_id, dst, dst_id)
        elif kind == "b":
            B.phase_b(li, cur, cur_id, dst, dst_id)
        elif kind == "c":
            B.phase_c(li, cur, cur_id, dst, dst_id)
        cur, cur_id = dst, dst_id
    B.S.barrier()
    B.op("sp", lambda e: e.nop())
    B.S.emit()
    return B


def const_inputs(B):
    hc = B.host_consts
    m = {}
    for name in B.dram_in:
        if name.startswith("k_"):
            m[name] = np.ascontiguousarray(hc[name[2:]])
    return m


def _setup_bias(self):
    if getattr(self, "bias_ready", False):
        return
    self.bias_ready = True
    nc = self.nc
    rel_bias = self.din("rel_bias", [32, 16])
    oh_d = self.din("k_t5oh", [2, 33, TW])
    rb = self.sb(None, "rbaug", [33, 16], F32)
    self.op("dve", lambda e: e.memset(rb[32:33, :], 1.0), w=("rbaug",))
    self.dma_in(rb[0:32, :], rel_bias[:, :], "rbaug")
    oh = self.sb(None, "t5oh", [33, 2, TW], F32)
    self.dma_in(oh[:], oh_d.rearrange("v b r -> b v r"), "t5oh")
    fsb = self.sb(None, "fsb", [16, 2, TW], F32)
    fd = nc.dram_tensor("fd_scr", [2, 16, TW], F32, kind="Internal").ap()
    for v in range(2):
        b = self.gbank()
        bk = "bank%d" % b
        ps = self.banks[b]
        self.op("pe", lambda e, v=v, ps=ps: e.matmul(ps[0:16, 0:TW], lhsT=rb[:, :], rhs=oh[:, v, :], start=True, stop=True),
                r=("rbaug", "t5oh"), w=(bk,))
        self.op("dve", lambda e, v=v, ps=ps: e.tensor_copy(out=fsb[:, v, :], in_=ps[0:16, 0:TW]), r=(bk,), w=("fsb",))
    self.dma_out(fd.rearrange("v h r -> h v r"), fsb[:], "fsb", w=("fd",))
    self.fd = fd


def _load_bias_tile(self, name, v, delta):
    fd = self.fd
    tile = self.sb(None, name, [128, 16, 128], F32)
    key = "%s_%d" % (name, self.uid)
    for s_ in range(128):
        src = bass.AP(tensor=fd.tensor, offset=v * 16 * TW + 127 - s_ + 128 * delta, ap=[[0, 1], [TW, 16], [1, 128]])
        self.dma_in(tile[s_:s_ + 1, :, :], src, key, r=("fd",))
    return tile, key


def _proj_front(self, ti, xts, hs, hTs, sts, prs, g, gk, win, wink, nin, x_src, src_id, junk):
    xt, xk = xts.get(ti)
    h, hk = hs.get(ti)
    hT, hTk = hTs.get(ti)
    st, sk = sts.get(ti)
    pr, prk = prs.get(ti)
    self.tile_norm(xt, xk, st, sk, 0, g, gk, h, hk, junk)
    self.transpose_full(h, hk, 8, hT, hTk)

    def evac(b, bk, n0, wd):
        ps = self.banks[b]
        self.op("act", lambda e: e.copy(out=pr[:, n0:n0 + wd], in_=ps[:, 0:wd]), r=(bk,), w=(prk,))

    self.linear(hT, hTk, 8, win, wink, nin, evac)
    return xt, xk, pr, prk, st, sk


def _head_norm(self, src3, srck, H, dh, gbc, gbck, st, sk, col, tmp, tmpk, out3, outk):
    t3 = tmp[:, 0:H * dh].rearrange("p (h d) -> p h d", d=dh)
    ssq = st[:, col:col + H]
    rstd = st[:, col + H:col + 2 * H]
    self.op("dve", lambda e: e.tensor_tensor(out=t3, in0=src3, in1=src3, op=ALU.mult), r=(srck,), w=(tmpk,))
    self.op("dve", lambda e: e.tensor_reduce(out=ssq, in_=t3, axis=AX.X, op=ALU.add), r=(tmpk,), w=(sk,))
    self.rms_scale(None, ssq, sk, rstd, sk, dh, H)
    self.op("dve", lambda e: e.tensor_tensor(out=t3, in0=src3, in1=rstd.unsqueeze(2).to_broadcast([128, H, dh]),
                                             op=ALU.mult), r=(srck, sk), w=(tmpk,))
    self.op("dve", lambda e: e.tensor_tensor(out=out3, in0=t3, in1=gbc[:, 0:dh].unsqueeze(1).to_broadcast([128, H, dh]),
                                             op=ALU.mult), r=(tmpk, gbck), w=(outk,))


PVB = 5


def _pv_slot(h):
    return PVB + h // 7, (h % 7) * 65


def _attend(self, QT, QTk, dk, scale, ktiles, get_kT, get_v, bias_for, mask_for, PTs, tmps):
    started = set()
    nkt = len(ktiles)
    for jj, j in enumerate(ktiles):
        groups = get_kT(j)
        v3, vk, vh = get_v(j)
        PT, PTk = PTs.next()
        m = mask_for(j) if mask_for is not None else None
        for c in range(4):
            b = 3 + (c % 2)
            bk = "bank%d" % b
            ps = self.banks[b]
            first = True
            for (kT, kk, h0, nh) in groups:
                lo, hi = max(h0, 4 * c), min(h0 + nh, 4 * c + 4)
                if lo >= hi:
                    continue
                self.op("pe", lambda e, kT=kT, lo=lo, hi=hi, ps=ps, c=c: e.matmul(
                    ps[:, (lo - 4 * c) * 128:(hi - 4 * c) * 128], lhsT=kT, rhs=QT[0:dk, lo * 128:hi * 128],
                    start=True, stop=True, skip_group_check=True), r=(kk, QTk), w=(bk,))
            bias = bias_for(j, c)
            pt_c = PT[:, c * 512:(c + 1) * 512]
            if bias is None:
                self.op("act", lambda e, ps=ps, pt_c=pt_c: e.activation(out=pt_c, in_=ps[:, :], func=AF.Exp, scale=scale),
                        r=(bk,), w=(PTk,))
            else:
                bap, bkey = bias
                tmp, tmpk = tmps.next()
                self.op("dve", lambda e, ps=ps, tmp=tmp, bap=bap: e.scalar_tensor_tensor(
                    out=tmp[:, 0:512].rearrange("p (a b) -> p a b", b=128), in0=ps[:, :].rearrange("p (a b) -> p a b", b=128),
                    scalar=scale, in1=bap, op0=ALU.mult, op1=ALU.add), r=(bk, bkey), w=(tmpk,))
                self.op("act", lambda e, tmp=tmp, pt_c=pt_c: e.activation(out=pt_c, in_=tmp[:, 0:512], func=AF.Exp),
                        r=(tmpk,), w=(PTk,))
            if m is not None:
                map_, mkey = m
                self.op("dve", lambda e, pt_c=pt_c, map_=map_: e.tensor_tensor(
                    out=pt_c.rearrange("p (a b) -> p a b", b=128), in0=pt_c.rearrange("p (a b) -> p a b", b=128),
                    in1=map_.unsqueeze(1).to_broadcast([128, 4, 128]), op=ALU.mult), r=(PTk, mkey), w=(PTk,))
        for h in range(16):
            b, off = _pv_slot(h)
            bk = "bank%d" % b
            st_flag = b not in started
            started.add(b)
            self.op("pe", lambda e, h=h, b=b, off=off, PT=PT, v3=v3, st_flag=st_flag, last=(jj == nkt - 1): e.matmul(
                self.banks[b][:, off:off + 65], lhsT=PT[:, h * 128:(h + 1) * 128], rhs=v3[:, vh(h), :],
                start=st_flag, stop=last, skip_group_check=True), r=(PTk, vk), w=(bk,))


def _attn_finish(self, st, sk, col, ao, aok, sinkexp=None):
    den = st[:, col:col + 16]
    rden = st[:, col + 16:col + 32]
    for b in range(PVB, PVB + 3):
        h0 = (b - PVB) * 7
        nh = min(7, 16 - h0)
        bk = "bank%d" % b
        o3 = self.banks[b][:, 0:nh * 65].rearrange("p (h d) -> p h d", d=65)
        self.op("dve", lambda e, o3=o3, h0=h0, nh=nh: e.tensor_copy(out=den[:, h0:h0 + nh].unsqueeze(2), in_=o3[:, :, 64:65]),
                r=(bk,), w=(sk,))
    if sinkexp is not None:
        se, sek = sinkexp
        self.op("dve", lambda e: e.tensor_tensor(out=den, in0=den, in1=se[:, 0:16], op=ALU.add), r=(sk, sek), w=(sk,))
    self.op("dve", lambda e: e.reciprocal(out=rden, in_=den), r=(sk,), w=(sk,))
    ao3 = ao[:, :].rearrange("p (h d) -> p h d", d=64)
    for b in range(PVB, PVB + 3):
        h0 = (b - PVB) * 7
        nh = min(7, 16 - h0)
        bk = "bank%d" % b
        o3 = self.banks[b][:, 0:nh * 65].rearrange("p (h d) -> p h d", d=65)
        self.op("dve", lambda e, o3=o3, h0=h0, nh=nh: e.tensor_tensor(
            out=ao3[:, h0:h0 + nh, :], in0=o3[:, :, 0:64], in1=rden[:, h0:h0 + nh].unsqueeze(2).to_broadcast([128, nh, 64]),
            op=ALU.mult), r=(bk, sk), w=(aok,))


def _out_proj(self, ao, aok, aT, aTk, wout, woutk, xt, xk, xo, xok, x_dst, dst_id, ti):
    self.transpose_full(ao, aok, 8, aT, aTk)

    def evac(b, bk, n0, wd):
        ps = self.banks[b]
        self.op("dve", lambda e: e.tensor_tensor(out=xo[:, n0:n0 + wd], in0=ps[:, 0:wd], in1=xt[:, n0:n0 + wd], op=ALU.add),
                r=(bk, xk), w=(xok,))

    self.linear(aT, aTk, 8, wout, woutk, D, evac)
    self.dma_out(x_dst[ti * 128:(ti + 1) * 128, :], xo[:], xok, w=(("xd", dst_id, ti),))


def _phase_b(self, li, x_src, src_id, x_dst, dst_id):
    S = self.S
    self.ng = 3
    _setup_bias(self)
    mark = self.sb_off
    NT = self.NT
    ib = 0
    biasD, biasDk = _load_bias_tile(self, "biasD", 0, 0)
    biasOB, biasOBk = _load_bias_tile(self, "biasOB", 1, 1)
    b_w_in = self.din("b_w_in", [1, D, 1536])
    b_w_out = self.din("b_w_out", [1, D, D])
    norm_mix = self.din("norm_mix", [4, D])
    g, gk = self.load_bcast_row(None, "gmix", norm_mix[li, :], D)
    gq, gqk = self.load_bcast_row(None, "gq", self.din("b_q_gain", [1, 64])[ib, :], 64)
    gkk_t, gkk = self.load_bcast_row(None, "gk", self.din("b_k_gain", [1, 64])[ib, :], 64)
    sk_t, skk = self.load_bcast_row(None, "sinks", self.din("b_sinks", [1, 16])[ib, :], 16)
    b31, b31k = self.load_bcast_row(None, "b31", self.din("rel_bias", [32, 16])[31, :], 16)
    self.op("dve", lambda e: e.tensor_tensor(out=sk_t[:], in0=sk_t[:], in1=b31[:], op=ALU.subtract), r=(skk, b31k), w=(skk,))
    self.op("act", lambda e: e.activation(out=sk_t[:], in_=sk_t[:], func=AF.Exp), r=(skk,), w=(skk,))
    win, wink = self.load_weight(None, "bwin", b_w_in[ib], D, 1536)
    wout, woutk = self.load_weight(None, "bwout", b_w_out[ib], D, D)
    xts = self.rot(None, "xt", [128, D], F32, 2)
    hs = self.rot(None, "h", [128, D], BF16, 2)
    hTs = self.rot(None, "hT", [128, 8, 128], BF16, 2)
    sts = self.rot(None, "st", [128, 128], F32, 2)
    prs = self.rot(None, "pr", [128, 1536], F32, 2)
    tmpn = self.rot(None, "tmpn", [128, D], F32, 2)
    qns = self.rot(None, "qn", [128, D], BF16, 2)
    kns = self.rot(None, "kn", [128, 256], BF16, 2)
    QTs = self.rot(None, "QT", [64, 16 * 128], BF16, 2)
    KTs = self.rot(None, "KT", [64, 4, 128], BF16, 2)
    VAs = self.rot(None, "VA", [128, 4, 65], BF16, 2)
    PTs = self.rot(None, "PT", [128, 16 * 128], BF16, 2)
    tmps = self.rot(None, "tmpb", [128, 512], F32, 2)
    aos = self.rot(None, "ao", [128, D], BF16, 2)
    aTs = self.rot(None, "aT", [128, 8, 128], BF16, 2)
    xos = self.rot(None, "xo", [128, D], F32, 2)
    junk = self.sb(None, "junk", [128, D], BF16)
    for (va, vak) in VAs.tiles:
        self.op("dve", lambda e, va=va: e.memset(va[:, :, 64:65], 1.0), w=(vak,))
    ntile = self.TOK // 128

    def load(ti):
        xt, xk = xts.get(ti)
        self.dma_in(xt[:], x_src[ti * 128:(ti + 1) * 128, :], xk, r=(("xd", src_id, ti),))

    load(0)
    for ti in range(ntile):
        if ti + 1 < ntile:
            load(ti + 1)
        i = ti % NT
        xt, xk, pr, prk, st, sk = _proj_front(self, ti, xts, hs, hTs, sts, prs, g, gk, win, wink, 1536, x_src, src_id, junk)
        tmp, tmpk = tmpn.get(ti)
        qn, qnk = qns.get(ti)
        kn, knk = kns.get(ti)
        QT, QTk = QTs.get(ti)
        KT, KTk = KTs.get(ti)
        VA, VAk = VAs.get(ti)
        _head_norm(self, pr[:, 0:1024].rearrange("p (h d) -> p h d", d=64), prk, 16, 64, gq, gqk, st, sk, 8, tmp, tmpk,
                   qn[:, :].rearrange("p (h d) -> p h d", d=64), qnk)
        _head_norm(self, pr[:, 1024:1280].rearrange("p (h d) -> p h d", d=64), prk, 4, 64, gkk_t, gkk, st, sk, 48, tmp, tmpk,
                   kn[:, :].rearrange("p (h d) -> p h d", d=64), knk)
        self.transpose_to(qn, qnk, [h * 64 for h in range(16)], 64, QT[:, :].rearrange("p (h t) -> p h t", t=128), QTk)
        self.transpose_to(kn, knk, [h * 64 for h in range(4)], 64, KT, KTk)
        self.op("act", lambda e, VA=VA, pr=pr: e.copy(out=VA[:, :, 0:64], in_=pr[:, 1280:1536].rearrange("p (h d) -> p h d", d=64)),
                r=(prk,), w=(VAk,))
        ktiles = ([i - 1] if i > 0 else []) + [i]

        def get_kT(j, ti=ti, i=i):
            KTj, KTjk = KTs.get(ti - (i - j))
            return [(KTj[:, kv, :], KTjk, kv * 4, 4) for kv in range(4)]

        def get_v(j, ti=ti, i=i):
            VAj, VAjk = VAs.get(ti - (i - j))
            return VAj, VAjk, (lambda h: h // 4)

        def bias_for(j, c, i=i):
            if j == i:
                return biasD[:, 4 * c:4 * c + 4, :], biasDk
            return biasOB[:, 4 * c:4 * c + 4, :], biasOBk

        _attend(self, QT, QTk, 64, 0.125, ktiles, get_kT, get_v, bias_for, None, PTs, tmps)
        ao, aok = aos.get(ti)
        aT, aTk = aTs.get(ti)
        xo, xok = xos.get(ti)
        _attn_finish(self, st, sk, 64, ao, aok, sinkexp=(sk_t, skk))
        _out_proj(self, ao, aok, aT, aTk, wout, woutk, xt, xk, xo, xok, x_dst, dst_id, ti)
    S.barrier()
    S.recycle()
    self.sb_off = mark


Builder.phase_b = _phase_b


def _rope(self, src3, srck, sc, sck, r1, r1k, r2, r2k, dst3, dstk, H):
    t1 = src3[:, :, 64:80]
    t2 = src3[:, :, 80:96]
    sin_b = sc[:, 0:16].unsqueeze(1).to_broadcast([128, H, 16])
    cos_b = sc[:, 16:32].unsqueeze(1).to_broadcast([128, H, 16])
    a = r1[:, 0:H * 16].rearrange("p (h d) -> p h d", d=16)
    b = r2[:, 0:H * 16].rearrange("p (h d) -> p h d", d=16)
    self.op("dve", lambda e: e.tensor_tensor(out=a, in0=t1, in1=cos_b, op=ALU.mult), r=(srck, sck), w=(r1k,))
    self.op("dve", lambda e: e.tensor_tensor(out=b, in0=t2, in1=sin_b, op=ALU.mult), r=(srck, sck), w=(r2k,))
    self.op("dve", lambda e: e.tensor_tensor(out=dst3[:, :, 64:80], in0=a, in1=b, op=ALU.subtract), r=(r1k, r2k), w=(dstk,))
    self.op("dve", lambda e: e.tensor_tensor(out=a, in0=t2, in1=cos_b, op=ALU.mult), r=(srck, sck), w=(r1k,))
    self.op("dve", lambda e: e.tensor_tensor(out=b, in0=t1, in1=sin_b, op=ALU.mult), r=(srck, sck), w=(r2k,))
    self.op("dve", lambda e: e.tensor_tensor(out=dst3[:, :, 80:96], in0=a, in1=b, op=ALU.add), r=(r1k, r2k), w=(dstk,))


def _phase_c(self, li, x_src, src_id, x_dst, dst_id):
    S = self.S
    nc = self.nc
    self.ng = 3
    mark = self.sb_off
    NT = self.NT
    ic = 0
    ntile = self.TOK // 128
    TWO_PI = 2.0 * math.pi
    c_w_in = self.din("c_w_in", [1, D, 416])
    c_w_q_b = self.din("c_w_q_b", [1, 256, 1536])
    c_w_kv_b = self.din("c_w_kv_b", [1, 128, 2048])
    c_w_out = self.din("c_w_out", [1, D, D])
    norm_mix = self.din("norm_mix", [4, D])
    pos_d = self.din("positions", [self.TOK, 1], I32)
    ktd = nc.dram_tensor("ktd_scr", [ntile, 96, 16 * 128], BF16, kind="Internal").ap()
    vad = nc.dram_tensor("vad_scr", [ntile, 128, 16 * 65], BF16, kind="Internal").ap()
    g, gk = self.load_bcast_row(None, "gmix", norm_mix[li, :], D)
    gqa, gqak = self.load_bcast_row(None, "gqa", self.din("c_q_a_gain", [1, 256])[ic, :], 256)
    gkva, gkvak = self.load_bcast_row(None, "gkva", self.din("c_kv_a_gain", [1, 128])[ic, :], 128)
    gq, gqk = self.load_bcast_row(None, "gq", self.din("c_q_gain", [1, 96])[ic, :], 96)
    gkg, gkgk = self.load_bcast_row(None, "gk", self.din("c_k_gain", [1, 96])[ic, :], 96)
    invf = self.sb(None, "invf", [128, 16], F32)
    self.dma_in(invf[:], self.din("k_invf", [128, 16])[:, :], "invf")
    caus = self.sb(None, "causst", [128, 128], F32)
    self.dma_in(caus[:], self.din("k_caus_st", [128, 128])[:, :], "causst")
    negpi = self.sb(None, "negpi", [128, 1], F32)
    win, wink = self.load_weight(None, "cwin", c_w_in[ic], D, 416)
    wqb, wqbk = self.load_weight(None, "cwqb", c_w_q_b[ic], 256, 1536)
    wkvb, wkvbk = self.load_weight(None, "cwkvb", c_w_kv_b[ic], 128, 2048)
    wout, woutk = self.load_weight(None, "cwout", c_w_out[ic], D, D)
    xts = self.rot(None, "xt", [128, D], F32, 2)
    hs = self.rot(None, "h", [128, D], BF16, 2)
    hTs = self.rot(None, "hT", [128, 8, 128], BF16, 2)
    sts = self.rot(None, "st", [128, 256], F32, 2)
    prs = self.rot(None, "pr", [128, 416], F32, 2)
    tmpn = self.rot(None, "tmpn", [128, 1536], F32, 1)
    lat = self.rot(None, "lat", [128, 384], BF16, 2)
    qlTs = self.rot(None, "qlT", [128, 3, 128], BF16, 2)
    q32s = self.rot(None, "q32", [128, 1536], F32, 1)
    k32s = self.rot(None, "k32", [128, 1536], F32, 1)
    qfs = self.rot(None, "qf", [128, 1536], BF16, 2)
    kfs = self.rot(None, "kf", [128, 1536], BF16, 2)
    r1s = self.rot(None, "r1", [128, 256], F32, 2)
    r2s = self.rot(None, "r2", [128, 256], F32, 2)
    posi_s = self.rot(None, "posi", [128, 1], I32, 2)
    angs = self.rot(None, "ang", [128, 64], F32, 2)
    angi = self.rot(None, "angi", [128, 32], I32, 2)
    QTs = self.rot(None, "QT", [96, 16 * 128], BF16, 2)
    KTt = self.rot(None, "KTt", [96, 16, 128], BF16, 2)
    VAt = self.rot(None, "VAt", [128, 16, 65], BF16, 2)
    KTb = self.rot(None, "KTb", [96, 16, 128], BF16, 3)
    VAb = self.rot(None, "VAb", [128, 16, 65], BF16, 3)
    PTs = self.rot(None, "PT", [128, 16 * 128], BF16, 2)
    tmps = self.rot(None, "tmpb", [128, 512], F32, 2)
    aos = self.rot(None, "ao", [128, D], BF16, 2)
    aTs = self.rot(None, "aT", [128, 8, 128], BF16, 2)
    xos = self.rot(None, "xo", [128, D], F32, 2)
    junk = self.sb(None, "junk", [128, D], BF16)
    self.op("dve", lambda e: e.memset(negpi[:], -math.pi), w=("negpi",))
    for (va, vak) in VAt.tiles:
        self.op("dve", lambda e, va=va: e.memset(va[:, :, 64:65], 1.0), w=(vak,))

    def load(ti):
        xt, xk = xts.get(ti)
        self.dma_in(xt[:], x_src[ti * 128:(ti + 1) * 128, :], xk, r=(("xd", src_id, ti),))
        pi_, pik = posi_s.get(ti)
        self.dma_in(pi_[:], pos_d[ti * 128:(ti + 1) * 128, :], pik)

    def tile_body(ti):
        i = ti % NT
        xt, xk, pr, prk, st, sk = _proj_front(self, ti, xts, hs, hTs, sts, prs, g, gk, win, wink, 416, x_src, src_id, junk)
        tmp, tmpk = tmpn.get(ti)
        la, lak = lat.get(ti)
        qlT, qlTk = qlTs.get(ti)
        q32, q32k = q32s.get(ti)
        k32, k32k = k32s.get(ti)
        qf, qfk = qfs.get(ti)
        kf, kfk = kfs.get(ti)
        r1, r1k = r1s.get(ti)
        r2, r2k = r2s.get(ti)
        VA, VAk = VAt.get(ti)
        KT, KTk = KTt.get(ti)
        QT, QTk = QTs.get(ti)
        pi_, pik = posi_s.get(ti)
        ang, angk = angs.get(ti)
        ai, aik = angi.get(ti)
        posf = ang[:, 32:33]
        a32 = ang[:, 0:32]
        kf32 = ang[:, 33:33 + 31]
        self.op("dve", lambda e: e.tensor_copy(out=posf, in_=pi_[:]), r=(pik,), w=(angk,))
        self.op("dve", lambda e: e.tensor_scalar(out=ang[:, 0:16], in0=invf[:], scalar1=posf, scalar2=None, op0=ALU.mult),
                r=(angk, "invf"), w=(angk,))
        self.op("dve", lambda e: e.tensor_scalar(out=ang[:, 16:32], in0=ang[:, 0:16], scalar1=math.pi / 2, scalar2=None,
                                                 op0=ALU.add), r=(angk,), w=(angk,))
        self.op("dve", lambda e: e.tensor_scalar(out=ang[:, 32:64], in0=a32, scalar1=1.0 / TWO_PI, scalar2=None,
                                                 op0=ALU.mult), r=(angk,), w=(angk,))
        self.op("dve", lambda e: e.tensor_copy(out=ai[:], in_=ang[:, 32:64]), r=(angk,), w=(aik,))
        self.op("dve", lambda e: e.tensor_copy(out=ang[:, 32:64], in_=ai[:]), r=(aik,), w=(angk,))
        self.op("dve", lambda e: e.scalar_tensor_tensor(out=a32, in0=ang[:, 32:64], scalar=-TWO_PI, in1=a32, op0=ALU.mult,
                                                        op1=ALU.add), r=(angk,), w=(angk,))
        self.op("dve", lambda e: e.tensor_scalar(out=ang[:, 32:64], in0=a32, scalar1=math.pi, scalar2=-TWO_PI,
                                                 op0=ALU.is_gt, op1=ALU.mult), r=(angk,), w=(angk,))
        self.op("dve", lambda e: e.tensor_tensor(out=a32, in0=a32, in1=ang[:, 32:64], op=ALU.add), r=(angk,), w=(angk,))
        self.op("dve", lambda e: e.tensor_scalar(out=ang[:, 32:64], in0=a32, scalar1=-math.pi, scalar2=TWO_PI,
                                                 op0=ALU.is_lt, op1=ALU.mult), r=(angk,), w=(angk,))
        self.op("dve", lambda e: e.tensor_tensor(out=a32, in0=a32, in1=ang[:, 32:64], op=ALU.add), r=(angk,), w=(angk,))
        self.op("act", lambda e: e.activation(out=a32, in_=a32, func=AF.Sin), r=(angk,), w=(angk,))
        sc, sck = ang, angk
        _head_norm(self, pr[:, 0:256].unsqueeze(1), prk, 1, 256, gqa, gqak, st, sk, 8, tmp, tmpk, la[:, 0:256].unsqueeze(1), lak)
        _head_norm(self, pr[:, 256:384].unsqueeze(1), prk, 1, 128, gkva, gkvak, st, sk, 12, tmp, tmpk,
                   la[:, 256:384].unsqueeze(1), lak)
        self.transpose_full(la, lak, 3, qlT, qlTk)

        def evac_q(b, bk, n0, wd):
            ps = self.banks[b]
            self.op("act", lambda e: e.copy(out=q32[:, n0:n0 + wd], in_=ps[:, 0:wd]), r=(bk,), w=(q32k,))

        self.linear(qlT, qlTk, 2, wqb, wqbk, 1536, evac_q)
        k3 = k32[:, :].rearrange("p (h d) -> p h d", d=96)

        def evac_kv(b, bk, n0, wd):
            ps3 = self.banks[b][:, 0:512].rearrange("p (h d) -> p h d", d=128)
            h0 = n0 // 128
            self.op("act", lambda e: e.copy(out=k3[:, h0:h0 + 4, 0:64], in_=ps3[:, :, 0:64]), r=(bk,), w=(k32k,))
            self.op("dve", lambda e: e.tensor_copy(out=VA[:, h0:h0 + 4, 0:64], in_=ps3[:, :, 64:128]), r=(bk,), w=(VAk,))

        self.linear(qlT[:, 2:3, :], qlTk, 1, wkvb, wkvbk, 2048, evac_kv)
        self.op("dve", lambda e: e.tensor_copy(out=k3[:, :, 64:96], in_=pr[:, 384:416].unsqueeze(1).to_broadcast([128, 16, 32])),
                r=(prk,), w=(k32k,))
        q3 = q32[:, :].rearrange("p (h d) -> p h d", d=96)
        qf3 = qf[:, :].rearrange("p (h d) -> p h d", d=96)
        kf3 = kf[:, :].rearrange("p (h d) -> p h d", d=96)
        _head_norm(self, q3, q32k, 16, 96, gq, gqk, st, sk, 16, tmp, tmpk, q3, q32k)
        _head_norm(self, k3, k32k, 16, 96, gkg, gkgk, st, sk, 48, tmp, tmpk, k3, k32k)
        self.op("act", lambda e: e.copy(out=qf3[:, :, 0:64], in_=q3[:, :, 0:64]), r=(q32k,), w=(qfk,))
        self.op("act", lambda e: e.copy(out=kf3[:, :, 0:64], in_=k3[:, :, 0:64]), r=(k32k,), w=(kfk,))
        _rope(self, q3, q32k, sc, sck, r1, r1k, r2, r2k, qf3, qfk, 16)
        _rope(self, k3, k32k, sc, sck, r1, r1k, r2, r2k, kf3, kfk, 16)
        self.transpose_to(qf, qfk, [h * 96 for h in range(16)], 96, QT[:, :].rearrange("p (h t) -> p h t", t=128), QTk)
        self.transpose_to(kf, kfk, [h * 96 for h in range(16)], 96, KT, KTk)
        self.dma_out(ktd[ti].rearrange("p (h t) -> p h t", t=128), KT[:], KTk, w=(("ktd", ti),))
        self.dma_out(vad[ti].rearrange("p (h d) -> p h d", d=65), VA[:], VAk, w=(("vad", ti),))
        ktiles = list(range(i + 1))
        base = ti - i

        def get_kT(j):
            kb, kbk = KTb.next()
            self.dma_in(kb[:], ktd[base + j].rearrange("p (h t) -> p h t", t=128), kbk, r=(("ktd", base + j),))
            return [(kb[:, h, :], kbk, h, 1) for h in range(16)]

        def get_v(j):
            vb, vbk = VAb.next()
            self.dma_in(vb[:], vad[base + j].rearrange("p (h d) -> p h d", d=65), vbk, r=(("vad", base + j),))
            return vb, vbk, (lambda h: h)

        def bias_for(j, c, i=i):
            if j == i:
                return caus[:, :].unsqueeze(1).to_broadcast([128, 4, 128]), "causst"
            return None

        _attend(self, QT, QTk, 96, 96.0 ** -0.5, ktiles, get_kT, get_v, bias_for, None, PTs, tmps)
        ao, aok = aos.get(ti)
        aT, aTk = aTs.get(ti)
        xo, xok = xos.get(ti)
        _attn_finish(self, st, sk, 96, ao, aok)
        _out_proj(self, ao, aok, aT, aTk, wout, woutk, xt, xk, xo, xok, x_dst, dst_id, ti)

    load(0)
    for ti in range(ntile):
        if ti + 1 < ntile:
            load(ti + 1)
        tile_body(ti)
    S.barrier()
    S.recycle()
    self.sb_off = mark


Builder.phase_c = _phase_c


NIT = 20


def _phase_a(self, li, ia, x_src, src_id, x_dst, dst_id):
    S = self.S
    self.ng = 3
    _setup_bias(self)
    mark = self.sb_off
    NT = self.NT
    SEQ = self.SEQ
    topk = self.topk
    ntile = self.TOK // 128
    a_w_in = self.din("a_w_in", [2, D, 1736])
    a_w_out = self.din("a_w_out", [2, D, D])
    norm_mix = self.din("norm_mix", [4, D])
    biasD, biasDk = _load_bias_tile(self, "biasD", 0, 0)
    biasOA, biasOAk = _load_bias_tile(self, "biasOA", 0, 1)
    g, gk = self.load_bcast_row(None, "gmix", norm_mix[li, :], D)
    gq, gqk = self.load_bcast_row(None, "gq", self.din("a_q_gain", [2, 64])[ia, :], 64)
    gkt, gkk = self.load_bcast_row(None, "gk", self.din("a_k_gain", [2, 64])[ia, :], 64)
    causts = self.sb(None, "causts", [128, 128], F32)
    self.dma_in(causts[:], self.din("k_caus_ts", [128, 128])[:, :], "causts")
    win, wink = self.load_weight(None, "awin", a_w_in[ia], D, 1736)
    wout, woutk = self.load_weight(None, "awout", a_w_out[ia], D, D)
    KT = self.sb(None, "KT", [64, NT, 128], BF16)
    KIT = self.sb(None, "KIT", [64, NT * 128], BF16)
    VA = self.sb(None, "VA", [128, NT, 65], BF16)
    score = self.sb(None, "score", [128, SEQ], F32)
    Mt = self.sb(None, "Mt", [128, SEQ], BF16)
    self.op("dve", lambda e: e.memset(VA[:, :, 64:65], 1.0), w=tuple(("VA", j) for j in range(NT)))
    xts = self.rot(None, "xt", [128, D], F32, 2)
    hs = self.rot(None, "h", [128, D], BF16, 2)
    hTs = self.rot(None, "hT", [128, 8, 128], BF16, 1)
    sts = self.rot(None, "st", [128, 128], F32, 2)
    prs = self.rot(None, "pr", [128, 1736], F32, 1)
    tmpn = self.rot(None, "tmpn", [128, D], F32, 1)
    qns = self.rot(None, "qn", [128, D], BF16, 2)
    kns = self.rot(None, "kn", [128, 64], BF16, 2)
    qkis = self.rot(None, "qki", [128, 576], BF16, 2)
    QTs = self.rot(None, "QT", [64, 16 * 128], BF16, 2)
    QITs = self.rot(None, "QIT", [64, 8 * 128], BF16, 2)
    rls = self.rot(None, "rl", [128, 512], F32, 3)
    MTs = self.rot(None, "MT", [128, 1, 128], BF16, 3)
    PTs = self.rot(None, "PT", [128, 16 * 128], BF16, 2)
    tmps = self.rot(None, "tmpb", [128, 512], F32, 2)
    aos = self.rot(None, "ao", [128, D], BF16, 2)
    aTs = self.rot(None, "aT", [128, 8, 128], BF16, 1)
    xos = self.rot(None, "xo", [128, D], F32, 1)
    junk = self.sb(None, "junk", [128, D], BF16)
    WS = 8.0 ** -0.5 / 8.0

    def load(ti):
        xt, xk = xts.get(ti)
        self.dma_in(xt[:], x_src[ti * 128:(ti + 1) * 128, :], xk, r=(("xd", src_id, ti),))

    def tile_body(ti):
        i = ti % NT
        nk = (i + 1) * 128
        xt, xk, pr, prk, st, sk = _proj_front(self, ti, xts, hs, hTs, sts, prs, g, gk, win, wink, 1736, x_src, src_id, junk)
        tmp, tmpk = tmpn.get(ti)
        qn, qnk = qns.get(ti)
        kn, knk = kns.get(ti)
        qki, qkik = qkis.get(ti)
        QT, QTk = QTs.get(ti)
        QIT, QITk = QITs.get(ti)
        _head_norm(self, pr[:, 0:1024].rearrange("p (h d) -> p h d", d=64), prk, 16, 64, gq, gqk, st, sk, 8, tmp, tmpk,
                   qn[:, :].rearrange("p (h d) -> p h d", d=64), qnk)
        _head_norm(self, pr[:, 1024:1088].unsqueeze(1), prk, 1, 64, gkt, gkk, st, sk, 48, tmp, tmpk, kn[:, :].unsqueeze(1), knk)
        self.op("act", lambda e: e.copy(out=qki[:], in_=pr[:, 1152:1728]), r=(prk,), w=(qkik,))
        self.transpose_to(qn, qnk, [h * 64 for h in range(16)], 64, QT[:, :].rearrange("p (h t) -> p h t", t=128), QTk)
        self.transpose_to(kn, knk, [0], 64, KT, ("KT", i), dst_blk0=i)
        self.transpose_to(qki, qkik, [h * 64 for h in range(8)], 64, QIT[:, :].rearrange("p (h t) -> p h t", t=128), QITk)
        self.transpose_to(qki, qkik, [512], 64, KIT[:, :].rearrange("p (j t) -> p j t", t=128), ("KIT", i), dst_blk0=i)
        self.op("act", lambda e: e.copy(out=VA[:, i, 0:64], in_=pr[:, 1088:1152]), r=(prk,), w=(("VA", i),))
        select = nk > topk
        if select:
            wsc = st[:, 100:108]
            lo = st[:, 110:111]
            w0 = st[:, 111:112]
            hi = st[:, 112:113]
            cnt = st[:, 113:114]
            tt = st[:, 114:115]
            mid = st[:, 115:116]
            self.op("dve", lambda e: e.tensor_scalar(out=wsc, in0=pr[:, 1728:1736], scalar1=WS, scalar2=None, op0=ALU.mult),
                    r=(prk,), w=(sk,))
            for c0 in range(0, nk, 512):
                wd = min(512, nk - c0)
                kkeys = tuple(("KIT", j) for j in range(c0 // 128, (c0 + wd) // 128))
                for h in range(8):
                    b = self.gbank()
                    bk = "bank%d" % b
                    ps = self.banks[b]
                    self.op("pe", lambda e, h=h, ps=ps, c0=c0, wd=wd: e.matmul(
                        ps[:, 0:wd], lhsT=QIT[:, h * 128:(h + 1) * 128], rhs=KIT[:, c0:c0 + wd], start=True, stop=True),
                        r=(QITk,) + kkeys, w=(bk,))
                    rl, rlk = rls.next()
                    self.op("act", lambda e, ps=ps, rl=rl, wd=wd: e.activation(out=rl[:, 0:wd], in_=ps[:, 0:wd], func=AF.Relu),
                            r=(bk,), w=(rlk,))
                    if h == 0:
                        self.op("dve", lambda e, rl=rl, c0=c0, wd=wd: e.tensor_scalar(
                            out=score[:, c0:c0 + wd], in0=rl[:, 0:wd], scalar1=wsc[:, 0:1], scalar2=None, op0=ALU.mult),
                            r=(rlk, sk), w=("score",))
                    else:
                        self.op("dve", lambda e, rl=rl, c0=c0, wd=wd, h=h: e.scalar_tensor_tensor(
                            out=score[:, c0:c0 + wd], in0=rl[:, 0:wd], scalar=wsc[:, h:h + 1], in1=score[:, c0:c0 + wd],
                            op0=ALU.mult, op1=ALU.add), r=(rlk, sk, "score"), w=("score",))
            self.op("dve", lambda e: e.tensor_reduce(out=hi, in_=score[:, 0:nk], axis=AX.X, op=ALU.max), r=("score",), w=(sk,))
            self.op("dve", lambda e: e.tensor_reduce(out=lo, in_=score[:, 0:nk], axis=AX.X, op=ALU.min), r=("score",), w=(sk,))
            self.op("dve", lambda e: e.tensor_tensor(out=w0, in0=hi, in1=lo, op=ALU.subtract), r=(sk,), w=(sk,))
            self.op("dve", lambda e: e.tensor_tensor(out=score[:, nk - 128:nk], in0=score[:, nk - 128:nk], in1=causts[:],
                                                     op=ALU.add), r=("score", "causts"), w=("score",))
            for it in range(NIT):
                cst = 2.0 ** -(it + 1)
                self.op("dve", lambda e, cst=cst: e.scalar_tensor_tensor(out=mid, in0=w0, scalar=cst, in1=lo, op0=ALU.mult,
                                                                         op1=ALU.add), r=(sk,), w=(sk,))
                self.op("dve", lambda e: e.tensor_scalar(out=Mt[:, 0:nk], in0=score[:, 0:nk], scalar1=mid, scalar2=0.0,
                                                         op0=ALU.is_ge, op1=ALU.add, accum_out=cnt),
                        r=("score", sk), w=("Mt", sk))
                self.op("dve", lambda e, cst=cst: e.tensor_scalar(out=tt, in0=cnt, scalar1=float(topk) - 0.5, scalar2=cst,
                                                                  op0=ALU.is_ge, op1=ALU.mult), r=(sk,), w=(sk,))
                self.op("dve", lambda e: e.scalar_tensor_tensor(out=lo, in0=tt, scalar=w0, in1=lo, op0=ALU.mult, op1=ALU.add),
                        r=(sk,), w=(sk,))
            self.op("dve", lambda e: e.tensor_scalar(out=Mt[:, 0:nk], in0=score[:, 0:nk], scalar1=lo, scalar2=None,
                                                     op0=ALU.is_ge), r=("score", sk), w=("Mt",))

        def get_kT(j):
            return [(KT[:, j, :], ("KT", j), 0, 16)]

        def get_v(j):
            return VA[:, j:j + 1, :], ("VA", j), (lambda h: 0)

        def bias_for(j, c):
            if j == i:
                return biasD[:, 4 * c:4 * c + 4, :], biasDk
            if j == i - 1:
                return biasOA[:, 4 * c:4 * c + 4, :], biasOAk
            return None

        def mask_for(j):
            MT, MTk = MTs.next()
            self.transpose_to(Mt, "Mt", [j * 128], 128, MT, MTk, evac_eng="dve")
            return MT[:, 0, :], MTk

        _attend(self, QT, QTk, 64, 0.125, list(range(i + 1)), get_kT, get_v, bias_for, mask_for if select else None, PTs, tmps)
        ao, aok = aos.get(ti)
        aT, aTk = aTs.get(ti)
        xo, xok = xos.get(ti)
        _attn_finish(self, st, sk, 64, ao, aok)
        _out_proj(self, ao, aok, aT, aTk, wout, woutk, xt, xk, xo, xok, x_dst, dst_id, ti)

    load(0)
    for ti in range(ntile):
        if ti + 1 < ntile:
            load(ti + 1)
        tile_body(ti)
    S.barrier()
    S.recycle()
    self.sb_off = mark


Builder.phase_a = _phase_a


FULL_PHASES = [("a", 0), ("mlp", 0), ("b", 1), ("mlp", 1), ("c", 2), ("mlp", 2), ("a", 3), ("mlp", 3)]
LAUNCH_PLAN = [FULL_PHASES]


def _run_group(phases, xs, pos, inputs, SEQ, NSEQ, topk):
    B = build_program(phases, SEQ, NSEQ, topk)
    consts = const_inputs(B)
    in_maps = []
    for c in range(len(xs)):
        m = {}
        for name in B.dram_in:
            if name == "x":
                m[name] = xs[c]
            elif name == "positions":
                m[name] = pos[c]
            elif name in consts:
                m[name] = consts[name]
            else:
                m[name] = inputs[name]
        in_maps.append(m)
    res = run_bass_kernel_spmd(B.nc, in_maps, core_ids=list(range(len(xs))))
    return [np.asarray(r["out"]) for r in res.results]


def kernel(**inputs):
    inputs = {k: np.ascontiguousarray(np.asarray(v)) for k, v in inputs.items()}
    x = inputs["x"]
    Bsz, SEQ, _ = x.shape
    NSEQ = Bsz // NCORES
    topk = min(256, SEQ // 4)
    xs = [np.ascontiguousarray(x[c * NSEQ:(c + 1) * NSEQ].reshape(NSEQ * SEQ, D)) for c in range(NCORES)]
    pos = [np.ascontiguousarray(inputs["positions"][c * NSEQ:(c + 1) * NSEQ].reshape(NSEQ * SEQ, 1).astype(np.int32))
           for c in range(NCORES)]
    for group in LAUNCH_PLAN:
        xs = _run_group(group, xs, pos, inputs, SEQ, NSEQ, topk)
    out = np.stack([o.reshape(NSEQ, SEQ, D) for o in xs], axis=0).reshape(Bsz, SEQ, D)
    return out.astype(np.float32, copy=False)
```

```python
import math
from contextlib import ExitStack

import numpy as np
import concourse.bass as bass
import concourse.mybir as mybir
from concourse.bass_utils import run_bass_kernel_spmd

F32 = mybir.dt.float32
BF16 = mybir.dt.bfloat16
I32 = mybir.dt.int32
AF = mybir.ActivationFunctionType
ALU = mybir.AluOpType
AX = mybir.AxisListType

D = 1024
DFF = 4096
EPS = 1e-6
NEG = -30000.0
NCORES = 8


class Op:
    __slots__ = ("idx", "eng", "eidx", "fn", "cdeps", "dwaits", "is_dma", "dkey", "dh", "sig", "sigidx")


class Sched:
    ENGS = ("pe", "act", "dve", "pool", "sp")

    def __init__(self, nc):
        self.nc = nc
        self.ops = []
        self.eops = {e: [] for e in self.ENGS}
        self.kw = {}
        self.kr = {}
        self.dsem = {}
        self.excl = set()
        self.pending = {}
        self.free_dsems = {"sp": [], "pool": [], "act": []}

    def add(self, eng, fn, r=(), w=(), dma=None):
        op = Op()
        op.idx = len(self.ops)
        op.eng = eng
        op.eidx = len(self.eops[eng])
        op.fn = fn
        op.is_dma = dma is not None
        op.dkey = dma
        op.cdeps = {}
        op.dwaits = {}
        op.sig = False
        op.sigidx = 0

        def dep(p, kind):
            if p is None or p is op:
                return
            if p.is_dma:
                if op.is_dma and p.dkey == dma and kind == "waw":
                    return
                ent = self.dsem.get(p.dkey)
                if ent is None or ent[0] is not p.dh:
                    return
                op.dwaits[id(ent[0])] = (ent[0], ent[1])
                return
            if p.eng == eng and not op.is_dma:
                if eng == "pe":
                    return
            cur = op.cdeps.get(p.eng)
            if cur is None or p.eidx > cur.eidx:
                op.cdeps[p.eng] = p

        for k in r:
            dep(self.kw.get(k), "raw")
            rd = self.kr.setdefault(k, {})
            if k in self.excl:
                for q in rd.values():
                    if q.eng != eng:
                        dep(q, "war")
            rd[("d", dma) if op.is_dma else ("c", eng)] = op
        for k in w:
            dep(self.kw.get(k), "waw")
            for q in self.kr.get(k, {}).values():
                dep(q, "war")
            self.kw[k] = op
            self.kr[k] = {}
        pb = self.pending.pop(eng, None)
        if pb is not None:
            for p in pb[0]:
                if p.eng != eng:
                    dep(p, "raw")
            for h, c in pb[1]:
                old = op.dwaits.get(id(h))
                if old is None or old[1] < c:
                    op.dwaits[id(h)] = (h, c)
        if op.is_dma:
            if dma not in self.dsem:
                if self.free_dsems[eng]:
                    self.dsem[dma] = self.free_dsems[eng].pop()
                else:
                    self.nsem = getattr(self, "nsem", 0) + 1
                    self.dsem[dma] = [self.nc.alloc_semaphore("d_%d" % self.nsem), 0, eng]
            self.dsem[dma][1] += 16
            op.dh = self.dsem[dma][0]
        self.ops.append(op)
        self.eops[eng].append(op)
        return op

    def barrier(self):
        last = [l[-1] for l in self.eops.values() if l and not l[-1].is_dma]
        for l in self.eops.values():
            for o in reversed(l):
                if not o.is_dma:
                    if o not in last:
                        last.append(o)
                    break
        dw = [(v[0], v[1]) for v in self.dsem.values()]
        for e in self.ENGS:
            old = self.pending.get(e)
            if old is None:
                self.pending[e] = (list(last), list(dw))
            else:
                old[0].extend(last)
                old[1].extend(dw)

    def recycle(self, keep=()):
        for k in list(self.dsem.keys()):
            if k in keep:
                continue
            ent = self.dsem.pop(k)
            self.free_dsems[ent[2]].append(ent)

    def emit(self):
        nc = self.nc
        for op in self.ops:
            for p in op.cdeps.values():
                p.sig = True
        esem = {}
        for e in self.ENGS:
            n = 0
            for op in self.eops[e]:
                if op.sig:
                    n += 1
                    op.sigidx = n
            if n:
                esem[e] = nc.alloc_semaphore("e_" + e)
        sched = self

        def run(e, eng):
            waited = {}
            for op in sched.eops[e]:
                for pe_, p in op.cdeps.items():
                    if waited.get(pe_, 0) < p.sigidx:
                        eng.wait_ge(esem[pe_], p.sigidx)
                        waited[pe_] = p.sigidx
                for hid, (h, c) in op.dwaits.items():
                    if waited.get(hid, 0) < c:
                        eng.wait_ge(h, c)
                        waited[hid] = c
                ins = op.fn(eng)
                if op.is_dma:
                    ins.then_inc(op.dh, 16)
                elif op.sig:
                    ins.then_inc(esem[e], 1)

        with nc.Block() as block:
            @block.tensor
            def _(eng):
                run("pe", eng)

            @block.scalar
            def _(eng):
                run("act", eng)

            @block.vector
            def _(eng):
                run("dve", eng)

            @block.gpsimd
            def _(eng):
                run("pool", eng)

            @block.sync
            def _(eng):
                run("sp", eng)


def _t5_bucket(n):
    n = np.maximum(n, 0)
    nf = np.maximum(n, 1).astype(np.float32)
    large = 16 + (np.log(nf / np.float32(16)) / np.float32(math.log(128 / 16)) * np.float32(16)).astype(np.int32)
    large = np.minimum(large, 31)
    return np.where(n < 16, n, large)


TW = 384


def _host_consts():
    c = {}
    c["ident"] = np.eye(128, dtype=np.float32)
    rel = np.arange(TW) - 127
    b = _t5_bucket(rel)
    oh = np.zeros((2, 33, TW), np.float32)
    for v in range(2):
        for r in range(TW):
            if rel[r] >= 0:
                oh[v, b[r], r] += 1.0
                oh[v, 31, r] -= 1.0
            masked = rel[r] < 0 or (v == 1 and rel[r] >= 128)
            oh[v, 32, r] = NEG if masked else 0.0
    c["t5oh"] = oh
    t = np.arange(128)
    c["caus_ts"] = np.where(t[None, :] <= t[:, None], 0.0, -1e30).astype(np.float32)
    c["caus_st"] = np.where(t[:, None] <= t[None, :], 0.0, NEG).astype(np.float32)
    inv_freq = (10000.0 ** (-np.arange(0, 32, 2, dtype=np.float32) / np.float32(32))).astype(np.float32)
    c["invf"] = np.broadcast_to(inv_freq[None, :], (128, 16)).copy()
    return c


class Builder:
    def __init__(self, phases, SEQ, NSEQ, topk):
        self.phases = phases
        self.SEQ = SEQ
        self.NSEQ = NSEQ
        self.NT = SEQ // 128
        self.TOK = SEQ * NSEQ
        self.topk = topk
        self.nc = bass.Bass("TRN2", target_bir_lowering=False)
        self.S = Sched(self.nc)
        self.uid = 0
        self.dram_in = {}
        self.sb_off = 16512
        self.sb_peak = 0
        self.sb_cap = 229312

    def din(self, name, shape, dt=F32):
        if name not in self.dram_in:
            self.dram_in[name] = self.nc.dram_tensor(name, list(shape), dt, kind="ExternalInput").ap()
        return self.dram_in[name]

    def sb(self, es, name, shape, dt):
        self.uid += 1
        esz = 2 if dt == BF16 else 4
        n = 1
        for d in shape[1:]:
            n *= d
        nbytes = (n * esz + 63) // 64 * 64
        off = self.sb_off
        self.sb_off += nbytes
        assert self.sb_off <= self.sb_cap, "SBUF overflow: %d > %d (%s)" % (self.sb_off, self.sb_cap, name)
        self.sb_peak = max(self.sb_peak, self.sb_off)
        return self.nc.alloc_sbuf_tensor_at("%s_%d" % (name, self.uid), list(shape), dt, offset=off)

    def op(self, eng, fn, r=(), w=(), dma=None):
        return self.S.add(eng, fn, r, w, dma)

    def dma_in(self, dst_ap, src_ap, tkey, r=(), w=(), eng="sp"):
        return self.op(eng, lambda e: e.dma_start(out=dst_ap, in_=src_ap), r=r, w=tuple(w) + (tkey,), dma=tkey)

    def dma_out(self, dst_ap, src_ap, tkey, r=(), w=(), eng="sp"):
        return self.op(eng, lambda e: e.dma_start(out=dst_ap, in_=src_ap), r=tuple(r) + (tkey,), w=w, dma=tkey)

    def setup_psum(self, es):
        self.banks = []
        for i in range(8):
            t = self.nc.alloc_psum_tensor("bank%d" % i, [128, 512], F32)
            self.banks.append(t)
            self.S.excl.add("bank%d" % i)
        self.grot = 0
        self.ng = 3

    def gbank(self):
        i = self.grot % self.ng
        self.grot += 1
        return i

    def setup_consts(self, es):
        hc = _host_consts()
        self.host_consts = hc
        nc = self.nc
        ident_f = self.sb(es, "identf", [128, 128], F32)
        self.ident = self.sb(es, "ident", [128, 128], BF16)
        d = self.din("k_ident", [128, 128])
        self.dma_in(ident_f[:], d[:, :], "identf")
        self.op("dve", lambda e: e.tensor_copy(out=self.ident[:], in_=ident_f[:]), r=("identf",), w=("ident",))
        self.eps_t = self.sb(es, "eps", [128, 1], F32)
        self.op("dve", lambda e: e.memset(self.eps_t[:], EPS), w=("eps",))

    def transpose_to(self, src_tile, src_key, col_offs, width, dst3, dst_key, dst_blk0=0, evac_eng="act"):
        n = len(col_offs)
        i0 = 0
        while i0 < n:
            cnt = min(8, n - i0)
            b = self.gbank()
            bk = "bank%d" % b
            pb = self.banks[b].bitcast(BF16)
            for j in range(cnt):
                off = col_offs[i0 + j]
                self.op("pe", lambda e, j=j, off=off, pb=pb: e.transpose(
                    out=pb[0:width, j * 128:(j + 1) * 128], in_=src_tile[:, off:off + width], identity=self.ident[:]),
                    r=(src_key, "ident"), w=(bk,))
            dst = dst3[0:width, dst_blk0 + i0:dst_blk0 + i0 + cnt, :]
            src = pb[0:width, 0:cnt * 128].rearrange("p (a b) -> p a b", b=128)
            if evac_eng == "act":
                self.op("act", lambda e, dst=dst, src=src: e.copy(out=dst, in_=src), r=(bk,), w=(dst_key,))
            else:
                self.op("dve", lambda e, dst=dst, src=src: e.tensor_copy(out=dst, in_=src), r=(bk,), w=(dst_key,))
            i0 += cnt

    def transpose_full(self, src_tile, src_key, nblk, dstT, dst_key, evac_eng="act"):
        i0 = 0
        while i0 < nblk:
            cnt = min(8, nblk - i0)
            b = self.gbank()
            bk = "bank%d" % b
            pb = self.banks[b].bitcast(BF16)
            for j in range(cnt):
                off = (i0 + j) * 128
                self.op("pe", lambda e, j=j, off=off, pb=pb: e.transpose(
                    out=pb[:, j * 128:(j + 1) * 128], in_=src_tile[:, off:off + 128], identity=self.ident[:]),
                    r=(src_key, "ident"), w=(bk,))
            dst = dstT[:, i0:i0 + cnt, :]
            src = pb[:, 0:cnt * 128].rearrange("p (a b) -> p a b", b=128)
            if evac_eng == "act":
                self.op("act", lambda e, dst=dst, src=src: e.copy(out=dst, in_=src), r=(bk,), w=(dst_key,))
            else:
                self.op("dve", lambda e, dst=dst, src=src: e.tensor_copy(out=dst, in_=src), r=(bk,), w=(dst_key,))
            i0 += cnt

    def linear(self, xT, xT_key, nk, W, W_key, N, evac):
        n0 = 0
        while n0 < N:
            wd = min(512, N - n0)
            b = self.gbank()
            bk = "bank%d" % b
            ps = self.banks[b]
            for k in range(nk):
                self.op("pe", lambda e, k=k, n0=n0, wd=wd, ps=ps: e.matmul(
                    ps[:, 0:wd], lhsT=xT[:, k, :], rhs=W[:, k, n0:n0 + wd], start=(k == 0), stop=(k == nk - 1)),
                    r=(xT_key, W_key), w=(bk,))
            evac(b, bk, n0, wd)
            n0 += wd

    def load_weight(self, es, name, dram_ap2d, K, N):
        nk = max(1, K // 128)
        kp = min(K, 128)
        t = self.sb(es, name, [kp, nk, N], BF16)
        key = name + "_%d" % self.uid
        src = dram_ap2d.rearrange("(k p) n -> p k n", p=kp)
        step = max(1, min(nk, 8192 // N if N <= 8192 else 1))
        k0 = 0
        while k0 < nk:
            k1 = min(nk, k0 + step)
            self.dma_in(t[:, k0:k1, :], src[:, k0:k1, :], key, eng="pool")
            k0 = k1
        return t, key

    def load_bcast_row(self, es, name, dram_row_ap, n):
        t = self.sb(es, name, [128, n], F32)
        key = name + "_%d" % self.uid
        src = bass.AP(tensor=dram_row_ap.tensor, offset=dram_row_ap.offset, ap=[[0, 128], [1, n]])
        self.dma_in(t[:], src, key)
        return t, key

    def rms_scale(self, es_tiles, ssq_ap, ssq_key, out_ap, out_key, n, shape_cols):
        self.op("act", lambda e: e.activation(out=out_ap, in_=ssq_ap, func=AF.Ln, bias=self.eps_t[:, 0:1], scale=1.0 / n),
                r=(ssq_key, "eps"), w=(out_key,))
        self.op("act", lambda e: e.activation(out=out_ap, in_=out_ap, func=AF.Exp, scale=-0.5),
                r=(out_key,), w=(out_key,))

    def rot(self, es, name, shape, dt, n=2):
        tiles = []
        for i in range(n):
            t = self.sb(es, name + str(i), shape, dt)
            tiles.append((t, "%s%d_%d" % (name, i, self.uid)))
        return Rot(tiles)

    def tile_norm(self, xt, xk, st, sk, col, g, gk, h, hk, junk):
        ssq = st[:, col:col + 1]
        rstd = st[:, col + 1:col + 2]
        self.op("act", lambda e: e.activation(out=junk[:], in_=xt[:], func=AF.Square, accum_out=ssq), r=(xk, sk), w=(sk,))
        self.rms_scale(None, ssq, sk, rstd, sk, D, 1)
        self.op("dve", lambda e: e.scalar_tensor_tensor(out=h[:], in0=xt[:], scalar=rstd, in1=g[:], op0=ALU.mult,
                                                        op1=ALU.mult), r=(xk, sk, gk), w=(hk,))

    def phase_mlp(self, li, x_src, src_id, x_dst, dst_id):
        S = self.S
        ntile = self.TOK // 128
        self.ng = 8
        w_up = self.din("w_up", [4, D, DFF])
        w_down = self.din("w_down", [4, DFF, D])
        norm_mlp = self.din("norm_mlp", [4, D])
        mark = self.sb_off
        with ExitStack() as es:
            g, gk = self.load_bcast_row(es, "gml", norm_mlp[li, :], D)
            wup, wupk = self.load_weight(es, "wup", w_up[li], D, DFF)
            wdn, wdnk = self.load_weight(es, "wdn", w_down[li], DFF, D)
            xts = self.rot(es, "xt", [128, D], F32, 3)
            hs = self.rot(es, "h", [128, D], BF16, 2)
            hTs = self.rot(es, "hT", [128, 8, 128], BF16, 2)
            sts = self.rot(es, "st", [128, 4], F32, 2)
            rls = self.rot(es, "rl", [128, 512], F32, 3)
            hids = self.rot(es, "hid", [128, DFF], BF16, 2)
            hidTs = self.rot(es, "hidT", [128, 32, 128], BF16, 2)
            xos = self.rot(es, "xo", [128, D], F32, 2)
            junk = self.sb(es, "junk", [128, D], BF16)

            def load(ti):
                xt, xk = xts.get(ti)
                self.dma_in(xt[:], x_src[ti * 128:(ti + 1) * 128, :], xk, r=(("xd", src_id, ti),))

            def front(ti):
                xt, xk = xts.get(ti)
                h, hk = hs.get(ti)
                hT, hTk = hTs.get(ti)
                st, sk = sts.get(ti)
                self.tile_norm(xt, xk, st, sk, 0, g, gk, h, hk, junk)
                self.transpose_full(h, hk, 8, hT, hTk)

            def body(ti):
                xt, xk = xts.get(ti)
                hT, hTk = hTs.get(ti)
                hid, hidk = hids.get(ti)
                hidT, hidTk = hidTs.get(ti)
                xo, xok = xos.get(ti)

                def evac_up(b, bk, n0, wd):
                    rl, rlk = rls.next()
                    ps = self.banks[b]
                    self.op("act", lambda e: e.activation(out=rl[:, 0:wd], in_=ps[:, 0:wd], func=AF.Relu), r=(bk,), w=(rlk,))
                    self.op("dve", lambda e: e.tensor_tensor(out=hid[:, n0:n0 + wd], in0=rl[:, 0:wd], in1=rl[:, 0:wd],
                                                             op=ALU.mult), r=(rlk,), w=(hidk,))

                self.linear(hT, hTk, 8, wup, wupk, DFF, evac_up)
                self.transpose_full(hid, hidk, 32, hidT, hidTk)

                def evac_dn(b, bk, n0, wd):
                    ps = self.banks[b]
                    self.op("dve", lambda e: e.tensor_tensor(out=xo[:, n0:n0 + wd], in0=ps[:, 0:wd], in1=xt[:, n0:n0 + wd],
                                                             op=ALU.add), r=(bk, xk), w=(xok,))

                self.linear(hidT, hidTk, 32, wdn, wdnk, D, evac_dn)
                self.dma_out(x_dst[ti * 128:(ti + 1) * 128, :], xo[:], xok, w=(("xd", dst_id, ti),))

            load(0)
            if ntile > 1:
                load(1)
            front(0)
            for ti in range(ntile):
                if ti + 2 < ntile:
                    load(ti + 2)
                if ti + 1 < ntile:
                    front(ti + 1)
                body(ti)
        S.barrier()
        S.recycle()
        self.sb_off = mark


class Rot:
    def __init__(self, tiles):
        self.tiles = tiles
        self.i = 0

    def get(self, i):
        return self.tiles[i % len(self.tiles)]

    def next(self):
        t = self.tiles[self.i % len(self.tiles)]
        self.i += 1
        return t


def build_program(phases, SEQ, NSEQ, topk):
    B = Builder(phases, SEQ, NSEQ, topk)
    nc = B.nc
    TOK = B.TOK
    B.setup_psum(None)
    B.setup_consts(None)
    x_in = B.din("x", [TOK, D])
    out = nc.dram_tensor("out", [TOK, D], F32, kind="ExternalOutput").ap()
    scr = [nc.dram_tensor("xs%d" % i, [TOK, D], F32, kind="Internal").ap() for i in range(2)] if len(phases) > 1 else []
    cur, cur_id = x_in, "in"
    for pi, (kind, li) in enumerate(phases):
        last = pi == len(phases) - 1
        dst, dst_id = (out, "out") if last else (scr[pi % 2], "s%d_%d" % (pi % 2, pi))
        if kind == "mlp":
            B.phase_mlp(li, cur, cur_id, dst, dst_id)
        elif kind == "a":
            B.phase_a(li, li // 3, cur, cur_id, dst, dst_id)
        elif kind == "b":
            B.phase_b(li, cur, cur_id, dst, dst_id)
        elif kind == "c":
            B.phase_c(li, cur, cur_id, dst, dst_id)
        cur, cur_id = dst, dst_id
    B.S.barrier()
    B.op("sp", lambda e: e.nop())
    B.S.emit()
    return B


def const_inputs(B):
    hc = B.host_consts
    m = {}
    for name in B.dram_in:
        if name.startswith("k_"):
            m[name] = np.ascontiguousarray(hc[name[2:]])
    return m


def _setup_bias(self):
    if getattr(self, "bias_ready", False):
        return
    self.bias_ready = True
    nc = self.nc
    rel_bias = self.din("rel_bias", [32, 16])
    oh_d = self.din("k_t5oh", [2, 33, TW])
    rb = self.sb(None, "rbaug", [33, 16], F32)
    self.op("dve", lambda e: e.memset(rb[32:33, :], 1.0), w=("rbaug",))
    self.dma_in(rb[0:32, :], rel_bias[:, :], "rbaug")
    oh = self.sb(None, "t5oh", [33, 2, TW], F32)
    self.dma_in(oh[:], oh_d.rearrange("v b r -> b v r"), "t5oh")
    fsb = self.sb(None, "fsb", [16, 2, TW], F32)
    fd = nc.dram_tensor("fd_scr", [2, 16, TW], F32, kind="Internal").ap()
    for v in range(2):
        b = self.gbank()
        bk = "bank%d" % b
        ps = self.banks[b]
        self.op("pe", lambda e, v=v, ps=ps: e.matmul(ps[0:16, 0:TW], lhsT=rb[:, :], rhs=oh[:, v, :], start=True, stop=True),
                r=("rbaug", "t5oh"), w=(bk,))
        self.op("dve", lambda e, v=v, ps=ps: e.tensor_copy(out=fsb[:, v, :], in_=ps[0:16, 0:TW]), r=(bk,), w=("fsb",))
    self.dma_out(fd.rearrange("v h r -> h v r"), fsb[:], "fsb", w=("fd",))
    self.fd = fd


def _load_bias_tile(self, name, v, delta):
    fd = self.fd
    tile = self.sb(None, name, [128, 16, 128], F32)
    key = "%s_%d" % (name, self.uid)
    for s_ in range(128):
        src = bass.AP(tensor=fd.tensor, offset=v * 16 * TW + 127 - s_ + 128 * delta, ap=[[0, 1], [TW, 16], [1, 128]])
        self.dma_in(tile[s_:s_ + 1, :, :], src, key, r=("fd",))
    return tile, key


def _proj_front(self, ti, xts, hs, hTs, sts, prs, g, gk, win, wink, nin, x_src, src_id, junk):
    xt, xk = xts.get(ti)
    h, hk = hs.get(ti)
    hT, hTk = hTs.get(ti)
    st, sk = sts.get(ti)
    pr, prk = prs.get(ti)
    self.tile_norm(xt, xk, st, sk, 0, g, gk, h, hk, junk)
    self.transpose_full(h, hk, 8, hT, hTk)

    def evac(b, bk, n0, wd):
        ps = self.banks[b]
        self.op("act", lambda e: e.copy(out=pr[:, n0:n0 + wd], in_=ps[:, 0:wd]), r=(bk,), w=(prk,))

    self.linear(hT, hTk, 8, win, wink, nin, evac)
    return xt, xk, pr, prk, st, sk


def _head_norm(self, src3, srck, H, dh, gbc, gbck, st, sk, col, tmp, tmpk, out3, outk):
    t3 = tmp[:, 0:H * dh].rearrange("p (h d) -> p h d", d=dh)
    ssq = st[:, col:col + H]
    rstd = st[:, col + H:col + 2 * H]
    self.op("dve", lambda e: e.tensor_tensor(out=t3, in0=src3, in1=src3, op=ALU.mult), r=(srck,), w=(tmpk,))
    self.op("dve", lambda e: e.tensor_reduce(out=ssq, in_=t3, axis=AX.X, op=ALU.add), r=(tmpk,), w=(sk,))
    self.rms_scale(None, ssq, sk, rstd, sk, dh, H)
    self.op("dve", lambda e: e.tensor_tensor(out=t3, in0=src3, in1=rstd.unsqueeze(2).to_broadcast([128, H, dh]),
                                             op=ALU.mult), r=(srck, sk), w=(tmpk,))
    self.op("dve", lambda e: e.tensor_tensor(out=out3, in0=t3, in1=gbc[:, 0:dh].unsqueeze(1).to_broadcast([128, H, dh]),
                                             op=ALU.mult), r=(tmpk, gbck), w=(outk,))


PVB = 5


def _pv_slot(h):
    return PVB + h // 7, (h % 7) * 65


def _attend(self, QT, QTk, dk, scale, ktiles, get_kT, get_v, bias_for, mask_for, PTs, tmps):
    started = set()
    nkt = len(ktiles)
    state = {}

    def stage1(jj):
        j = ktiles[jj]
        groups = get_kT(j)
        PT, PTk = PTs.next()
        state[jj] = (PT, PTk)
        m = mask_for(j) if mask_for is not None else None
        for c in range(4):
            b = 3 + (c % 2)
            bk = "bank%d" % b
            ps = self.banks[b]
            for (kT, kk, h0, nh) in groups:
                lo, hi = max(h0, 4 * c), min(h0 + nh, 4 * c + 4)
                if lo >= hi:
                    continue
                self.op("pe", lambda e, kT=kT, lo=lo, hi=hi, ps=ps, c=c: e.matmul(
                    ps[:, (lo - 4 * c) * 128:(hi - 4 * c) * 128], lhsT=kT, rhs=QT[0:dk, lo * 128:hi * 128],
                    start=True, stop=(m is None), skip_group_check=True), r=(kk, QTk), w=(bk,))
            if m is not None:
                map_, mkey = m
                self.op("pe", lambda e, ps=ps, map_=map_: e.matmul(
                    ps[:, 0:512], lhsT=self.ident[:, :], rhs=map_, start=False, stop=True, skip_group_check=True),
                    r=(mkey, "ident"), w=(bk,))
            bias = bias_for(j, c)
            pt_c = PT[:, c * 512:(c + 1) * 512]
            if bias is None:
                self.op("act", lambda e, ps=ps, pt_c=pt_c: e.activation(out=pt_c, in_=ps[:, :], func=AF.Exp, scale=scale),
                        r=(bk,), w=(PTk,))
            else:
                bap, bkey = bias
                tmp, tmpk = tmps.next()
                self.op("dve", lambda e, ps=ps, tmp=tmp, bap=bap: e.scalar_tensor_tensor(
                    out=tmp[:, 0:512].rearrange("p (a b) -> p a b", b=128), in0=ps[:, :].rearrange("p (a b) -> p a b", b=128),
                    scalar=scale, in1=bap, op0=ALU.mult, op1=ALU.add), r=(bk, bkey), w=(tmpk,))
                self.op("act", lambda e, tmp=tmp, pt_c=pt_c: e.activation(out=pt_c, in_=tmp[:, 0:512], func=AF.Exp),
                        r=(tmpk,), w=(PTk,))

    def stage2(jj):
        j = ktiles[jj]
        PT, PTk = state.pop(jj)
        v3, vk, vh = get_v(j)
        for h in range(16):
            b, off = _pv_slot(h)
            bk = "bank%d" % b
            st_flag = b not in started
            started.add(b)
            self.op("pe", lambda e, h=h, b=b, off=off, PT=PT, v3=v3, st_flag=st_flag, last=(jj == nkt - 1): e.matmul(
                self.banks[b][:, off:off + 65], lhsT=PT[:, h * 128:(h + 1) * 128], rhs=v3[:, vh(h), :],
                start=st_flag, stop=last, skip_group_check=True), r=(PTk, vk), w=(bk,))

    stage1(0)
    for jj in range(nkt):
        if jj + 1 < nkt:
            stage1(jj + 1)
        stage2(jj)


def _attn_finish(self, st, sk, col, ao, aok, sinkexp=None):
    den = st[:, col:col + 16]
    rden = st[:, col + 16:col + 32]
    for b in range(PVB, PVB + 3):
        h0 = (b - PVB) * 7
        nh = min(7, 16 - h0)
        bk = "bank%d" % b
        o3 = self.banks[b][:, 0:nh * 65].rearrange("p (h d) -> p h d", d=65)
        self.op("dve", lambda e, o3=o3, h0=h0, nh=nh: e.tensor_copy(out=den[:, h0:h0 + nh].unsqueeze(2), in_=o3[:, :, 64:65]),
                r=(bk,), w=(sk,))
    if sinkexp is not None:
        se, sek = sinkexp
        self.op("dve", lambda e: e.tensor_tensor(out=den, in0=den, in1=se[:, 0:16], op=ALU.add), r=(sk, sek), w=(sk,))
    self.op("dve", lambda e: e.reciprocal(out=rden, in_=den), r=(sk,), w=(sk,))
    ao3 = ao[:, :].rearrange("p (h d) -> p h d", d=64)
    for b in range(PVB, PVB + 3):
        h0 = (b - PVB) * 7
        nh = min(7, 16 - h0)
        bk = "bank%d" % b
        o3 = self.banks[b][:, 0:nh * 65].rearrange("p (h d) -> p h d", d=65)
        self.op("dve", lambda e, o3=o3, h0=h0, nh=nh: e.tensor_tensor(
            out=ao3[:, h0:h0 + nh, :], in0=o3[:, :, 0:64], in1=rden[:, h0:h0 + nh].unsqueeze(2).to_broadcast([128, nh, 64]),
            op=ALU.mult), r=(bk, sk), w=(aok,))


def _out_proj(self, ao, aok, aT, aTk, wout, woutk, xt, xk, xo, xok, x_dst, dst_id, ti):
    self.transpose_full(ao, aok, 8, aT, aTk)

    def evac(b, bk, n0, wd):
        ps = self.banks[b]
        self.op("dve", lambda e: e.tensor_tensor(out=xo[:, n0:n0 + wd], in0=ps[:, 0:wd], in1=xt[:, n0:n0 + wd], op=ALU.add),
                r=(bk, xk), w=(xok,))

    self.linear(aT, aTk, 8, wout, woutk, D, evac)
    self.dma_out(x_dst[ti * 128:(ti + 1) * 128, :], xo[:], xok, w=(("xd", dst_id, ti),))


def _phase_b(self, li, x_src, src_id, x_dst, dst_id):
    S = self.S
    self.ng = 3
    _setup_bias(self)
    mark = self.sb_off
    NT = self.NT
    ib = 0
    biasD, biasDk = _load_bias_tile(self, "biasD", 0, 0)
    biasOB, biasOBk = _load_bias_tile(self, "biasOB", 1, 1)
    b_w_in = self.din("b_w_in", [1, D, 1536])
    b_w_out = self.din("b_w_out", [1, D, D])
    norm_mix = self.din("norm_mix", [4, D])
    g, gk = self.load_bcast_row(None, "gmix", norm_mix[li, :], D)
    gq, gqk = self.load_bcast_row(None, "gq", self.din("b_q_gain", [1, 64])[ib, :], 64)
    gkk_t, gkk = self.load_bcast_row(None, "gk", self.din("b_k_gain", [1, 64])[ib, :], 64)
    sk_t, skk = self.load_bcast_row(None, "sinks", self.din("b_sinks", [1, 16])[ib, :], 16)
    b31, b31k = self.load_bcast_row(None, "b31", self.din("rel_bias", [32, 16])[31, :], 16)
    self.op("dve", lambda e: e.tensor_tensor(out=sk_t[:], in0=sk_t[:], in1=b31[:], op=ALU.subtract), r=(skk, b31k), w=(skk,))
    self.op("act", lambda e: e.activation(out=sk_t[:], in_=sk_t[:], func=AF.Exp), r=(skk,), w=(skk,))
    win, wink = self.load_weight(None, "bwin", b_w_in[ib], D, 1536)
    wout, woutk = self.load_weight(None, "bwout", b_w_out[ib], D, D)
    xts = self.rot(None, "xt", [128, D], F32, 2)
    hs = self.rot(None, "h", [128, D], BF16, 2)
    hTs = self.rot(None, "hT", [128, 8, 128], BF16, 2)
    sts = self.rot(None, "st", [128, 128], F32, 2)
    prs = self.rot(None, "pr", [128, 1536], F32, 2)
    tmpn = self.rot(None, "tmpn", [128, D], F32, 2)
    qns = self.rot(None, "qn", [128, D], BF16, 2)
    kns = self.rot(None, "kn", [128, 256], BF16, 2)
    QTs = self.rot(None, "QT", [64, 16 * 128], BF16, 2)
    KTs = self.rot(None, "KT", [64, 4, 128], BF16, 2)
    VAs = self.rot(None, "VA", [128, 4, 65], BF16, 2)
    PTs = self.rot(None, "PT", [128, 16 * 128], BF16, 2)
    tmps = self.rot(None, "tmpb", [128, 512], F32, 2)
    aos = self.rot(None, "ao", [128, D], BF16, 2)
    aTs = self.rot(None, "aT", [128, 8, 128], BF16, 2)
    xos = self.rot(None, "xo", [128, D], F32, 2)
    junk = self.sb(None, "junk", [128, D], BF16)
    for (va, vak) in VAs.tiles:
        self.op("dve", lambda e, va=va: e.memset(va[:, :, 64:65], 1.0), w=(vak,))
    ntile = self.TOK // 128

    def load(ti):
        xt, xk = xts.get(ti)
        self.dma_in(xt[:], x_src[ti * 128:(ti + 1) * 128, :], xk, r=(("xd", src_id, ti),))

    load(0)
    for ti in range(ntile):
        if ti + 1 < ntile:
            load(ti + 1)
        i = ti % NT
        xt, xk, pr, prk, st, sk = _proj_front(self, ti, xts, hs, hTs, sts, prs, g, gk, win, wink, 1536, x_src, src_id, junk)
        tmp, tmpk = tmpn.get(ti)
        qn, qnk = qns.get(ti)
        kn, knk = kns.get(ti)
        QT, QTk = QTs.get(ti)
        KT, KTk = KTs.get(ti)
        VA, VAk = VAs.get(ti)
        _head_norm(self, pr[:, 0:1024].rearrange("p (h d) -> p h d", d=64), prk, 16, 64, gq, gqk, st, sk, 8, tmp, tmpk,
                   qn[:, :].rearrange("p (h d) -> p h d", d=64), qnk)
        _head_norm(self, pr[:, 1024:1280].rearrange("p (h d) -> p h d", d=64), prk, 4, 64, gkk_t, gkk, st, sk, 48, tmp, tmpk,
                   kn[:, :].rearrange("p (h d) -> p h d", d=64), knk)
        self.transpose_to(qn, qnk, [h * 64 for h in range(16)], 64, QT[:, :].rearrange("p (h t) -> p h t", t=128), QTk)
        self.transpose_to(kn, knk, [h * 64 for h in range(4)], 64, KT, KTk)
        self.op("act", lambda e, VA=VA, pr=pr: e.copy(out=VA[:, :, 0:64], in_=pr[:, 1280:1536].rearrange("p (h d) -> p h d", d=64)),
                r=(prk,), w=(VAk,))
        ktiles = ([i - 1] if i > 0 else []) + [i]

        def get_kT(j, ti=ti, i=i):
            KTj, KTjk = KTs.get(ti - (i - j))
            return [(KTj[:, kv, :], KTjk, kv * 4, 4) for kv in range(4)]

        def get_v(j, ti=ti, i=i):
            VAj, VAjk = VAs.get(ti - (i - j))
            return VAj, VAjk, (lambda h: h // 4)

        def bias_for(j, c, i=i):
            if j == i:
                return biasD[:, 4 * c:4 * c + 4, :], biasDk
            return biasOB[:, 4 * c:4 * c + 4, :], biasOBk

        _attend(self, QT, QTk, 64, 0.125, ktiles, get_kT, get_v, bias_for, None, PTs, tmps)
        ao, aok = aos.get(ti)
        aT, aTk = aTs.get(ti)
        xo, xok = xos.get(ti)
        _attn_finish(self, st, sk, 64, ao, aok, sinkexp=(sk_t, skk))
        _out_proj(self, ao, aok, aT, aTk, wout, woutk, xt, xk, xo, xok, x_dst, dst_id, ti)
    S.barrier()
    S.recycle()
    self.sb_off = mark


Builder.phase_b = _phase_b


def _rope(self, src3, srck, sc, sck, r1, r1k, r2, r2k, dst3, dstk, H):
    t1 = src3[:, :, 64:80]
    t2 = src3[:, :, 80:96]
    sin_b = sc[:, 0:16].unsqueeze(1).to_broadcast([128, H, 16])
    cos_b = sc[:, 16:32].unsqueeze(1).to_broadcast([128, H, 16])
    a = r1[:, 0:H * 16].rearrange("p (h d) -> p h d", d=16)
    b = r2[:, 0:H * 16].rearrange("p (h d) -> p h d", d=16)
    self.op("dve", lambda e: e.tensor_tensor(out=a, in0=t1, in1=cos_b, op=ALU.mult), r=(srck, sck), w=(r1k,))
    self.op("dve", lambda e: e.tensor_tensor(out=b, in0=t2, in1=sin_b, op=ALU.mult), r=(srck, sck), w=(r2k,))
    self.op("dve", lambda e: e.tensor_tensor(out=dst3[:, :, 64:80], in0=a, in1=b, op=ALU.subtract), r=(r1k, r2k), w=(dstk,))
    self.op("dve", lambda e: e.tensor_tensor(out=a, in0=t2, in1=cos_b, op=ALU.mult), r=(srck, sck), w=(r1k,))
    self.op("dve", lambda e: e.tensor_tensor(out=b, in0=t1, in1=sin_b, op=ALU.mult), r=(srck, sck), w=(r2k,))
    self.op("dve", lambda e: e.tensor_tensor(out=dst3[:, :, 80:96], in0=a, in1=b, op=ALU.add), r=(r1k, r2k), w=(dstk,))


def _phase_c(self, li, x_src, src_id, x_dst, dst_id):
    S = self.S
    nc = self.nc
    self.ng = 3
    mark = self.sb_off
    NT = self.NT
    ic = 0
    ntile = self.TOK // 128
    TWO_PI = 2.0 * math.pi
    c_w_in = self.din("c_w_in", [1, D, 416])
    c_w_q_b = self.din("c_w_q_b", [1, 256, 1536])
    c_w_kv_b = self.din("c_w_kv_b", [1, 128, 2048])
    c_w_out = self.din("c_w_out", [1, D, D])
    norm_mix = self.din("norm_mix", [4, D])
    pos_d = self.din("positions", [self.TOK, 1], I32)
    ktd = nc.dram_tensor("ktd_scr", [ntile, 96, 16 * 128], BF16, kind="Internal").ap()
    vad = nc.dram_tensor("vad_scr", [ntile, 128, 16 * 65], BF16, kind="Internal").ap()
    g, gk = self.load_bcast_row(None, "gmix", norm_mix[li, :], D)
    gqa, gqak = self.load_bcast_row(None, "gqa", self.din("c_q_a_gain", [1, 256])[ic, :], 256)
    gkva, gkvak = self.load_bcast_row(None, "gkva", self.din("c_kv_a_gain", [1, 128])[ic, :], 128)
    gq, gqk = self.load_bcast_row(None, "gq", self.din("c_q_gain", [1, 96])[ic, :], 96)
    gkg, gkgk = self.load_bcast_row(None, "gk", self.din("c_k_gain", [1, 96])[ic, :], 96)
    invf = self.sb(None, "invf", [128, 16], F32)
    self.dma_in(invf[:], self.din("k_invf", [128, 16])[:, :], "invf")
    caus = self.sb(None, "causst", [128, 128], F32)
    self.dma_in(caus[:], self.din("k_caus_st", [128, 128])[:, :], "causst")
    negpi = self.sb(None, "negpi", [128, 1], F32)
    win, wink = self.load_weight(None, "cwin", c_w_in[ic], D, 416)
    wqb, wqbk = self.load_weight(None, "cwqb", c_w_q_b[ic], 256, 1536)
    wkvb, wkvbk = self.load_weight(None, "cwkvb", c_w_kv_b[ic], 128, 2048)
    wout, woutk = self.load_weight(None, "cwout", c_w_out[ic], D, D)
    xts = self.rot(None, "xt", [128, D], F32, 2)
    hs = self.rot(None, "h", [128, D], BF16, 2)
    hTs = self.rot(None, "hT", [128, 8, 128], BF16, 2)
    sts = self.rot(None, "st", [128, 256], F32, 2)
    prs = self.rot(None, "pr", [128, 416], F32, 2)
    tmpn = self.rot(None, "tmpn", [128, 1536], F32, 1)
    lat = self.rot(None, "lat", [128, 384], BF16, 2)
    qlTs = self.rot(None, "qlT", [128, 3, 128], BF16, 2)
    q32s = self.rot(None, "q32", [128, 1536], F32, 1)
    k32s = self.rot(None, "k32", [128, 1536], F32, 1)
    qfs = self.rot(None, "qf", [128, 1536], BF16, 2)
    kfs = self.rot(None, "kf", [128, 1536], BF16, 2)
    r1s = self.rot(None, "r1", [128, 256], F32, 2)
    r2s = self.rot(None, "r2", [128, 256], F32, 2)
    posi_s = self.rot(None, "posi", [128, 1], I32, 2)
    angs = self.rot(None, "ang", [128, 64], F32, 2)
    angi = self.rot(None, "angi", [128, 32], I32, 2)
    QTs = self.rot(None, "QT", [96, 16 * 128], BF16, 2)
    KTt = self.rot(None, "KTt", [96, 16, 128], BF16, 2)
    VAt = self.rot(None, "VAt", [128, 16, 65], BF16, 2)
    KTb = self.rot(None, "KTb", [96, 16, 128], BF16, 3)
    VAb = self.rot(None, "VAb", [128, 16, 65], BF16, 3)
    PTs = self.rot(None, "PT", [128, 16 * 128], BF16, 2)
    tmps = self.rot(None, "tmpb", [128, 512], F32, 2)
    aos = self.rot(None, "ao", [128, D], BF16, 2)
    aTs = self.rot(None, "aT", [128, 8, 128], BF16, 2)
    xos = self.rot(None, "xo", [128, D], F32, 2)
    junk = self.sb(None, "junk", [128, D], BF16)
    self.op("dve", lambda e: e.memset(negpi[:], -math.pi), w=("negpi",))
    for (va, vak) in VAt.tiles:
        self.op("dve", lambda e, va=va: e.memset(va[:, :, 64:65], 1.0), w=(vak,))

    def load(ti):
        xt, xk = xts.get(ti)
        self.dma_in(xt[:], x_src[ti * 128:(ti + 1) * 128, :], xk, r=(("xd", src_id, ti),))
        pi_, pik = posi_s.get(ti)
        self.dma_in(pi_[:], pos_d[ti * 128:(ti + 1) * 128, :], pik)

    def tile_body(ti):
        i = ti % NT
        xt, xk, pr, prk, st, sk = _proj_front(self, ti, xts, hs, hTs, sts, prs, g, gk, win, wink, 416, x_src, src_id, junk)
        tmp, tmpk = tmpn.get(ti)
        la, lak = lat.get(ti)
        qlT, qlTk = qlTs.get(ti)
        q32, q32k = q32s.get(ti)
        k32, k32k = k32s.get(ti)
        qf, qfk = qfs.get(ti)
        kf, kfk = kfs.get(ti)
        r1, r1k = r1s.get(ti)
        r2, r2k = r2s.get(ti)
        VA, VAk = VAt.get(ti)
        KT, KTk = KTt.get(ti)
        QT, QTk = QTs.get(ti)
        pi_, pik = posi_s.get(ti)
        ang, angk = angs.get(ti)
        ai, aik = angi.get(ti)
        posf = ang[:, 32:33]
        a32 = ang[:, 0:32]
        kf32 = ang[:, 33:33 + 31]
        self.op("dve", lambda e: e.tensor_copy(out=posf, in_=pi_[:]), r=(pik,), w=(angk,))
        self.op("dve", lambda e: e.tensor_scalar(out=ang[:, 0:16], in0=invf[:], scalar1=posf, scalar2=None, op0=ALU.mult),
                r=(angk, "invf"), w=(angk,))
        self.op("dve", lambda e: e.tensor_scalar(out=ang[:, 16:32], in0=ang[:, 0:16], scalar1=math.pi / 2, scalar2=None,
                                                 op0=ALU.add), r=(angk,), w=(angk,))
        self.op("dve", lambda e: e.tensor_scalar(out=ang[:, 32:64], in0=a32, scalar1=1.0 / TWO_PI, scalar2=None,
                                                 op0=ALU.mult), r=(angk,), w=(angk,))
        self.op("dve", lambda e: e.tensor_copy(out=ai[:], in_=ang[:, 32:64]), r=(angk,), w=(aik,))
        self.op("dve", lambda e: e.tensor_copy(out=ang[:, 32:64], in_=ai[:]), r=(aik,), w=(angk,))
        self.op("dve", lambda e: e.scalar_tensor_tensor(out=a32, in0=ang[:, 32:64], scalar=-TWO_PI, in1=a32, op0=ALU.mult,
                                                        op1=ALU.add), r=(angk,), w=(angk,))
        self.op("dve", lambda e: e.tensor_scalar(out=ang[:, 32:64], in0=a32, scalar1=math.pi, scalar2=-TWO_PI,
                                                 op0=ALU.is_gt, op1=ALU.mult), r=(angk,), w=(angk,))
        self.op("dve", lambda e: e.tensor_tensor(out=a32, in0=a32, in1=ang[:, 32:64], op=ALU.add), r=(angk,), w=(angk,))
        self.op("dve", lambda e: e.tensor_scalar(out=ang[:, 32:64], in0=a32, scalar1=-math.pi, scalar2=TWO_PI,
                                                 op0=ALU.is_lt, op1=ALU.mult), r=(angk,), w=(angk,))
        self.op("dve", lambda e: e.tensor_tensor(out=a32, in0=a32, in1=ang[:, 32:64], op=ALU.add), r=(angk,), w=(angk,))
        self.op("act", lambda e: e.activation(out=a32, in_=a32, func=AF.Sin), r=(angk,), w=(angk,))
        sc, sck = ang, angk
        _head_norm(self, pr[:, 0:256].unsqueeze(1), prk, 1, 256, gqa, gqak, st, sk, 8, tmp, tmpk, la[:, 0:256].unsqueeze(1), lak)
        _head_norm(self, pr[:, 256:384].unsqueeze(1), prk, 1, 128, gkva, gkvak, st, sk, 12, tmp, tmpk,
                   la[:, 256:384].unsqueeze(1), lak)
        self.transpose_full(la, lak, 3, qlT, qlTk)

        def evac_q(b, bk, n0, wd):
            ps = self.banks[b]
            self.op("act", lambda e: e.copy(out=q32[:, n0:n0 + wd], in_=ps[:, 0:wd]), r=(bk,), w=(q32k,))

        self.linear(qlT, qlTk, 2, wqb, wqbk, 1536, evac_q)
        k3 = k32[:, :].rearrange("p (h d) -> p h d", d=96)

        def evac_kv(b, bk, n0, wd):
            ps3 = self.banks[b][:, 0:512].rearrange("p (h d) -> p h d", d=128)
            h0 = n0 // 128
            self.op("act", lambda e: e.copy(out=k3[:, h0:h0 + 4, 0:64], in_=ps3[:, :, 0:64]), r=(bk,), w=(k32k,))
            self.op("dve", lambda e: e.tensor_copy(out=VA[:, h0:h0 + 4, 0:64], in_=ps3[:, :, 64:128]), r=(bk,), w=(VAk,))

        self.linear(qlT[:, 2:3, :], qlTk, 1, wkvb, wkvbk, 2048, evac_kv)
        self.op("dve", lambda e: e.tensor_copy(out=k3[:, :, 64:96], in_=pr[:, 384:416].unsqueeze(1).to_broadcast([128, 16, 32])),
                r=(prk,), w=(k32k,))
        q3 = q32[:, :].rearrange("p (h d) -> p h d", d=96)
        qf3 = qf[:, :].rearrange("p (h d) -> p h d", d=96)
        kf3 = kf[:, :].rearrange("p (h d) -> p h d", d=96)
        _head_norm(self, q3, q32k, 16, 96, gq, gqk, st, sk, 16, tmp, tmpk, q3, q32k)
        _head_norm(self, k3, k32k, 16, 96, gkg, gkgk, st, sk, 48, tmp, tmpk, k3, k32k)
        self.op("act", lambda e: e.copy(out=qf3[:, :, 0:64], in_=q3[:, :, 0:64]), r=(q32k,), w=(qfk,))
        self.op("act", lambda e: e.copy(out=kf3[:, :, 0:64], in_=k3[:, :, 0:64]), r=(k32k,), w=(kfk,))
        _rope(self, q3, q32k, sc, sck, r1, r1k, r2, r2k, qf3, qfk, 16)
        _rope(self, k3, k32k, sc, sck, r1, r1k, r2, r2k, kf3, kfk, 16)
        self.transpose_to(qf, qfk, [h * 96 for h in range(16)], 96, QT[:, :].rearrange("p (h t) -> p h t", t=128), QTk)
        self.transpose_to(kf, kfk, [h * 96 for h in range(16)], 96, KT, KTk)
        self.dma_out(ktd[ti].rearrange("p (h t) -> p h t", t=128), KT[:], KTk, w=(("ktd", ti),))
        self.dma_out(vad[ti].rearrange("p (h d) -> p h d", d=65), VA[:], VAk, w=(("vad", ti),))
        ktiles = list(range(i + 1))
        base = ti - i

        def get_kT(j):
            kb, kbk = KTb.next()
            self.dma_in(kb[:], ktd[base + j].rearrange("p (h t) -> p h t", t=128), kbk, r=(("ktd", base + j),))
            return [(kb[:, h, :], kbk, h, 1) for h in range(16)]

        def get_v(j):
            vb, vbk = VAb.next()
            self.dma_in(vb[:], vad[base + j].rearrange("p (h d) -> p h d", d=65), vbk, r=(("vad", base + j),))
            return vb, vbk, (lambda h: h)

        def bias_for(j, c, i=i):
            if j == i:
                return caus[:, :].unsqueeze(1).to_broadcast([128, 4, 128]), "causst"
            return None

        _attend(self, QT, QTk, 96, 96.0 ** -0.5, ktiles, get_kT, get_v, bias_for, None, PTs, tmps)
        ao, aok = aos.get(ti)
        aT, aTk = aTs.get(ti)
        xo, xok = xos.get(ti)
        _attn_finish(self, st, sk, 96, ao, aok)
        _out_proj(self, ao, aok, aT, aTk, wout, woutk, xt, xk, xo, xok, x_dst, dst_id, ti)

    load(0)
    for ti in range(ntile):
        if ti + 1 < ntile:
            load(ti + 1)
        tile_body(ti)
    S.barrier()
    S.recycle()
    self.sb_off = mark


Builder.phase_c = _phase_c


BIS = "dve"
NIT = 18
BIGM = 32768.0


def _phase_a(self, li, ia, x_src, src_id, x_dst, dst_id):
    S = self.S
    self.ng = 3
    _setup_bias(self)
    mark = self.sb_off
    NT = self.NT
    SEQ = self.SEQ
    topk = self.topk
    ntile = self.TOK // 128
    a_w_in = self.din("a_w_in", [2, D, 1736])
    a_w_out = self.din("a_w_out", [2, D, D])
    norm_mix = self.din("norm_mix", [4, D])
    biasD, biasDk = _load_bias_tile(self, "biasD", 0, 0)
    biasOA, biasOAk = _load_bias_tile(self, "biasOA", 0, 1)
    g, gk = self.load_bcast_row(None, "gmix", norm_mix[li, :], D)
    gq, gqk = self.load_bcast_row(None, "gq", self.din("a_q_gain", [2, 64])[ia, :], 64)
    gkt, gkk = self.load_bcast_row(None, "gk", self.din("a_k_gain", [2, 64])[ia, :], 64)
    causts = self.sb(None, "causts", [128, 128], F32)
    self.dma_in(causts[:], self.din("k_caus_ts", [128, 128])[:, :], "causts")
    ctab = self.sb(None, "ctab", [128, NIT], F32)
    cb = self.sb(None, "cb", [128, NT], F32)
    for k in range(NIT):
        self.op("dve", lambda e, k=k: e.memset(ctab[:, k:k + 1], -(2.0 ** -(k + 2))), w=("ctab",))
    for i_ in range(NT):
        self.op("dve", lambda e, i_=i_: e.memset(cb[:, i_:i_ + 1], float((i_ + 1) * 128 - 2 * topk) + 0.5), w=("cb",))
    win, wink = self.load_weight(None, "awin", a_w_in[ia], D, 1736)
    wout, woutk = self.load_weight(None, "awout", a_w_out[ia], D, D)
    KT = self.sb(None, "KT", [64, NT, 128], BF16)
    KIT = self.sb(None, "KIT", [64, NT * 128], BF16)
    VA = self.sb(None, "VA", [128, NT, 65], BF16)
    score = self.sb(None, "score", [128, SEQ], F32)
    Mts = self.rot(None, "Mt", [128, SEQ], BF16, 2)
    self.op("dve", lambda e: e.memset(VA[:, :, 64:65], 1.0), w=tuple(("VA", j) for j in range(NT)))
    xts = self.rot(None, "xt", [128, D], F32, 3)
    hs = self.rot(None, "h", [128, D], BF16, 1)
    hTs = self.rot(None, "hT", [128, 8, 128], BF16, 1)
    sts = self.rot(None, "st", [128, 160], F32, 2)
    prs = self.rot(None, "pr", [128, 1736], F32, 1)
    tmpn = self.rot(None, "tmpn", [128, D], F32, 1)
    qns = self.rot(None, "qn", [128, D], BF16, 1)
    kns = self.rot(None, "kn", [128, 64], BF16, 1)
    qkis = self.rot(None, "qki", [128, 576], BF16, 1)
    QTs = self.rot(None, "QT", [64, 16 * 128], BF16, 2)
    QITs = self.rot(None, "QIT", [64, 8 * 128], BF16, 1)
    rls = self.rot(None, "rl", [128, 512], F32, 3)
    MTs = self.rot(None, "MT", [128, 4, 128], BF16, 3)
    PTs = self.rot(None, "PT", [128, 16 * 128], BF16, 3)
    tmps = self.rot(None, "tmpb", [128, 512], F32, 2)
    aos = self.rot(None, "ao", [128, D], BF16, 1)
    aTs = self.rot(None, "aT", [128, 8, 128], BF16, 1)
    xos = self.rot(None, "xo", [128, D], F32, 1)
    junk = self.sb(None, "junk", [128, D], BF16)
    WS = 8.0 ** -0.5 / 8.0

    def load(ti):
        xt, xk = xts.get(ti)
        self.dma_in(xt[:], x_src[ti * 128:(ti + 1) * 128, :], xk, r=(("xd", src_id, ti),))

    def stage1(ti):
        i = ti % NT
        nk = (i + 1) * 128
        xt, xk, pr, prk, st, sk = _proj_front(self, ti, xts, hs, hTs, sts, prs, g, gk, win, wink, 1736, x_src, src_id, junk)
        tmp, tmpk = tmpn.get(ti)
        qn, qnk = qns.get(ti)
        kn, knk = kns.get(ti)
        qki, qkik = qkis.get(ti)
        QT, QTk = QTs.get(ti)
        QIT, QITk = QITs.get(ti)
        Mt, Mtk = Mts.get(ti)
        _head_norm(self, pr[:, 0:1024].rearrange("p (h d) -> p h d", d=64), prk, 16, 64, gq, gqk, st, sk, 8, tmp, tmpk,
                   qn[:, :].rearrange("p (h d) -> p h d", d=64), qnk)
        _head_norm(self, pr[:, 1024:1088].unsqueeze(1), prk, 1, 64, gkt, gkk, st, sk, 48, tmp, tmpk, kn[:, :].unsqueeze(1), knk)
        self.op("act", lambda e: e.copy(out=qki[:], in_=pr[:, 1152:1728]), r=(prk,), w=(qkik,))
        self.transpose_to(qn, qnk, [h * 64 for h in range(16)], 64, QT[:, :].rearrange("p (h t) -> p h t", t=128), QTk)
        self.transpose_to(kn, knk, [0], 64, KT, ("KT", i), dst_blk0=i)
        self.transpose_to(qki, qkik, [h * 64 for h in range(8)], 64, QIT[:, :].rearrange("p (h t) -> p h t", t=128), QITk)
        self.transpose_to(qki, qkik, [512], 64, KIT[:, :].rearrange("p (j t) -> p j t", t=128), ("KIT", i), dst_blk0=i)
        self.op("act", lambda e: e.copy(out=VA[:, i, 0:64], in_=pr[:, 1088:1152]), r=(prk,), w=(("VA", i),))
        if nk <= topk:
            return
        bk_ = sk + "_bis"
        wsc = st[:, 100:108]
        lo = st[:, 110:111]
        w0 = st[:, 111:112]
        hi = st[:, 112:113]
        acc = st[:, 113:114]
        sg = st[:, 114:115]
        negmid = st[:, 115:116]
        negW = st[:, 120:120 + NIT]
        self.op("dve", lambda e: e.tensor_scalar(out=wsc, in0=pr[:, 1728:1736], scalar1=WS, scalar2=None, op0=ALU.mult),
                r=(prk,), w=(bk_,))
        for c0 in range(0, nk, 512):
            wd = min(512, nk - c0)
            kkeys = tuple(("KIT", j) for j in range(c0 // 128, (c0 + wd) // 128))
            for h in range(8):
                b = self.gbank()
                bkb = "bank%d" % b
                ps = self.banks[b]
                self.op("pe", lambda e, h=h, ps=ps, c0=c0, wd=wd: e.matmul(
                    ps[:, 0:wd], lhsT=QIT[:, h * 128:(h + 1) * 128], rhs=KIT[:, c0:c0 + wd], start=True, stop=True),
                    r=(QITk,) + kkeys, w=(bkb,))
                rl, rlk = rls.next()
                self.op("act", lambda e, ps=ps, rl=rl, wd=wd: e.activation(out=rl[:, 0:wd], in_=ps[:, 0:wd], func=AF.Relu),
                        r=(bkb,), w=(rlk,))
                if h == 0:
                    self.op("dve", lambda e, rl=rl, c0=c0, wd=wd: e.tensor_scalar(
                        out=score[:, c0:c0 + wd], in0=rl[:, 0:wd], scalar1=wsc[:, 0:1], scalar2=None, op0=ALU.mult),
                        r=(rlk, bk_), w=("score",))
                else:
                    self.op("dve", lambda e, rl=rl, c0=c0, wd=wd, h=h: e.scalar_tensor_tensor(
                        out=score[:, c0:c0 + wd], in0=rl[:, 0:wd], scalar=wsc[:, h:h + 1], in1=score[:, c0:c0 + wd],
                        op0=ALU.mult, op1=ALU.add), r=(rlk, bk_, "score"), w=("score",))
        self.op("dve", lambda e: e.tensor_reduce(out=hi, in_=score[:, 0:nk], axis=AX.X, op=ALU.max), r=("score",), w=(bk_,))
        self.op("dve", lambda e: e.tensor_reduce(out=lo, in_=score[:, 0:nk], axis=AX.X, op=ALU.min), r=("score",), w=(bk_,))
        self.op("dve", lambda e: e.tensor_tensor(out=w0, in0=hi, in1=lo, op=ALU.subtract), r=(bk_,), w=(bk_,))
        self.op("dve", lambda e: e.tensor_tensor(out=score[:, nk - 128:nk], in0=score[:, nk - 128:nk], in1=causts[:],
                                                 op=ALU.add), r=("score", "causts"), w=("score",))
        self.op("dve", lambda e: e.tensor_scalar(out=negW, in0=ctab[:, 0:NIT], scalar1=w0, scalar2=None, op0=ALU.mult),
                r=(bk_, "ctab"), w=(bk_,))
        self.op("dve", lambda e: e.scalar_tensor_tensor(out=negmid, in0=w0, scalar=-0.5, in1=lo, op0=ALU.mult,
                                                        op1=ALU.subtract), r=(bk_,), w=(bk_,))
        if BIS == "act":
            for it in range(NIT):
                self.op("act", lambda e: e.activation(out=Mt[:, 0:nk], in_=score[:, 0:nk], func=AF.Sign, bias=negmid,
                                                      accum_out=acc), r=("score", bk_), w=(bk_, Mtk))
                self.op("act", lambda e: e.activation(out=sg, in_=acc, func=AF.Sign, bias=cb[:, i:i + 1]), r=(bk_, "cb"), w=(bk_,))
                self.op("act", lambda e, it=it: e.activation(out=negmid, in_=sg, func=AF.Identity, bias=negmid,
                                                             scale=negW[:, it:it + 1]), r=(bk_,), w=(bk_,))
            self.op("dve", lambda e: e.tensor_tensor(out=lo, in0=negW[:, NIT - 1:NIT], in1=negmid, op=ALU.subtract), r=(bk_,), w=(bk_,))

        else:
            self.op("dve", lambda e: e.tensor_scalar(out=negmid, in0=negmid, scalar1=-1.0, scalar2=None, op0=ALU.mult),
                    r=(bk_,), w=(bk_,))
            self.op("dve", lambda e: e.tensor_scalar(out=negW, in0=negW, scalar1=-2.0, scalar2=None, op0=ALU.mult),
                    r=(bk_,), w=(bk_,))
            for it in range(NIT):
                self.op("dve", lambda e: e.tensor_scalar(out=Mt[:, 0:nk], in0=score[:, 0:nk], scalar1=negmid, scalar2=0.0,
                                                         op0=ALU.is_ge, op1=ALU.add, accum_out=acc),
                        r=("score", bk_), w=(bk_, Mtk))
                self.op("dve", lambda e: e.tensor_scalar(out=sg, in0=acc, scalar1=float(topk) - 0.5, scalar2=0.5,
                                                         op0=ALU.is_ge, op1=ALU.subtract), r=(bk_,), w=(bk_,))
                self.op("dve", lambda e, it=it: e.scalar_tensor_tensor(out=negmid, in0=sg, scalar=negW[:, it:it + 1], in1=negmid,
                                                                       op0=ALU.mult, op1=ALU.add), r=(bk_,), w=(bk_,))
            self.op("dve", lambda e: e.scalar_tensor_tensor(out=lo, in0=negW[:, NIT - 1:NIT], scalar=-0.5, in1=negmid,
                                                            op0=ALU.mult, op1=ALU.add), r=(bk_,), w=(bk_,))
        self.op("dve", lambda e: e.tensor_scalar(out=Mt[:, 0:nk], in0=score[:, 0:nk], scalar1=lo, scalar2=None,
                                                 op0=ALU.is_ge), r=("score", bk_), w=(Mtk,))

    def stage2(ti):
        i = ti % NT
        nk = (i + 1) * 128
        select = nk > topk
        xt, xk = xts.get(ti)
        st, sk = sts.get(ti)
        QT, QTk = QTs.get(ti)
        Mt, Mtk = Mts.get(ti)

        def get_kT(j):
            return [(KT[:, j, :], ("KT", j), 0, 16)]

        def get_v(j):
            return VA[:, j:j + 1, :], ("VA", j), (lambda h: 0)

        def bias_for(j, c):
            if j == i:
                return biasD[:, 4 * c:4 * c + 4, :], biasDk
            if j == i - 1:
                return biasOA[:, 4 * c:4 * c + 4, :], biasOAk
            return None

        def mask_for(j):
            MT, MTk = MTs.next()
            b = self.gbank()
            bkb = "bank%d" % b
            pb = self.banks[b].bitcast(BF16)
            self.op("pe", lambda e: e.transpose(out=pb[:, 0:128], in_=Mt[:, j * 128:(j + 1) * 128], identity=self.ident[:]),
                    r=(Mtk, "ident"), w=(bkb,))
            self.op("dve", lambda e: e.tensor_scalar(out=MT[:, :, :], in0=pb[:, 0:128].unsqueeze(1).to_broadcast([128, 4, 128]),
                                                     scalar1=-1.0, scalar2=BIGM, op0=ALU.add, op1=ALU.mult),
                    r=(bkb,), w=(MTk,))
            return MT[:, :, :].rearrange("p a b -> p (a b)"), MTk

        _attend(self, QT, QTk, 64, 0.125, list(range(i + 1)), get_kT, get_v, bias_for, mask_for if select else None, PTs, tmps)
        ao, aok = aos.get(ti)
        aT, aTk = aTs.get(ti)
        xo, xok = xos.get(ti)
        _attn_finish(self, st, sk, 64, ao, aok)
        _out_proj(self, ao, aok, aT, aTk, wout, woutk, xt, xk, xo, xok, x_dst, dst_id, ti)

    load(0)
    if ntile > 1:
        load(1)
    stage1(0)
    for ti in range(ntile):
        if ti + 2 < ntile:
            load(ti + 2)
        nxt = ti + 1 < ntile
        if nxt and (ti + 1) % NT != 0:
            stage1(ti + 1)
            stage2(ti)
        else:
            stage2(ti)
            if nxt:
                stage1(ti + 1)
    S.barrier()
    S.recycle()
    self.sb_off = mark


Builder.phase_a = _phase_a


FULL_PHASES = [("a", 0), ("mlp", 0), ("b", 1), ("mlp", 1), ("c", 2), ("mlp", 2), ("a", 3), ("mlp", 3)]
LAUNCH_PLAN = [FULL_PHASES]


def _run_group(phases, xs, pos, inputs, SEQ, NSEQ, topk):
    B = build_program(phases, SEQ, NSEQ, topk)
    consts = const_inputs(B)
    in_maps = []
    for c in range(len(xs)):
        m = {}
        for name in B.dram_in:
            if name == "x":
                m[name] = xs[c]
            elif name == "positions":
                m[name] = pos[c]
            elif name in consts:
                m[name] = consts[name]
            else:
                m[name] = inputs[name]
        in_maps.append(m)
    res = run_bass_kernel_spmd(B.nc, in_maps, core_ids=list(range(len(xs))))
    return [np.asarray(r["out"]) for r in res.results]


def kernel(**inputs):
    inputs = {k: np.ascontiguousarray(np.asarray(v)) for k, v in inputs.items()}
    x = inputs["x"]
    Bsz, SEQ, _ = x.shape
    NSEQ = Bsz // NCORES
    topk = min(256, SEQ // 4)
    xs = [np.ascontiguousarray(x[c * NSEQ:(c + 1) * NSEQ].reshape(NSEQ * SEQ, D)) for c in range(NCORES)]
    pos = [np.ascontiguousarray(inputs["positions"][c * NSEQ:(c + 1) * NSEQ].reshape(NSEQ * SEQ, 1).astype(np.int32))
           for c in range(NCORES)]
    for group in LAUNCH_PLAN:
        xs = _run_group(group, xs, pos, inputs, SEQ, NSEQ, topk)
    out = np.stack([o.reshape(NSEQ, SEQ, D) for o in xs], axis=0).reshape(Bsz, SEQ, D)
    return out.astype(np.float32, copy=False)
```

```python
import math
from contextlib import ExitStack

import numpy as np
import concourse.bass as bass
import concourse.mybir as mybir
from concourse.bass_utils import run_bass_kernel_spmd

F32 = mybir.dt.float32
BF16 = mybir.dt.bfloat16
I32 = mybir.dt.int32
AF = mybir.ActivationFunctionType
ALU = mybir.AluOpType
AX = mybir.AxisListType

D = 1024
DFF = 4096
EPS = 1e-6
NEG = -30000.0
NCORES = 8


class Op:
    __slots__ = ("idx", "eng", "eidx", "fn", "cdeps", "dwaits", "is_dma", "dkey", "dh", "sig", "sigidx")


class Sched:
    ENGS = ("pe", "act", "dve", "pool", "sp")

    def __init__(self, nc):
        self.nc = nc
        self.ops = []
        self.eops = {e: [] for e in self.ENGS}
        self.kw = {}
        self.kr = {}
        self.dsem = {}
        self.excl = set()
        self.pending = {}
        self.free_dsems = {"sp": [], "pool": [], "act": []}

    def add(self, eng, fn, r=(), w=(), dma=None):
        op = Op()
        op.idx = len(self.ops)
        op.eng = eng
        op.eidx = len(self.eops[eng])
        op.fn = fn
        op.is_dma = dma is not None
        op.dkey = dma
        op.cdeps = {}
        op.dwaits = {}
        op.sig = False
        op.sigidx = 0

        def dep(p, kind):
            if p is None or p is op:
                return
            if p.is_dma:
                if op.is_dma and p.dkey == dma and kind == "waw":
                    return
                ent = self.dsem.get(p.dkey)
                if ent is None or ent[0] is not p.dh:
                    return
                op.dwaits[id(ent[0])] = (ent[0], ent[1])
                return
            if p.eng == eng and not op.is_dma:
                if eng == "pe":
                    return
            cur = op.cdeps.get(p.eng)
            if cur is None or p.eidx > cur.eidx:
                op.cdeps[p.eng] = p

        for k in r:
            dep(self.kw.get(k), "raw")
            rd = self.kr.setdefault(k, {})
            if k in self.excl:
                for q in rd.values():
                    if q.eng != eng:
                        dep(q, "war")
            rd[("d", dma) if op.is_dma else ("c", eng)] = op
        for k in w:
            dep(self.kw.get(k), "waw")
            for q in self.kr.get(k, {}).values():
                dep(q, "war")
            self.kw[k] = op
            self.kr[k] = {}
        pb = self.pending.pop(eng, None)
        if pb is not None:
            for p in pb[0]:
                if p.eng != eng:
                    dep(p, "raw")
            for h, c in pb[1]:
                old = op.dwaits.get(id(h))
                if old is None or old[1] < c:
                    op.dwaits[id(h)] = (h, c)
        if op.is_dma:
            if dma not in self.dsem:
                if self.free_dsems[eng]:
                    self.dsem[dma] = self.free_dsems[eng].pop()
                else:
                    self.nsem = getattr(self, "nsem", 0) + 1
                    self.dsem[dma] = [self.nc.alloc_semaphore("d_%d" % self.nsem), 0, eng]
            self.dsem[dma][1] += 16
            op.dh = self.dsem[dma][0]
        self.ops.append(op)
        self.eops[eng].append(op)
        return op

    def barrier(self):
        last = [l[-1] for l in self.eops.values() if l and not l[-1].is_dma]
        for l in self.eops.values():
            for o in reversed(l):
                if not o.is_dma:
                    if o not in last:
                        last.append(o)
                    break
        dw = [(v[0], v[1]) for v in self.dsem.values()]
        for e in self.ENGS:
            old = self.pending.get(e)
            if old is None:
                self.pending[e] = (list(last), list(dw))
            else:
                old[0].extend(last)
                old[1].extend(dw)

    def recycle(self, keep=()):
        for k in list(self.dsem.keys()):
            if k in keep:
                continue
            ent = self.dsem.pop(k)
            self.free_dsems[ent[2]].append(ent)

    def emit(self):
        nc = self.nc
        for op in self.ops:
            for p in op.cdeps.values():
                p.sig = True
        esem = {}
        for e in self.ENGS:
            n = 0
            for op in self.eops[e]:
                if op.sig:
                    n += 1
                    op.sigidx = n
            if n:
                esem[e] = nc.alloc_semaphore("e_" + e)
        sched = self

        def run(e, eng):
            waited = {}
            for op in sched.eops[e]:
                for pe_, p in op.cdeps.items():
                    if waited.get(pe_, 0) < p.sigidx:
                        eng.wait_ge(esem[pe_], p.sigidx)
                        waited[pe_] = p.sigidx
                for hid, (h, c) in op.dwaits.items():
                    if waited.get(hid, 0) < c:
                        eng.wait_ge(h, c)
                        waited[hid] = c
                ins = op.fn(eng)
                if op.is_dma:
                    ins.then_inc(op.dh, 16)
                elif op.sig:
                    ins.then_inc(esem[e], 1)

        with nc.Block() as block:
            @block.tensor
            def _(eng):
                run("pe", eng)

            @block.scalar
            def _(eng):
                run("act", eng)

            @block.vector
            def _(eng):
                run("dve", eng)

            @block.gpsimd
            def _(eng):
                run("pool", eng)

            @block.sync
            def _(eng):
                run("sp", eng)


def _t5_bucket(n):
    n = np.maximum(n, 0)
    nf = np.maximum(n, 1).astype(np.float32)
    large = 16 + (np.log(nf / np.float32(16)) / np.float32(math.log(128 / 16)) * np.float32(16)).astype(np.int32)
    large = np.minimum(large, 31)
    return np.where(n < 16, n, large)


TW = 384


def _host_consts():
    c = {}
    c["ident"] = np.eye(128, dtype=np.float32)
    rel = np.arange(TW) - 127
    b = _t5_bucket(rel)
    oh = np.zeros((2, 33, TW), np.float32)
    for v in range(2):
        for r in range(TW):
            if rel[r] >= 0:
                oh[v, b[r], r] += 1.0
                oh[v, 31, r] -= 1.0
            masked = rel[r] < 0 or (v == 1 and rel[r] >= 128)
            oh[v, 32, r] = NEG if masked else 0.0
    c["t5oh"] = oh
    t = np.arange(128)
    c["caus_ts"] = np.where(t[None, :] <= t[:, None], 0.0, -1e30).astype(np.float32)
    c["caus_st"] = np.where(t[:, None] <= t[None, :], 0.0, NEG).astype(np.float32)
    inv_freq = (10000.0 ** (-np.arange(0, 32, 2, dtype=np.float32) / np.float32(32))).astype(np.float32)
    c["invf"] = np.broadcast_to(inv_freq[None, :], (128, 16)).copy()
    return c


class Builder:
    def __init__(self, phases, SEQ, NSEQ, topk):
        self.phases = phases
        self.SEQ = SEQ
        self.NSEQ = NSEQ
        self.NT = SEQ // 128
        self.TOK = SEQ * NSEQ
        self.topk = topk
        self.nc = bass.Bass("TRN2", target_bir_lowering=False)
        self.S = Sched(self.nc)
        self.uid = 0
        self.dram_in = {}
        self.sb_off = 16512
        self.sb_peak = 0
        self.sb_cap = 229312

    def din(self, name, shape, dt=F32):
        if name not in self.dram_in:
            self.dram_in[name] = self.nc.dram_tensor(name, list(shape), dt, kind="ExternalInput").ap()
        return self.dram_in[name]

    def sb(self, es, name, shape, dt):
        self.uid += 1
        esz = 2 if dt == BF16 else 4
        n = 1
        for d in shape[1:]:
            n *= d
        nbytes = (n * esz + 63) // 64 * 64
        off = self.sb_off
        self.sb_off += nbytes
        assert self.sb_off <= self.sb_cap, "SBUF overflow: %d > %d (%s)" % (self.sb_off, self.sb_cap, name)
        self.sb_peak = max(self.sb_peak, self.sb_off)
        return self.nc.alloc_sbuf_tensor_at("%s_%d" % (name, self.uid), list(shape), dt, offset=off)

    def op(self, eng, fn, r=(), w=(), dma=None):
        return self.S.add(eng, fn, r, w, dma)

    def dma_in(self, dst_ap, src_ap, tkey, r=(), w=(), eng="sp"):
        return self.op(eng, lambda e: e.dma_start(out=dst_ap, in_=src_ap), r=r, w=tuple(w) + (tkey,), dma=tkey)

    def dma_out(self, dst_ap, src_ap, tkey, r=(), w=(), eng="sp"):
        return self.op(eng, lambda e: e.dma_start(out=dst_ap, in_=src_ap), r=tuple(r) + (tkey,), w=w, dma=tkey)

    def setup_psum(self, es):
        self.banks = []
        for i in range(8):
            t = self.nc.alloc_psum_tensor("bank%d" % i, [128, 512], F32)
            self.banks.append(t)
            self.S.excl.add("bank%d" % i)
        self.grot = 0
        self.ng = 3

    def gbank(self):
        i = self.grot % self.ng
        self.grot += 1
        return i

    def setup_consts(self, es):
        hc = _host_consts()
        self.host_consts = hc
        nc = self.nc
        ident_f = self.sb(es, "identf", [128, 128], F32)
        self.ident = self.sb(es, "ident", [128, 128], BF16)
        d = self.din("k_ident", [128, 128])
        self.dma_in(ident_f[:], d[:, :], "identf")
        self.op("dve", lambda e: e.tensor_copy(out=self.ident[:], in_=ident_f[:]), r=("identf",), w=("ident",))
        self.eps_t = self.sb(es, "eps", [128, 1], F32)
        self.op("dve", lambda e: e.memset(self.eps_t[:], EPS), w=("eps",))

    def transpose_to(self, src_tile, src_key, col_offs, width, dst3, dst_key, dst_blk0=0, evac_eng="act"):
        n = len(col_offs)
        i0 = 0
        while i0 < n:
            cnt = min(8, n - i0)
            b = self.gbank()
            bk = "bank%d" % b
            pb = self.banks[b].bitcast(BF16)
            for j in range(cnt):
                off = col_offs[i0 + j]
                self.op("pe", lambda e, j=j, off=off, pb=pb: e.transpose(
                    out=pb[0:width, j * 128:(j + 1) * 128], in_=src_tile[:, off:off + width], identity=self.ident[:]),
                    r=(src_key, "ident"), w=(bk,))
            dst = dst3[0:width, dst_blk0 + i0:dst_blk0 + i0 + cnt, :]
            src = pb[0:width, 0:cnt * 128].rearrange("p (a b) -> p a b", b=128)
            if evac_eng == "act":
                self.op("act", lambda e, dst=dst, src=src: e.copy(out=dst, in_=src), r=(bk,), w=(dst_key,))
            else:
                self.op("dve", lambda e, dst=dst, src=src: e.tensor_copy(out=dst, in_=src), r=(bk,), w=(dst_key,))
            i0 += cnt

    def transpose_full(self, src_tile, src_key, nblk, dstT, dst_key, evac_eng="act"):
        i0 = 0
        while i0 < nblk:
            cnt = min(8, nblk - i0)
            b = self.gbank()
            bk = "bank%d" % b
            pb = self.banks[b].bitcast(BF16)
            for j in range(cnt):
                off = (i0 + j) * 128
                self.op("pe", lambda e, j=j, off=off, pb=pb: e.transpose(
                    out=pb[:, j * 128:(j + 1) * 128], in_=src_tile[:, off:off + 128], identity=self.ident[:]),
                    r=(src_key, "ident"), w=(bk,))
            dst = dstT[:, i0:i0 + cnt, :]
            src = pb[:, 0:cnt * 128].rearrange("p (a b) -> p a b", b=128)
            if evac_eng == "act":
                self.op("act", lambda e, dst=dst, src=src: e.copy(out=dst, in_=src), r=(bk,), w=(dst_key,))
            else:
                self.op("dve", lambda e, dst=dst, src=src: e.tensor_copy(out=dst, in_=src), r=(bk,), w=(dst_key,))
            i0 += cnt

    def linear(self, xT, xT_key, nk, W, W_key, N, evac):
        n0 = 0
        while n0 < N:
            wd = min(512, N - n0)
            b = self.gbank()
            bk = "bank%d" % b
            ps = self.banks[b]
            for k in range(nk):
                self.op("pe", lambda e, k=k, n0=n0, wd=wd, ps=ps: e.matmul(
                    ps[:, 0:wd], lhsT=xT[:, k, :], rhs=W[:, k, n0:n0 + wd], start=(k == 0), stop=(k == nk - 1)),
                    r=(xT_key, W_key), w=(bk,))
            evac(b, bk, n0, wd)
            n0 += wd

    def load_weight(self, es, name, dram_ap2d, K, N):
        nk = max(1, K // 128)
        kp = min(K, 128)
        t = self.sb(es, name, [kp, nk, N], BF16)
        key = name + "_%d" % self.uid
        src = dram_ap2d.rearrange("(k p) n -> p k n", p=kp)
        step = max(1, min(nk, 8192 // N if N <= 8192 else 1))
        k0 = 0
        while k0 < nk:
            k1 = min(nk, k0 + step)
            self.dma_in(t[:, k0:k1, :], src[:, k0:k1, :], key, eng="pool")
            k0 = k1
        return t, key

    def load_bcast_row(self, es, name, dram_row_ap, n):
        t = self.sb(es, name, [128, n], F32)
        key = name + "_%d" % self.uid
        src = bass.AP(tensor=dram_row_ap.tensor, offset=dram_row_ap.offset, ap=[[0, 128], [1, n]])
        self.dma_in(t[:], src, key)
        return t, key

    def rms_scale(self, es_tiles, ssq_ap, ssq_key, out_ap, out_key, n, shape_cols):
        self.op("act", lambda e: e.activation(out=out_ap, in_=ssq_ap, func=AF.Ln, bias=self.eps_t[:, 0:1], scale=1.0 / n),
                r=(ssq_key, "eps"), w=(out_key,))
        self.op("act", lambda e: e.activation(out=out_ap, in_=out_ap, func=AF.Exp, scale=-0.5),
                r=(out_key,), w=(out_key,))

    def rot(self, es, name, shape, dt, n=2):
        tiles = []
        for i in range(n):
            t = self.sb(es, name + str(i), shape, dt)
            tiles.append((t, "%s%d_%d" % (name, i, self.uid)))
        return Rot(tiles)

    def tile_norm(self, xt, xk, st, sk, col, g, gk, h, hk, junk):
        ssq = st[:, col:col + 1]
        rstd = st[:, col + 1:col + 2]
        self.op("act", lambda e: e.activation(out=junk[:], in_=xt[:], func=AF.Square, accum_out=ssq), r=(xk, sk), w=(sk,))
        self.rms_scale(None, ssq, sk, rstd, sk, D, 1)
        self.op("dve", lambda e: e.scalar_tensor_tensor(out=h[:], in0=xt[:], scalar=rstd, in1=g[:], op0=ALU.mult,
                                                        op1=ALU.mult), r=(xk, sk, gk), w=(hk,))

    def phase_mlp(self, li, x_src, src_id, x_dst, dst_id):
        S = self.S
        ntile = self.TOK // 128
        self.ng = 8
        w_up = self.din("w_up", [4, D, DFF])
        w_down = self.din("w_down", [4, DFF, D])
        norm_mlp = self.din("norm_mlp", [4, D])
        mark = self.sb_off
        with ExitStack() as es:
            g, gk = self.load_bcast_row(es, "gml", norm_mlp[li, :], D)
            wup, wupk = self.load_weight(es, "wup", w_up[li], D, DFF)
            wdn, wdnk = self.load_weight(es, "wdn", w_down[li], DFF, D)
            xts = self.rot(es, "xt", [128, D], F32, 3)
            hs = self.rot(es, "h", [128, D], BF16, 2)
            hTs = self.rot(es, "hT", [128, 8, 128], BF16, 2)
            sts = self.rot(es, "st", [128, 4], F32, 2)
            rls = self.rot(es, "rl", [128, 512], F32, 3)
            hids = self.rot(es, "hid", [128, DFF], BF16, 2)
            hidTs = self.rot(es, "hidT", [128, 32, 128], BF16, 2)
            xos = self.rot(es, "xo", [128, D], F32, 2)
            junk = self.sb(es, "junk", [128, D], BF16)

            def load(ti):
                xt, xk = xts.get(ti)
                self.dma_in(xt[:], x_src[ti * 128:(ti + 1) * 128, :], xk, r=(("xd", src_id, ti),))

            def front(ti):
                xt, xk = xts.get(ti)
                h, hk = hs.get(ti)
                hT, hTk = hTs.get(ti)
                st, sk = sts.get(ti)
                self.tile_norm(xt, xk, st, sk, 0, g, gk, h, hk, junk)
                self.transpose_full(h, hk, 8, hT, hTk)

            def body(ti):
                xt, xk = xts.get(ti)
                hT, hTk = hTs.get(ti)
                hid, hidk = hids.get(ti)
                hidT, hidTk = hidTs.get(ti)
                xo, xok = xos.get(ti)

                def evac_up(b, bk, n0, wd):
                    rl, rlk = rls.next()
                    ps = self.banks[b]
                    self.op("act", lambda e: e.activation(out=rl[:, 0:wd], in_=ps[:, 0:wd], func=AF.Relu), r=(bk,), w=(rlk,))
                    self.op("dve", lambda e: e.tensor_tensor(out=hid[:, n0:n0 + wd], in0=rl[:, 0:wd], in1=rl[:, 0:wd],
                                                             op=ALU.mult), r=(rlk,), w=(hidk,))

                self.linear(hT, hTk, 8, wup, wupk, DFF, evac_up)
                self.transpose_full(hid, hidk, 32, hidT, hidTk)

                def evac_dn(b, bk, n0, wd):
                    ps = self.banks[b]
                    self.op("dve", lambda e: e.tensor_tensor(out=xo[:, n0:n0 + wd], in0=ps[:, 0:wd], in1=xt[:, n0:n0 + wd],
                                                             op=ALU.add), r=(bk, xk), w=(xok,))

                self.linear(hidT, hidTk, 32, wdn, wdnk, D, evac_dn)
                self.dma_out(x_dst[ti * 128:(ti + 1) * 128, :], xo[:], xok, w=(("xd", dst_id, ti),))

            load(0)
            if ntile > 1:
                load(1)
            front(0)
            for ti in range(ntile):
                if ti + 2 < ntile:
                    load(ti + 2)
                if ti + 1 < ntile:
                    front(ti + 1)
                body(ti)
        S.barrier()
        S.recycle()
        self.sb_off = mark


class Rot:
    def __init__(self, tiles):
        self.tiles = tiles
        self.i = 0

    def get(self, i):
        return self.tiles[i % len(self.tiles)]

    def next(self):
        t = self.tiles[self.i % len(self.tiles)]
        self.i += 1
        return t


def build_program(phases, SEQ, NSEQ, topk):
    B = Builder(phases, SEQ, NSEQ, topk)
    nc = B.nc
    TOK = B.TOK
    B.setup_psum(None)
    B.setup_consts(None)
    x_in = B.din("x", [TOK, D])
    out = nc.dram_tensor("out", [TOK, D], F32, kind="ExternalOutput").ap()
    scr = [nc.dram_tensor("xs%d" % i, [TOK, D], F32, kind="Internal").ap() for i in range(2)] if len(phases) > 1 else []
    cur, cur_id = x_in, "in"
    for pi, (kind, li) in enumerate(phases):
        last = pi == len(phases) - 1
        dst, dst_id = (out, "out") if last else (scr[pi % 2], "s%d_%d" % (pi % 2, pi))
        if kind == "mlp":
            B.phase_mlp(li, cur, cur_id, dst, dst_id)
        elif kind == "a":
            B.phase_a(li, li // 3, cur, cur_id, dst, dst_id)
        elif kind == "b":
            B.phase_b(li, cur, cur_id, dst, dst_id)
        elif kind == "c":
            B.phase_c(li, cur, cur_id, dst, dst_id)
        cur, cur_id = dst, dst_id
    B.S.barrier()
    B.op("sp", lambda e: e.nop())
    B.S.emit()
    return B


def const_inputs(B):
    hc = B.host_consts
    m = {}
    for name in B.dram_in:
        if name.startswith("k_"):
            m[name] = np.ascontiguousarray(hc[name[2:]])
    return m


def _setup_bias(self):
    if getattr(self, "bias_ready", False):
        return
    self.bias_ready = True
    nc = self.nc
    rel_bias = self.din("rel_bias", [32, 16])
    oh_d = self.din("k_t5oh", [2, 33, TW])
    rb = self.sb(None, "rbaug", [33, 16], F32)
    self.op("dve", lambda e: e.memset(rb[32:33, :], 1.0), w=("rbaug",))
    self.dma_in(rb[0:32, :], rel_bias[:, :], "rbaug")
    oh = self.sb(None, "t5oh", [33, 2, TW], F32)
    self.dma_in(oh[:], oh_d.rearrange("v b r -> b v r"), "t5oh")
    fsb = self.sb(None, "fsb", [16, 2, TW], F32)
    fd = nc.dram_tensor("fd_scr", [2, 16, TW], F32, kind="Internal").ap()
    for v in range(2):
        b = self.gbank()
        bk = "bank%d" % b
        ps = self.banks[b]
        self.op("pe", lambda e, v=v, ps=ps: e.matmul(ps[0:16, 0:TW], lhsT=rb[:, :], rhs=oh[:, v, :], start=True, stop=True),
                r=("rbaug", "t5oh"), w=(bk,))
        self.op("dve", lambda e, v=v, ps=ps: e.tensor_copy(out=fsb[:, v, :], in_=ps[0:16, 0:TW]), r=(bk,), w=("fsb",))
    self.dma_out(fd.rearrange("v h r -> h v r"), fsb[:], "fsb", w=("fd",))
    self.fd = fd


def _load_bias_tile(self, name, v, delta):
    fd = self.fd
    tile = self.sb(None, name, [128, 16, 128], F32)
    key = "%s_%d" % (name, self.uid)
    for s_ in range(128):
        src = bass.AP(tensor=fd.tensor, offset=v * 16 * TW + 127 - s_ + 128 * delta, ap=[[0, 1], [TW, 16], [1, 128]])
        self.dma_in(tile[s_:s_ + 1, :, :], src, key, r=("fd",))
    return tile, key


def _proj_front(self, ti, xts, hs, hTs, sts, prs, g, gk, win, wink, nin, x_src, src_id, junk):
    xt, xk = xts.get(ti)
    h, hk = hs.get(ti)
    hT, hTk = hTs.get(ti)
    st, sk = sts.get(ti)
    pr, prk = prs.get(ti)
    self.tile_norm(xt, xk, st, sk, 0, g, gk, h, hk, junk)
    self.transpose_full(h, hk, 8, hT, hTk)

    def evac(b, bk, n0, wd):
        ps = self.banks[b]
        self.op("act", lambda e: e.copy(out=pr[:, n0:n0 + wd], in_=ps[:, 0:wd]), r=(bk,), w=(prk,))

    self.linear(hT, hTk, 8, win, wink, nin, evac)
    return xt, xk, pr, prk, st, sk


def _head_norm(self, src3, srck, H, dh, gbc, gbck, st, sk, col, tmp, tmpk, out3, outk):
    t3 = tmp[:, 0:H * dh].rearrange("p (h d) -> p h d", d=dh)
    ssq = st[:, col:col + H]
    rstd = st[:, col + H:col + 2 * H]
    self.op("dve", lambda e: e.tensor_tensor(out=t3, in0=src3, in1=src3, op=ALU.mult), r=(srck,), w=(tmpk,))
    self.op("dve", lambda e: e.tensor_reduce(out=ssq, in_=t3, axis=AX.X, op=ALU.add), r=(tmpk,), w=(sk,))
    self.rms_scale(None, ssq, sk, rstd, sk, dh, H)
    self.op("dve", lambda e: e.tensor_tensor(out=t3, in0=src3, in1=rstd.unsqueeze(2).to_broadcast([128, H, dh]),
                                             op=ALU.mult), r=(srck, sk), w=(tmpk,))
    self.op("dve", lambda e: e.tensor_tensor(out=out3, in0=t3, in1=gbc[:, 0:dh].unsqueeze(1).to_broadcast([128, H, dh]),
                                             op=ALU.mult), r=(tmpk, gbck), w=(outk,))


PVB = 5


def _pv_slot(h):
    return PVB + h // 7, (h % 7) * 65


def _attend(self, QT, QTk, dk, scale, ktiles, get_kT, get_v, bias_for, mask_for, PTs, tmps):
    started = set()
    nkt = len(ktiles)
    state = {}

    def stage1(jj):
        j = ktiles[jj]
        groups = get_kT(j)
        PT, PTk = PTs.next()
        state[jj] = (PT, PTk)
        m = mask_for(j) if mask_for is not None else None
        for c in range(4):
            b = 3 + (c % 2)
            bk = "bank%d" % b
            ps = self.banks[b]
            for (kT, kk, h0, nh) in groups:
                lo, hi = max(h0, 4 * c), min(h0 + nh, 4 * c + 4)
                if lo >= hi:
                    continue
                self.op("pe", lambda e, kT=kT, lo=lo, hi=hi, ps=ps, c=c: e.matmul(
                    ps[:, (lo - 4 * c) * 128:(hi - 4 * c) * 128], lhsT=kT, rhs=QT[0:dk, lo * 128:hi * 128],
                    start=True, stop=(m is None), skip_group_check=True), r=(kk, QTk), w=(bk,))
            if m is not None:
                map_, mkey = m
                self.op("pe", lambda e, ps=ps, map_=map_: e.matmul(
                    ps[:, 0:512], lhsT=self.ident[:, :], rhs=map_, start=False, stop=True, skip_group_check=True),
                    r=(mkey, "ident"), w=(bk,))
            bias = bias_for(j, c)
            pt_c = PT[:, c * 512:(c + 1) * 512]
            if bias is None:
                self.op("act", lambda e, ps=ps, pt_c=pt_c: e.activation(out=pt_c, in_=ps[:, :], func=AF.Exp, scale=scale),
                        r=(bk,), w=(PTk,))
            else:
                bap, bkey = bias
                tmp, tmpk = tmps.next()
                self.op("dve", lambda e, ps=ps, tmp=tmp, bap=bap: e.scalar_tensor_tensor(
                    out=tmp[:, 0:512].rearrange("p (a b) -> p a b", b=128), in0=ps[:, :].rearrange("p (a b) -> p a b", b=128),
                    scalar=scale, in1=bap, op0=ALU.mult, op1=ALU.add), r=(bk, bkey), w=(tmpk,))
                self.op("act", lambda e, tmp=tmp, pt_c=pt_c: e.activation(out=pt_c, in_=tmp[:, 0:512], func=AF.Exp),
                        r=(tmpk,), w=(PTk,))

    def stage2(jj):
        j = ktiles[jj]
        PT, PTk = state.pop(jj)
        v3, vk, vh = get_v(j)
        for h in range(16):
            b, off = _pv_slot(h)
            bk = "bank%d" % b
            st_flag = b not in started
            started.add(b)
            self.op("pe", lambda e, h=h, b=b, off=off, PT=PT, v3=v3, st_flag=st_flag, last=(jj == nkt - 1): e.matmul(
                self.banks[b][:, off:off + 65], lhsT=PT[:, h * 128:(h + 1) * 128], rhs=v3[:, vh(h), :],
                start=st_flag, stop=last, skip_group_check=True), r=(PTk, vk), w=(bk,))

    stage1(0)
    for jj in range(nkt):
        if jj + 1 < nkt:
            stage1(jj + 1)
        stage2(jj)


def _attn_finish(self, st, sk, col, ao, aok, sinkexp=None):
    den = st[:, col:col + 16]
    rden = st[:, col + 16:col + 32]
    for b in range(PVB, PVB + 3):
        h0 = (b - PVB) * 7
        nh = min(7, 16 - h0)
        bk = "bank%d" % b
        o3 = self.banks[b][:, 0:nh * 65].rearrange("p (h d) -> p h d", d=65)
        self.op("dve", lambda e, o3=o3, h0=h0, nh=nh: e.tensor_copy(out=den[:, h0:h0 + nh].unsqueeze(2), in_=o3[:, :, 64:65]),
                r=(bk,), w=(sk,))
    if sinkexp is not None:
        se, sek = sinkexp
        self.op("dve", lambda e: e.tensor_tensor(out=den, in0=den, in1=se[:, 0:16], op=ALU.add), r=(sk, sek), w=(sk,))
    self.op("dve", lambda e: e.reciprocal(out=rden, in_=den), r=(sk,), w=(sk,))
    ao3 = ao[:, :].rearrange("p (h d) -> p h d", d=64)
    for b in range(PVB, PVB + 3):
        h0 = (b - PVB) * 7
        nh = min(7, 16 - h0)
        bk = "bank%d" % b
        o3 = self.banks[b][:, 0:nh * 65].rearrange("p (h d) -> p h d", d=65)
        self.op("dve", lambda e, o3=o3, h0=h0, nh=nh: e.tensor_tensor(
            out=ao3[:, h0:h0 + nh, :], in0=o3[:, :, 0:64], in1=rden[:, h0:h0 + nh].unsqueeze(2).to_broadcast([128, nh, 64]),
            op=ALU.mult), r=(bk, sk), w=(aok,))


def _out_proj(self, ao, aok, aT, aTk, wout, woutk, xt, xk, xo, xok, x_dst, dst_id, ti):
    self.transpose_full(ao, aok, 8, aT, aTk)

    def evac(b, bk, n0, wd):
        ps = self.banks[b]
        self.op("dve", lambda e: e.tensor_tensor(out=xo[:, n0:n0 + wd], in0=ps[:, 0:wd], in1=xt[:, n0:n0 + wd], op=ALU.add),
                r=(bk, xk), w=(xok,))

    self.linear(aT, aTk, 8, wout, woutk, D, evac)
    self.dma_out(x_dst[ti * 128:(ti + 1) * 128, :], xo[:], xok, w=(("xd", dst_id, ti),))


def _phase_b(self, li, x_src, src_id, x_dst, dst_id):
    S = self.S
    self.ng = 3
    _setup_bias(self)
    mark = self.sb_off
    NT = self.NT
    ib = 0
    biasD, biasDk = _load_bias_tile(self, "biasD", 0, 0)
    biasOB, biasOBk = _load_bias_tile(self, "biasOB", 1, 1)
    b_w_in = self.din("b_w_in", [1, D, 1536])
    b_w_out = self.din("b_w_out", [1, D, D])
    norm_mix = self.din("norm_mix", [4, D])
    g, gk = self.load_bcast_row(None, "gmix", norm_mix[li, :], D)
    gq, gqk = self.load_bcast_row(None, "gq", self.din("b_q_gain", [1, 64])[ib, :], 64)
    gkk_t, gkk = self.load_bcast_row(None, "gk", self.din("b_k_gain", [1, 64])[ib, :], 64)
    sk_t, skk = self.load_bcast_row(None, "sinks", self.din("b_sinks", [1, 16])[ib, :], 16)
    b31, b31k = self.load_bcast_row(None, "b31", self.din("rel_bias", [32, 16])[31, :], 16)
    self.op("dve", lambda e: e.tensor_tensor(out=sk_t[:], in0=sk_t[:], in1=b31[:], op=ALU.subtract), r=(skk, b31k), w=(skk,))
    self.op("act", lambda e: e.activation(out=sk_t[:], in_=sk_t[:], func=AF.Exp), r=(skk,), w=(skk,))
    win, wink = self.load_weight(None, "bwin", b_w_in[ib], D, 1536)
    wout, woutk = self.load_weight(None, "bwout", b_w_out[ib], D, D)
    xts = self.rot(None, "xt", [128, D], F32, 3)
    hs = self.rot(None, "h", [128, D], BF16, 2)
    hTs = self.rot(None, "hT", [128, 8, 128], BF16, 2)
    sts = self.rot(None, "st", [128, 128], F32, 2)
    prs = self.rot(None, "pr", [128, 1536], F32, 2)
    tmpn = self.rot(None, "tmpn", [128, D], F32, 2)
    qns = self.rot(None, "qn", [128, D], BF16, 2)
    kns = self.rot(None, "kn", [128, 256], BF16, 2)
    QTs = self.rot(None, "QT", [64, 16 * 128], BF16, 2)
    KTs = self.rot(None, "KT", [64, 4, 128], BF16, 3)
    VAs = self.rot(None, "VA", [128, 4, 65], BF16, 3)
    PTs = self.rot(None, "PT", [128, 16 * 128], BF16, 2)
    tmps = self.rot(None, "tmpb", [128, 512], F32, 2)
    aos = self.rot(None, "ao", [128, D], BF16, 2)
    aTs = self.rot(None, "aT", [128, 8, 128], BF16, 2)
    xos = self.rot(None, "xo", [128, D], F32, 2)
    junk = self.sb(None, "junk", [128, D], BF16)
    for (va, vak) in VAs.tiles:
        self.op("dve", lambda e, va=va: e.memset(va[:, :, 64:65], 1.0), w=(vak,))
    ntile = self.TOK // 128

    def load(ti):
        xt, xk = xts.get(ti)
        self.dma_in(xt[:], x_src[ti * 128:(ti + 1) * 128, :], xk, r=(("xd", src_id, ti),))

    def stage1(ti):
        i = ti % NT
        xt, xk, pr, prk, st, sk = _proj_front(self, ti, xts, hs, hTs, sts, prs, g, gk, win, wink, 1536, x_src, src_id, junk)
        tmp, tmpk = tmpn.get(ti)
        qn, qnk = qns.get(ti)
        kn, knk = kns.get(ti)
        QT, QTk = QTs.get(ti)
        KT, KTk = KTs.get(ti)
        VA, VAk = VAs.get(ti)
        _head_norm(self, pr[:, 0:1024].rearrange("p (h d) -> p h d", d=64), prk, 16, 64, gq, gqk, st, sk, 8, tmp, tmpk,
                   qn[:, :].rearrange("p (h d) -> p h d", d=64), qnk)
        _head_norm(self, pr[:, 1024:1280].rearrange("p (h d) -> p h d", d=64), prk, 4, 64, gkk_t, gkk, st, sk, 48, tmp, tmpk,
                   kn[:, :].rearrange("p (h d) -> p h d", d=64), knk)
        self.transpose_to(qn, qnk, [h * 64 for h in range(16)], 64, QT[:, :].rearrange("p (h t) -> p h t", t=128), QTk)
        self.transpose_to(kn, knk, [h * 64 for h in range(4)], 64, KT, KTk)
        self.op("act", lambda e: e.copy(out=VA[:, :, 0:64], in_=pr[:, 1280:1536].rearrange("p (h d) -> p h d", d=64)),
                r=(prk,), w=(VAk,))

    def stage2(ti):
        i = ti % NT
        xt, xk = xts.get(ti)
        st, sk = sts.get(ti)
        QT, QTk = QTs.get(ti)
        ktiles = ([i - 1] if i > 0 else []) + [i]

        def get_kT(j):
            KTj, KTjk = KTs.get(ti - (i - j))
            return [(KTj[:, kv, :], KTjk, kv * 4, 4) for kv in range(4)]

        def get_v(j):
            VAj, VAjk = VAs.get(ti - (i - j))
            return VAj, VAjk, (lambda h: h // 4)

        def bias_for(j, c):
            if j == i:
                return biasD[:, 4 * c:4 * c + 4, :], biasDk
            return biasOB[:, 4 * c:4 * c + 4, :], biasOBk

        _attend(self, QT, QTk, 64, 0.125, ktiles, get_kT, get_v, bias_for, None, PTs, tmps)
        ao, aok = aos.get(ti)
        aT, aTk = aTs.get(ti)
        xo, xok = xos.get(ti)
        _attn_finish(self, st, sk, 64, ao, aok, sinkexp=(sk_t, skk))
        _out_proj(self, ao, aok, aT, aTk, wout, woutk, xt, xk, xo, xok, x_dst, dst_id, ti)

    load(0)
    if ntile > 1:
        load(1)
    stage1(0)
    for ti in range(ntile):
        if ti + 2 < ntile:
            load(ti + 2)
        if ti + 1 < ntile:
            stage1(ti + 1)
        stage2(ti)
    S.barrier()
    S.recycle()
    self.sb_off = mark


Builder.phase_b = _phase_b


def _rope(self, src3, srck, sc, sck, r1, r1k, r2, r2k, dst3, dstk, H):
    t1 = src3[:, :, 64:80]
    t2 = src3[:, :, 80:96]
    sin_b = sc[:, 0:16].unsqueeze(1).to_broadcast([128, H, 16])
    cos_b = sc[:, 16:32].unsqueeze(1).to_broadcast([128, H, 16])
    a = r1[:, 0:H * 16].rearrange("p (h d) -> p h d", d=16)
    b = r2[:, 0:H * 16].rearrange("p (h d) -> p h d", d=16)
    self.op("dve", lambda e: e.tensor_tensor(out=a, in0=t1, in1=cos_b, op=ALU.mult), r=(srck, sck), w=(r1k,))
    self.op("dve", lambda e: e.tensor_tensor(out=b, in0=t2, in1=sin_b, op=ALU.mult), r=(srck, sck), w=(r2k,))
    self.op("dve", lambda e: e.tensor_tensor(out=dst3[:, :, 64:80], in0=a, in1=b, op=ALU.subtract), r=(r1k, r2k), w=(dstk,))
    self.op("dve", lambda e: e.tensor_tensor(out=a, in0=t2, in1=cos_b, op=ALU.mult), r=(srck, sck), w=(r1k,))
    self.op("dve", lambda e: e.tensor_tensor(out=b, in0=t1, in1=sin_b, op=ALU.mult), r=(srck, sck), w=(r2k,))
    self.op("dve", lambda e: e.tensor_tensor(out=dst3[:, :, 80:96], in0=a, in1=b, op=ALU.add), r=(r1k, r2k), w=(dstk,))


def _phase_c(self, li, x_src, src_id, x_dst, dst_id):
    S = self.S
    nc = self.nc
    self.ng = 3
    mark = self.sb_off
    NT = self.NT
    ic = 0
    ntile = self.TOK // 128
    TWO_PI = 2.0 * math.pi
    c_w_in = self.din("c_w_in", [1, D, 416])
    c_w_q_b = self.din("c_w_q_b", [1, 256, 1536])
    c_w_kv_b = self.din("c_w_kv_b", [1, 128, 2048])
    c_w_out = self.din("c_w_out", [1, D, D])
    norm_mix = self.din("norm_mix", [4, D])
    pos_d = self.din("positions", [self.TOK, 1], I32)
    ktd = nc.dram_tensor("ktd_scr", [ntile, 96, 16 * 128], BF16, kind="Internal").ap()
    vad = nc.dram_tensor("vad_scr", [ntile, 128, 16 * 65], BF16, kind="Internal").ap()
    g, gk = self.load_bcast_row(None, "gmix", norm_mix[li, :], D)
    gqa, gqak = self.load_bcast_row(None, "gqa", self.din("c_q_a_gain", [1, 256])[ic, :], 256)
    gkva, gkvak = self.load_bcast_row(None, "gkva", self.din("c_kv_a_gain", [1, 128])[ic, :], 128)
    gq, gqk = self.load_bcast_row(None, "gq", self.din("c_q_gain", [1, 96])[ic, :], 96)
    gkg, gkgk = self.load_bcast_row(None, "gk", self.din("c_k_gain", [1, 96])[ic, :], 96)
    invf = self.sb(None, "invf", [128, 16], F32)
    self.dma_in(invf[:], self.din("k_invf", [128, 16])[:, :], "invf")
    caus = self.sb(None, "causst", [128, 128], F32)
    self.dma_in(caus[:], self.din("k_caus_st", [128, 128])[:, :], "causst")
    negpi = self.sb(None, "negpi", [128, 1], F32)
    win, wink = self.load_weight(None, "cwin", c_w_in[ic], D, 416)
    wqb, wqbk = self.load_weight(None, "cwqb", c_w_q_b[ic], 256, 1536)
    wkvb, wkvbk = self.load_weight(None, "cwkvb", c_w_kv_b[ic], 128, 2048)
    wout, woutk = self.load_weight(None, "cwout", c_w_out[ic], D, D)
    xts = self.rot(None, "xt", [128, D], F32, 2)
    hs = self.rot(None, "h", [128, D], BF16, 2)
    hTs = self.rot(None, "hT", [128, 8, 128], BF16, 2)
    sts = self.rot(None, "st", [128, 256], F32, 2)
    prs = self.rot(None, "pr", [128, 416], F32, 2)
    tmpn = self.rot(None, "tmpn", [128, 1536], F32, 1)
    lat = self.rot(None, "lat", [128, 384], BF16, 2)
    qlTs = self.rot(None, "qlT", [128, 3, 128], BF16, 2)
    q32s = self.rot(None, "q32", [128, 1536], F32, 1)
    k32s = self.rot(None, "k32", [128, 1536], F32, 1)
    qfs = self.rot(None, "qf", [128, 1536], BF16, 2)
    kfs = self.rot(None, "kf", [128, 1536], BF16, 2)
    r1s = self.rot(None, "r1", [128, 256], F32, 2)
    r2s = self.rot(None, "r2", [128, 256], F32, 2)
    posi_s = self.rot(None, "posi", [128, 1], I32, 2)
    angs = self.rot(None, "ang", [128, 64], F32, 2)
    angi = self.rot(None, "angi", [128, 32], I32, 2)
    QTs = self.rot(None, "QT", [96, 16 * 128], BF16, 2)
    KTt = self.rot(None, "KTt", [96, 16, 128], BF16, 2)
    VAt = self.rot(None, "VAt", [128, 16, 65], BF16, 2)
    KTb = self.rot(None, "KTb", [96, 16, 128], BF16, 3)
    VAb = self.rot(None, "VAb", [128, 16, 65], BF16, 3)
    PTs = self.rot(None, "PT", [128, 16 * 128], BF16, 2)
    tmps = self.rot(None, "tmpb", [128, 512], F32, 2)
    aos = self.rot(None, "ao", [128, D], BF16, 2)
    aTs = self.rot(None, "aT", [128, 8, 128], BF16, 2)
    xos = self.rot(None, "xo", [128, D], F32, 2)
    junk = self.sb(None, "junk", [128, D], BF16)
    self.op("dve", lambda e: e.memset(negpi[:], -math.pi), w=("negpi",))
    for (va, vak) in VAt.tiles:
        self.op("dve", lambda e, va=va: e.memset(va[:, :, 64:65], 1.0), w=(vak,))

    def load(ti):
        xt, xk = xts.get(ti)
        self.dma_in(xt[:], x_src[ti * 128:(ti + 1) * 128, :], xk, r=(("xd", src_id, ti),))
        pi_, pik = posi_s.get(ti)
        self.dma_in(pi_[:], pos_d[ti * 128:(ti + 1) * 128, :], pik)

    def tile_body(ti):
        i = ti % NT
        xt, xk, pr, prk, st, sk = _proj_front(self, ti, xts, hs, hTs, sts, prs, g, gk, win, wink, 416, x_src, src_id, junk)
        tmp, tmpk = tmpn.get(ti)
        la, lak = lat.get(ti)
        qlT, qlTk = qlTs.get(ti)
        q32, q32k = q32s.get(ti)
        k32, k32k = k32s.get(ti)
        qf, qfk = qfs.get(ti)
        kf, kfk = kfs.get(ti)
        r1, r1k = r1s.get(ti)
        r2, r2k = r2s.get(ti)
        VA, VAk = VAt.get(ti)
        KT, KTk = KTt.get(ti)
        QT, QTk = QTs.get(ti)
        pi_, pik = posi_s.get(ti)
        ang, angk = angs.get(ti)
        ai, aik = angi.get(ti)
        posf = ang[:, 32:33]
        a32 = ang[:, 0:32]
        kf32 = ang[:, 33:33 + 31]
        self.op("dve", lambda e: e.tensor_copy(out=posf, in_=pi_[:]), r=(pik,), w=(angk,))
        self.op("dve", lambda e: e.tensor_scalar(out=ang[:, 0:16], in0=invf[:], scalar1=posf, scalar2=None, op0=ALU.mult),
                r=(angk, "invf"), w=(angk,))
        self.op("dve", lambda e: e.tensor_scalar(out=ang[:, 16:32], in0=ang[:, 0:16], scalar1=math.pi / 2, scalar2=None,
                                                 op0=ALU.add), r=(angk,), w=(angk,))
        self.op("dve", lambda e: e.tensor_scalar(out=ang[:, 32:64], in0=a32, scalar1=1.0 / TWO_PI, scalar2=None,
                                                 op0=ALU.mult), r=(angk,), w=(angk,))
        self.op("dve", lambda e: e.tensor_copy(out=ai[:], in_=ang[:, 32:64]), r=(angk,), w=(aik,))
        self.op("dve", lambda e: e.tensor_copy(out=ang[:, 32:64], in_=ai[:]), r=(aik,), w=(angk,))
        self.op("dve", lambda e: e.scalar_tensor_tensor(out=a32, in0=ang[:, 32:64], scalar=-TWO_PI, in1=a32, op0=ALU.mult,
                                                        op1=ALU.add), r=(angk,), w=(angk,))
        self.op("dve", lambda e: e.tensor_scalar(out=ang[:, 32:64], in0=a32, scalar1=math.pi, scalar2=-TWO_PI,
                                                 op0=ALU.is_gt, op1=ALU.mult), r=(angk,), w=(angk,))
        self.op("dve", lambda e: e.tensor_tensor(out=a32, in0=a32, in1=ang[:, 32:64], op=ALU.add), r=(angk,), w=(angk,))
        self.op("dve", lambda e: e.tensor_scalar(out=ang[:, 32:64], in0=a32, scalar1=-math.pi, scalar2=TWO_PI,
                                                 op0=ALU.is_lt, op1=ALU.mult), r=(angk,), w=(angk,))
        self.op("dve", lambda e: e.tensor_tensor(out=a32, in0=a32, in1=ang[:, 32:64], op=ALU.add), r=(angk,), w=(angk,))
        self.op("act", lambda e: e.activation(out=a32, in_=a32, func=AF.Sin), r=(angk,), w=(angk,))
        sc, sck = ang, angk
        _head_norm(self, pr[:, 0:256].unsqueeze(1), prk, 1, 256, gqa, gqak, st, sk, 8, tmp, tmpk, la[:, 0:256].unsqueeze(1), lak)
        _head_norm(self, pr[:, 256:384].unsqueeze(1), prk, 1, 128, gkva, gkvak, st, sk, 12, tmp, tmpk,
                   la[:, 256:384].unsqueeze(1), lak)
        self.transpose_full(la, lak, 3, qlT, qlTk)

        def evac_q(b, bk, n0, wd):
            ps = self.banks[b]
            self.op("act", lambda e: e.copy(out=q32[:, n0:n0 + wd], in_=ps[:, 0:wd]), r=(bk,), w=(q32k,))

        self.linear(qlT, qlTk, 2, wqb, wqbk, 1536, evac_q)
        k3 = k32[:, :].rearrange("p (h d) -> p h d", d=96)

        def evac_kv(b, bk, n0, wd):
            ps3 = self.banks[b][:, 0:512].rearrange("p (h d) -> p h d", d=128)
            h0 = n0 // 128
            self.op("act", lambda e: e.copy(out=k3[:, h0:h0 + 4, 0:64], in_=ps3[:, :, 0:64]), r=(bk,), w=(k32k,))
            self.op("dve", lambda e: e.tensor_copy(out=VA[:, h0:h0 + 4, 0:64], in_=ps3[:, :, 64:128]), r=(bk,), w=(VAk,))

        self.linear(qlT[:, 2:3, :], qlTk, 1, wkvb, wkvbk, 2048, evac_kv)
        self.op("dve", lambda e: e.tensor_copy(out=k3[:, :, 64:96], in_=pr[:, 384:416].unsqueeze(1).to_broadcast([128, 16, 32])),
                r=(prk,), w=(k32k,))
        q3 = q32[:, :].rearrange("p (h d) -> p h d", d=96)
        qf3 = qf[:, :].rearrange("p (h d) -> p h d", d=96)
        kf3 = kf[:, :].rearrange("p (h d) -> p h d", d=96)
        _head_norm(self, q3, q32k, 16, 96, gq, gqk, st, sk, 16, tmp, tmpk, q3, q32k)
        _head_norm(self, k3, k32k, 16, 96, gkg, gkgk, st, sk, 48, tmp, tmpk, k3, k32k)
        self.op("act", lambda e: e.copy(out=qf3[:, :, 0:64], in_=q3[:, :, 0:64]), r=(q32k,), w=(qfk,))
        self.op("act", lambda e: e.copy(out=kf3[:, :, 0:64], in_=k3[:, :, 0:64]), r=(k32k,), w=(kfk,))
        _rope(self, q3, q32k, sc, sck, r1, r1k, r2, r2k, qf3, qfk, 16)
        _rope(self, k3, k32k, sc, sck, r1, r1k, r2, r2k, kf3, kfk, 16)
        self.transpose_to(qf, qfk, [h * 96 for h in range(16)], 96, QT[:, :].rearrange("p (h t) -> p h t", t=128), QTk)
        self.transpose_to(kf, kfk, [h * 96 for h in range(16)], 96, KT, KTk)
        self.dma_out(ktd[ti].rearrange("p (h t) -> p h t", t=128), KT[:], KTk, w=(("ktd", ti),))
        self.dma_out(vad[ti].rearrange("p (h d) -> p h d", d=65), VA[:], VAk, w=(("vad", ti),))
        ktiles = list(range(i + 1))
        base = ti - i

        def get_kT(j):
            kb, kbk = KTb.next()
            self.dma_in(kb[:], ktd[base + j].rearrange("p (h t) -> p h t", t=128), kbk, r=(("ktd", base + j),))
            return [(kb[:, h, :], kbk, h, 1) for h in range(16)]

        def get_v(j):
            vb, vbk = VAb.next()
            self.dma_in(vb[:], vad[base + j].rearrange("p (h d) -> p h d", d=65), vbk, r=(("vad", base + j),))
            return vb, vbk, (lambda h: h)

        def bias_for(j, c, i=i):
            if j == i:
                return caus[:, :].unsqueeze(1).to_broadcast([128, 4, 128]), "causst"
            return None

        _attend(self, QT, QTk, 96, 96.0 ** -0.5, ktiles, get_kT, get_v, bias_for, None, PTs, tmps)
        ao, aok = aos.get(ti)
        aT, aTk = aTs.get(ti)
        xo, xok = xos.get(ti)
        _attn_finish(self, st, sk, 96, ao, aok)
        _out_proj(self, ao, aok, aT, aTk, wout, woutk, xt, xk, xo, xok, x_dst, dst_id, ti)

    load(0)
    for ti in range(ntile):
        if ti + 1 < ntile:
            load(ti + 1)
        tile_body(ti)
    S.barrier()
    S.recycle()
    self.sb_off = mark


Builder.phase_c = _phase_c


BIS = "dve"
NIT = 16
BIGM = 32768.0


def _phase_a(self, li, ia, x_src, src_id, x_dst, dst_id):
    S = self.S
    self.ng = 3
    _setup_bias(self)
    mark = self.sb_off
    NT = self.NT
    SEQ = self.SEQ
    topk = self.topk
    ntile = self.TOK // 128
    a_w_in = self.din("a_w_in", [2, D, 1736])
    a_w_out = self.din("a_w_out", [2, D, D])
    norm_mix = self.din("norm_mix", [4, D])
    biasD, biasDk = _load_bias_tile(self, "biasD", 0, 0)
    biasOA, biasOAk = _load_bias_tile(self, "biasOA", 0, 1)
    g, gk = self.load_bcast_row(None, "gmix", norm_mix[li, :], D)
    gq, gqk = self.load_bcast_row(None, "gq", self.din("a_q_gain", [2, 64])[ia, :], 64)
    gkt, gkk = self.load_bcast_row(None, "gk", self.din("a_k_gain", [2, 64])[ia, :], 64)
    causts = self.sb(None, "causts", [128, 128], F32)
    self.dma_in(causts[:], self.din("k_caus_ts", [128, 128])[:, :], "causts")
    ctab = self.sb(None, "ctab", [128, NIT], F32)
    cb = self.sb(None, "cb", [128, NT], F32)
    for k in range(NIT):
        self.op("dve", lambda e, k=k: e.memset(ctab[:, k:k + 1], -(2.0 ** -(k + 2))), w=("ctab",))
    for i_ in range(NT):
        self.op("dve", lambda e, i_=i_: e.memset(cb[:, i_:i_ + 1], float((i_ + 1) * 128 - 2 * topk) + 0.5), w=("cb",))
    win, wink = self.load_weight(None, "awin", a_w_in[ia], D, 1736)
    wout, woutk = self.load_weight(None, "awout", a_w_out[ia], D, D)
    KT = self.sb(None, "KT", [64, NT, 128], BF16)
    KIT = self.sb(None, "KIT", [64, NT * 128], BF16)
    VA = self.sb(None, "VA", [128, NT, 65], BF16)
    score = self.sb(None, "score", [128, SEQ], F32)
    Mts = self.rot(None, "Mt", [128, SEQ], BF16, 2)
    self.op("dve", lambda e: e.memset(VA[:, :, 64:65], 1.0), w=tuple(("VA", j) for j in range(NT)))
    xts = self.rot(None, "xt", [128, D], F32, 3)
    hs = self.rot(None, "h", [128, D], BF16, 1)
    hTs = self.rot(None, "hT", [128, 8, 128], BF16, 1)
    sts = self.rot(None, "st", [128, 160], F32, 2)
    prs = self.rot(None, "pr", [128, 1736], F32, 1)
    tmpn = self.rot(None, "tmpn", [128, D], F32, 1)
    qns = self.rot(None, "qn", [128, D], BF16, 1)
    kns = self.rot(None, "kn", [128, 64], BF16, 1)
    qkis = self.rot(None, "qki", [128, 576], BF16, 1)
    QTs = self.rot(None, "QT", [64, 16 * 128], BF16, 2)
    QITs = self.rot(None, "QIT", [64, 8 * 128], BF16, 1)
    rls = self.rot(None, "rl", [128, 512], F32, 3)
    MTs = self.rot(None, "MT", [128, 4, 128], BF16, 3)
    PTs = self.rot(None, "PT", [128, 16 * 128], BF16, 3)
    tmps = self.rot(None, "tmpb", [128, 512], F32, 2)
    aos = self.rot(None, "ao", [128, D], BF16, 1)
    aTs = self.rot(None, "aT", [128, 8, 128], BF16, 1)
    xos = self.rot(None, "xo", [128, D], F32, 1)
    junk = self.sb(None, "junk", [128, D], BF16)
    WS = 8.0 ** -0.5 / 8.0

    def load(ti):
        xt, xk = xts.get(ti)
        self.dma_in(xt[:], x_src[ti * 128:(ti + 1) * 128, :], xk, r=(("xd", src_id, ti),))

    def stage1(ti):
        i = ti % NT
        nk = (i + 1) * 128
        xt, xk, pr, prk, st, sk = _proj_front(self, ti, xts, hs, hTs, sts, prs, g, gk, win, wink, 1736, x_src, src_id, junk)
        tmp, tmpk = tmpn.get(ti)
        qn, qnk = qns.get(ti)
        kn, knk = kns.get(ti)
        qki, qkik = qkis.get(ti)
        QT, QTk = QTs.get(ti)
        QIT, QITk = QITs.get(ti)
        Mt, Mtk = Mts.get(ti)
        _head_norm(self, pr[:, 0:1024].rearrange("p (h d) -> p h d", d=64), prk, 16, 64, gq, gqk, st, sk, 8, tmp, tmpk,
                   qn[:, :].rearrange("p (h d) -> p h d", d=64), qnk)
        _head_norm(self, pr[:, 1024:1088].unsqueeze(1), prk, 1, 64, gkt, gkk, st, sk, 48, tmp, tmpk, kn[:, :].unsqueeze(1), knk)
        self.op("act", lambda e: e.copy(out=qki[:], in_=pr[:, 1152:1728]), r=(prk,), w=(qkik,))
        self.transpose_to(qn, qnk, [h * 64 for h in range(16)], 64, QT[:, :].rearrange("p (h t) -> p h t", t=128), QTk)
        self.transpose_to(kn, knk, [0], 64, KT, ("KT", i), dst_blk0=i)
        self.transpose_to(qki, qkik, [h * 64 for h in range(8)], 64, QIT[:, :].rearrange("p (h t) -> p h t", t=128), QITk)
        self.transpose_to(qki, qkik, [512], 64, KIT[:, :].rearrange("p (j t) -> p j t", t=128), ("KIT", i), dst_blk0=i)
        self.op("act", lambda e: e.copy(out=VA[:, i, 0:64], in_=pr[:, 1088:1152]), r=(prk,), w=(("VA", i),))
        if nk <= topk:
            return
        bk_ = sk + "_bis"
        wsc = st[:, 100:108]
        lo = st[:, 110:111]
        w0 = st[:, 111:112]
        hi = st[:, 112:113]
        acc = st[:, 113:114]
        sg = st[:, 114:115]
        negmid = st[:, 115:116]
        negW = st[:, 120:120 + NIT]
        self.op("dve", lambda e: e.tensor_scalar(out=wsc, in0=pr[:, 1728:1736], scalar1=WS, scalar2=None, op0=ALU.mult),
                r=(prk,), w=(bk_,))
        for c0 in range(0, nk, 512):
            wd = min(512, nk - c0)
            kkeys = tuple(("KIT", j) for j in range(c0 // 128, (c0 + wd) // 128))
            for h in range(8):
                b = self.gbank()
                bkb = "bank%d" % b
                ps = self.banks[b]
                self.op("pe", lambda e, h=h, ps=ps, c0=c0, wd=wd: e.matmul(
                    ps[:, 0:wd], lhsT=QIT[:, h * 128:(h + 1) * 128], rhs=KIT[:, c0:c0 + wd], start=True, stop=True),
                    r=(QITk,) + kkeys, w=(bkb,))
                rl, rlk = rls.next()
                self.op("act", lambda e, ps=ps, rl=rl, wd=wd: e.activation(out=rl[:, 0:wd], in_=ps[:, 0:wd], func=AF.Relu),
                        r=(bkb,), w=(rlk,))
                if h == 0:
                    self.op("dve", lambda e, rl=rl, c0=c0, wd=wd: e.tensor_scalar(
                        out=score[:, c0:c0 + wd], in0=rl[:, 0:wd], scalar1=wsc[:, 0:1], scalar2=None, op0=ALU.mult),
                        r=(rlk, bk_), w=("score",))
                else:
                    self.op("dve", lambda e, rl=rl, c0=c0, wd=wd, h=h: e.scalar_tensor_tensor(
                        out=score[:, c0:c0 + wd], in0=rl[:, 0:wd], scalar=wsc[:, h:h + 1], in1=score[:, c0:c0 + wd],
                        op0=ALU.mult, op1=ALU.add), r=(rlk, bk_, "score"), w=("score",))
        self.op("dve", lambda e: e.tensor_reduce(out=hi, in_=score[:, 0:nk], axis=AX.X, op=ALU.max), r=("score",), w=(bk_,))
        self.op("dve", lambda e: e.tensor_reduce(out=lo, in_=score[:, 0:nk], axis=AX.X, op=ALU.min), r=("score",), w=(bk_,))
        self.op("dve", lambda e: e.tensor_tensor(out=w0, in0=hi, in1=lo, op=ALU.subtract), r=(bk_,), w=(bk_,))
        self.op("dve", lambda e: e.tensor_tensor(out=score[:, nk - 128:nk], in0=score[:, nk - 128:nk], in1=causts[:],
                                                 op=ALU.add), r=("score", "causts"), w=("score",))
        self.op("dve", lambda e: e.tensor_scalar(out=negW, in0=ctab[:, 0:NIT], scalar1=w0, scalar2=None, op0=ALU.mult),
                r=(bk_, "ctab"), w=(bk_,))
        self.op("dve", lambda e: e.scalar_tensor_tensor(out=negmid, in0=w0, scalar=-0.5, in1=lo, op0=ALU.mult,
                                                        op1=ALU.subtract), r=(bk_,), w=(bk_,))
        if BIS == "act":
            for it in range(NIT):
                self.op("act", lambda e: e.activation(out=Mt[:, 0:nk], in_=score[:, 0:nk], func=AF.Sign, bias=negmid,
                                                      accum_out=acc), r=("score", bk_), w=(bk_, Mtk))
                self.op("act", lambda e: e.activation(out=sg, in_=acc, func=AF.Sign, bias=cb[:, i:i + 1]), r=(bk_, "cb"), w=(bk_,))
                self.op("act", lambda e, it=it: e.activation(out=negmid, in_=sg, func=AF.Identity, bias=negmid,
                                                             scale=negW[:, it:it + 1]), r=(bk_,), w=(bk_,))
            self.op("dve", lambda e: e.tensor_tensor(out=lo, in0=negW[:, NIT - 1:NIT], in1=negmid, op=ALU.subtract), r=(bk_,), w=(bk_,))

        else:
            self.op("dve", lambda e: e.tensor_scalar(out=negmid, in0=negmid, scalar1=-1.0, scalar2=None, op0=ALU.mult),
                    r=(bk_,), w=(bk_,))
            self.op("dve", lambda e: e.tensor_scalar(out=negW, in0=negW, scalar1=-2.0, scalar2=None, op0=ALU.mult),
                    r=(bk_,), w=(bk_,))
            for it in range(NIT):
                self.op("dve", lambda e: e.tensor_scalar(out=Mt[:, 0:nk], in0=score[:, 0:nk], scalar1=negmid, scalar2=0.0,
                                                         op0=ALU.is_ge, op1=ALU.add, accum_out=acc),
                        r=("score", bk_), w=(bk_, Mtk))
                self.op("dve", lambda e: e.tensor_scalar(out=sg, in0=acc, scalar1=float(topk) - 0.5, scalar2=0.5,
                                                         op0=ALU.is_ge, op1=ALU.subtract), r=(bk_,), w=(bk_,))
                self.op("dve", lambda e, it=it: e.scalar_tensor_tensor(out=negmid, in0=sg, scalar=negW[:, it:it + 1], in1=negmid,
                                                                       op0=ALU.mult, op1=ALU.add), r=(bk_,), w=(bk_,))
            self.op("dve", lambda e: e.scalar_tensor_tensor(out=lo, in0=negW[:, NIT - 1:NIT], scalar=-0.5, in1=negmid,
                                                            op0=ALU.mult, op1=ALU.add), r=(bk_,), w=(bk_,))
        self.op("dve", lambda e: e.tensor_scalar(out=Mt[:, 0:nk], in0=score[:, 0:nk], scalar1=lo, scalar2=None,
                                                 op0=ALU.is_ge), r=("score", bk_), w=(Mtk,))

    def stage2(ti):
        i = ti % NT
        nk = (i + 1) * 128
        select = nk > topk
        xt, xk = xts.get(ti)
        st, sk = sts.get(ti)
        QT, QTk = QTs.get(ti)
        Mt, Mtk = Mts.get(ti)

        def get_kT(j):
            return [(KT[:, j, :], ("KT", j), 0, 16)]

        def get_v(j):
            return VA[:, j:j + 1, :], ("VA", j), (lambda h: 0)

        def bias_for(j, c):
            if j == i:
                return biasD[:, 4 * c:4 * c + 4, :], biasDk
            if j == i - 1:
                return biasOA[:, 4 * c:4 * c + 4, :], biasOAk
            return None

        def mask_for(j):
            MT, MTk = MTs.next()
            b = self.gbank()
            bkb = "bank%d" % b
            pb = self.banks[b].bitcast(BF16)
            self.op("pe", lambda e: e.transpose(out=pb[:, 0:128], in_=Mt[:, j * 128:(j + 1) * 128], identity=self.ident[:]),
                    r=(Mtk, "ident"), w=(bkb,))
            self.op("dve", lambda e: e.tensor_scalar(out=MT[:, :, :], in0=pb[:, 0:128].unsqueeze(1).to_broadcast([128, 4, 128]),
                                                     scalar1=-1.0, scalar2=BIGM, op0=ALU.add, op1=ALU.mult),
                    r=(bkb,), w=(MTk,))
            return MT[:, :, :].rearrange("p a b -> p (a b)"), MTk

        _attend(self, QT, QTk, 64, 0.125, list(range(i + 1)), get_kT, get_v, bias_for, mask_for if select else None, PTs, tmps)
        ao, aok = aos.get(ti)
        aT, aTk = aTs.get(ti)
        xo, xok = xos.get(ti)
        _attn_finish(self, st, sk, 64, ao, aok)
        _out_proj(self, ao, aok, aT, aTk, wout, woutk, xt, xk, xo, xok, x_dst, dst_id, ti)

    load(0)
    if ntile > 1:
        load(1)
    stage1(0)
    for ti in range(ntile):
        if ti + 2 < ntile:
            load(ti + 2)
        nxt = ti + 1 < ntile
        if nxt and (ti + 1) % NT != 0:
            stage1(ti + 1)
            stage2(ti)
        else:
            stage2(ti)
            if nxt:
                stage1(ti + 1)
    S.barrier()
    S.recycle()
    self.sb_off = mark


Builder.phase_a = _phase_a


FULL_PHASES = [("a", 0), ("mlp", 0), ("b", 1), ("mlp", 1), ("c", 2), ("mlp", 2), ("a", 3), ("mlp", 3)]
LAUNCH_PLAN = [FULL_PHASES]


def _run_group(phases, xs, pos, inputs, SEQ, NSEQ, topk):
    B = build_program(phases, SEQ, NSEQ, topk)
    consts = const_inputs(B)
    in_maps = []
    for c in range(len(xs)):
        m = {}
        for name in B.dram_in:
            if name == "x":
                m[name] = xs[c]
            elif name == "positions":
                m[name] = pos[c]
            elif name in consts:
                m[name] = consts[name]
            else:
                m[name] = inputs[name]
        in_maps.append(m)
    res = run_bass_kernel_spmd(B.nc, in_maps, core_ids=list(range(len(xs))))
    return [np.asarray(r["out"]) for r in res.results]


def kernel(**inputs):
    inputs = {k: np.ascontiguousarray(np.asarray(v)) for k, v in inputs.items()}
    x = inputs["x"]
    Bsz, SEQ, _ = x.shape
    NSEQ = Bsz // NCORES
    topk = min(256, SEQ // 4)
    xs = [np.ascontiguousarray(x[c * NSEQ:(c + 1) * NSEQ].reshape(NSEQ * SEQ, D)) for c in range(NCORES)]
    pos = [np.ascontiguousarray(inputs["positions"][c * NSEQ:(c + 1) * NSEQ].reshape(NSEQ * SEQ, 1).astype(np.int32))
           for c in range(NCORES)]
    for group in LAUNCH_PLAN:
        xs = _run_group(group, xs, pos, inputs, SEQ, NSEQ, topk)
    out = np.stack([o.reshape(NSEQ, SEQ, D) for o in xs], axis=0).reshape(Bsz, SEQ, D)
    return out.astype(np.float32, copy=False)
```

```python
import math
from contextlib import ExitStack

import numpy as np
import concourse.bass as bass
import concourse.mybir as mybir
from concourse.bass_utils import run_bass_kernel_spmd

F32 = mybir.dt.float32
BF16 = mybir.dt.bfloat16
I32 = mybir.dt.int32
AF = mybir.ActivationFunctionType
ALU = mybir.AluOpType
AX = mybir.AxisListType

D = 1024
DFF = 4096
EPS = 1e-6
NEG = -30000.0
NCORES = 8


class Op:
    __slots__ = ("idx", "eng", "eidx", "fn", "cdeps", "dwaits", "is_dma", "dkey", "dh", "sig", "sigidx")


class Sched:
    ENGS = ("pe", "act", "dve", "pool", "sp")

    def __init__(self, nc):
        self.nc = nc
        self.ops = []
        self.eops = {e: [] for e in self.ENGS}
        self.kw = {}
        self.kr = {}
        self.dsem = {}
        self.excl = set()
        self.pending = {}
        self.free_dsems = {"sp": [], "pool": [], "act": []}

    def add(self, eng, fn, r=(), w=(), dma=None):
        op = Op()
        op.idx = len(self.ops)
        op.eng = eng
        op.eidx = len(self.eops[eng])
        op.fn = fn
        op.is_dma = dma is not None
        op.dkey = dma
        op.cdeps = {}
        op.dwaits = {}
        op.sig = False
        op.sigidx = 0

        def dep(p, kind):
            if p is None or p is op:
                return
            if p.is_dma:
                if op.is_dma and p.dkey == dma and kind == "waw":
                    return
                ent = self.dsem.get(p.dkey)
                if ent is None or ent[0] is not p.dh:
                    return
                op.dwaits[id(ent[0])] = (ent[0], ent[1])
                return
            if p.eng == eng and not op.is_dma:
                if eng == "pe":
                    return
            cur = op.cdeps.get(p.eng)
            if cur is None or p.eidx > cur.eidx:
                op.cdeps[p.eng] = p

        for k in r:
            dep(self.kw.get(k), "raw")
            rd = self.kr.setdefault(k, {})
            if k in self.excl:
                for q in rd.values():
                    if q.eng != eng:
                        dep(q, "war")
            rd[("d", dma) if op.is_dma else ("c", eng)] = op
        for k in w:
            dep(self.kw.get(k), "waw")
            for q in self.kr.get(k, {}).values():
                dep(q, "war")
            self.kw[k] = op
            self.kr[k] = {}
        pb = self.pending.pop(eng, None)
        if pb is not None:
            for p in pb[0]:
                if p.eng != eng:
                    dep(p, "raw")
            for h, c in pb[1]:
                old = op.dwaits.get(id(h))
                if old is None or old[1] < c:
                    op.dwaits[id(h)] = (h, c)
        if op.is_dma:
            if dma not in self.dsem:
                if self.free_dsems[eng]:
                    self.dsem[dma] = self.free_dsems[eng].pop()
                else:
                    self.nsem = getattr(self, "nsem", 0) + 1
                    self.dsem[dma] = [self.nc.alloc_semaphore("d_%d" % self.nsem), 0, eng]
            self.dsem[dma][1] += 16
            op.dh = self.dsem[dma][0]
        self.ops.append(op)
        self.eops[eng].append(op)
        return op

    def barrier(self):
        last = [l[-1] for l in self.eops.values() if l and not l[-1].is_dma]
        for l in self.eops.values():
            for o in reversed(l):
                if not o.is_dma:
                    if o not in last:
                        last.append(o)
                    break
        dw = [(v[0], v[1]) for v in self.dsem.values()]
        for e in self.ENGS:
            old = self.pending.get(e)
            if old is None:
                self.pending[e] = (list(last), list(dw))
            else:
                old[0].extend(last)
                old[1].extend(dw)

    def recycle(self, keep=()):
        for k in list(self.dsem.keys()):
            if k in keep:
                continue
            ent = self.dsem.pop(k)
            self.free_dsems[ent[2]].append(ent)

    def emit(self):
        nc = self.nc
        for op in self.ops:
            for p in op.cdeps.values():
                p.sig = True
        esem = {}
        for e in self.ENGS:
            n = 0
            for op in self.eops[e]:
                if op.sig:
                    n += 1
                    op.sigidx = n
            if n:
                esem[e] = nc.alloc_semaphore("e_" + e)
        sched = self

        def run(e, eng):
            waited = {}
            for op in sched.eops[e]:
                for pe_, p in op.cdeps.items():
                    if waited.get(pe_, 0) < p.sigidx:
                        eng.wait_ge(esem[pe_], p.sigidx)
                        waited[pe_] = p.sigidx
                for hid, (h, c) in op.dwaits.items():
                    if waited.get(hid, 0) < c:
                        eng.wait_ge(h, c)
                        waited[hid] = c
                ins = op.fn(eng)
                if op.is_dma:
                    ins.then_inc(op.dh, 16)
                elif op.sig:
                    ins.then_inc(esem[e], 1)

        with nc.Block() as block:
            @block.tensor
            def _(eng):
                run("pe", eng)

            @block.scalar
            def _(eng):
                run("act", eng)

            @block.vector
            def _(eng):
                run("dve", eng)

            @block.gpsimd
            def _(eng):
                run("pool", eng)

            @block.sync
            def _(eng):
                run("sp", eng)


def _t5_bucket(n):
    n = np.maximum(n, 0)
    nf = np.maximum(n, 1).astype(np.float32)
    large = 16 + (np.log(nf / np.float32(16)) / np.float32(math.log(128 / 16)) * np.float32(16)).astype(np.int32)
    large = np.minimum(large, 31)
    return np.where(n < 16, n, large)


TW = 384


def _host_consts():
    c = {}
    c["ident"] = np.eye(128, dtype=np.float32)
    rel = np.arange(TW) - 127
    b = _t5_bucket(rel)
    oh = np.zeros((2, 33, TW), np.float32)
    for v in range(2):
        for r in range(TW):
            if rel[r] >= 0:
                oh[v, b[r], r] += 1.0
                oh[v, 31, r] -= 1.0
            masked = rel[r] < 0 or (v == 1 and rel[r] >= 128)
            oh[v, 32, r] = NEG if masked else 0.0
    c["t5oh"] = oh
    t = np.arange(128)
    c["caus_ts"] = np.where(t[None, :] <= t[:, None], 0.0, -1e30).astype(np.float32)
    c["caus_st"] = np.where(t[:, None] <= t[None, :], 0.0, NEG).astype(np.float32)
    inv_freq = (10000.0 ** (-np.arange(0, 32, 2, dtype=np.float32) / np.float32(32))).astype(np.float32)
    c["invf"] = np.broadcast_to(inv_freq[None, :], (128, 16)).copy()
    return c


class Builder:
    def __init__(self, phases, SEQ, NSEQ, topk):
        self.phases = phases
        self.SEQ = SEQ
        self.NSEQ = NSEQ
        self.NT = SEQ // 128
        self.TOK = SEQ * NSEQ
        self.topk = topk
        self.nc = bass.Bass("TRN2", target_bir_lowering=False)
        self.S = Sched(self.nc)
        self.uid = 0
        self.dram_in = {}
        self.sb_off = 16512
        self.sb_peak = 0
        self.sb_cap = 229312

    def din(self, name, shape, dt=F32):
        if name not in self.dram_in:
            self.dram_in[name] = self.nc.dram_tensor(name, list(shape), dt, kind="ExternalInput").ap()
        return self.dram_in[name]

    def sb(self, es, name, shape, dt):
        self.uid += 1
        esz = 2 if dt == BF16 else 4
        n = 1
        for d in shape[1:]:
            n *= d
        nbytes = (n * esz + 63) // 64 * 64
        off = self.sb_off
        self.sb_off += nbytes
        assert self.sb_off <= self.sb_cap, "SBUF overflow: %d > %d (%s)" % (self.sb_off, self.sb_cap, name)
        self.sb_peak = max(self.sb_peak, self.sb_off)
        return self.nc.alloc_sbuf_tensor_at("%s_%d" % (name, self.uid), list(shape), dt, offset=off)

    def op(self, eng, fn, r=(), w=(), dma=None):
        return self.S.add(eng, fn, r, w, dma)

    def dma_in(self, dst_ap, src_ap, tkey, r=(), w=(), eng="sp"):
        return self.op(eng, lambda e: e.dma_start(out=dst_ap, in_=src_ap), r=r, w=tuple(w) + (tkey,), dma=tkey)

    def dma_out(self, dst_ap, src_ap, tkey, r=(), w=(), eng="sp"):
        return self.op(eng, lambda e: e.dma_start(out=dst_ap, in_=src_ap), r=tuple(r) + (tkey,), w=w, dma=tkey)

    def setup_psum(self, es):
        self.banks = []
        for i in range(8):
            t = self.nc.alloc_psum_tensor("bank%d" % i, [128, 512], F32)
            self.banks.append(t)
            self.S.excl.add("bank%d" % i)
        self.grot = 0
        self.ng = 3

    def gbank(self):
        i = self.grot % self.ng
        self.grot += 1
        return i

    def setup_consts(self, es):
        hc = _host_consts()
        self.host_consts = hc
        nc = self.nc
        ident_f = self.sb(es, "identf", [128, 128], F32)
        self.ident = self.sb(es, "ident", [128, 128], BF16)
        d = self.din("k_ident", [128, 128])
        self.dma_in(ident_f[:], d[:, :], "identf")
        self.op("dve", lambda e: e.tensor_copy(out=self.ident[:], in_=ident_f[:]), r=("identf",), w=("ident",))
        self.eps_t = self.sb(es, "eps", [128, 1], F32)
        self.op("dve", lambda e: e.memset(self.eps_t[:], EPS), w=("eps",))

    def transpose_to(self, src_tile, src_key, col_offs, width, dst3, dst_key, dst_blk0=0, evac_eng="act"):
        n = len(col_offs)
        i0 = 0
        while i0 < n:
            cnt = min(8, n - i0)
            b = self.gbank()
            bk = "bank%d" % b
            pb = self.banks[b].bitcast(BF16)
            for j in range(cnt):
                off = col_offs[i0 + j]
                self.op("pe", lambda e, j=j, off=off, pb=pb: e.transpose(
                    out=pb[0:width, j * 128:(j + 1) * 128], in_=src_tile[:, off:off + width], identity=self.ident[:]),
                    r=(src_key, "ident"), w=(bk,))
            dst = dst3[0:width, dst_blk0 + i0:dst_blk0 + i0 + cnt, :]
            src = pb[0:width, 0:cnt * 128].rearrange("p (a b) -> p a b", b=128)
            if evac_eng == "act":
                self.op("act", lambda e, dst=dst, src=src: e.copy(out=dst, in_=src), r=(bk,), w=(dst_key,))
            else:
                self.op("dve", lambda e, dst=dst, src=src: e.tensor_copy(out=dst, in_=src), r=(bk,), w=(dst_key,))
            i0 += cnt

    def transpose_full(self, src_tile, src_key, nblk, dstT, dst_key, evac_eng="act"):
        i0 = 0
        while i0 < nblk:
            cnt = min(8, nblk - i0)
            b = self.gbank()
            bk = "bank%d" % b
            pb = self.banks[b].bitcast(BF16)
            for j in range(cnt):
                off = (i0 + j) * 128
                self.op("pe", lambda e, j=j, off=off, pb=pb: e.transpose(
                    out=pb[:, j * 128:(j + 1) * 128], in_=src_tile[:, off:off + 128], identity=self.ident[:]),
                    r=(src_key, "ident"), w=(bk,))
            dst = dstT[:, i0:i0 + cnt, :]
            src = pb[:, 0:cnt * 128].rearrange("p (a b) -> p a b", b=128)
            if evac_eng == "act":
                self.op("act", lambda e, dst=dst, src=src: e.copy(out=dst, in_=src), r=(bk,), w=(dst_key,))
            else:
                self.op("dve", lambda e, dst=dst, src=src: e.tensor_copy(out=dst, in_=src), r=(bk,), w=(dst_key,))
            i0 += cnt

    def linear(self, xT, xT_key, nk, W, W_key, N, evac):
        n0 = 0
        while n0 < N:
            wd = min(512, N - n0)
            b = self.gbank()
            bk = "bank%d" % b
            ps = self.banks[b]
            for k in range(nk):
                self.op("pe", lambda e, k=k, n0=n0, wd=wd, ps=ps: e.matmul(
                    ps[:, 0:wd], lhsT=xT[:, k, :], rhs=W[:, k, n0:n0 + wd], start=(k == 0), stop=(k == nk - 1)),
                    r=(xT_key, W_key), w=(bk,))
            evac(b, bk, n0, wd)
            n0 += wd

    def load_weight(self, es, name, dram_ap2d, K, N):
        nk = max(1, K // 128)
        kp = min(K, 128)
        t = self.sb(es, name, [kp, nk, N], BF16)
        key = name + "_%d" % self.uid
        src = dram_ap2d.rearrange("(k p) n -> p k n", p=kp)
        step = max(1, min(nk, 8192 // N if N <= 8192 else 1))
        k0 = 0
        while k0 < nk:
            k1 = min(nk, k0 + step)
            self.dma_in(t[:, k0:k1, :], src[:, k0:k1, :], key, eng="pool")
            k0 = k1
        return t, key

    def load_bcast_row(self, es, name, dram_row_ap, n):
        t = self.sb(es, name, [128, n], F32)
        key = name + "_%d" % self.uid
        src = bass.AP(tensor=dram_row_ap.tensor, offset=dram_row_ap.offset, ap=[[0, 128], [1, n]])
        self.dma_in(t[:], src, key)
        return t, key

    def rms_scale(self, es_tiles, ssq_ap, ssq_key, out_ap, out_key, n, shape_cols):
        self.op("act", lambda e: e.activation(out=out_ap, in_=ssq_ap, func=AF.Ln, bias=self.eps_t[:, 0:1], scale=1.0 / n),
                r=(ssq_key, "eps"), w=(out_key,))
        self.op("act", lambda e: e.activation(out=out_ap, in_=out_ap, func=AF.Exp, scale=-0.5),
                r=(out_key,), w=(out_key,))

    def rot(self, es, name, shape, dt, n=2):
        tiles = []
        for i in range(n):
            t = self.sb(es, name + str(i), shape, dt)
            tiles.append((t, "%s%d_%d" % (name, i, self.uid)))
        return Rot(tiles)

    def tile_norm(self, xt, xk, st, sk, col, g, gk, h, hk, junk):
        ssq = st[:, col:col + 1]
        rstd = st[:, col + 1:col + 2]
        self.op("act", lambda e: e.activation(out=junk[:], in_=xt[:], func=AF.Square, accum_out=ssq), r=(xk, sk), w=(sk,))
        self.rms_scale(None, ssq, sk, rstd, sk, D, 1)
        self.op("dve", lambda e: e.scalar_tensor_tensor(out=h[:], in0=xt[:], scalar=rstd, in1=g[:], op0=ALU.mult,
                                                        op1=ALU.mult), r=(xk, sk, gk), w=(hk,))

    def phase_mlp(self, li, x_src, src_id, x_dst, dst_id):
        S = self.S
        ntile = self.TOK // 128
        self.ng = 8
        w_up = self.din("w_up", [4, D, DFF])
        w_down = self.din("w_down", [4, DFF, D])
        norm_mlp = self.din("norm_mlp", [4, D])
        mark = self.sb_off
        with ExitStack() as es:
            g, gk = self.load_bcast_row(es, "gml", norm_mlp[li, :], D)
            wup, wupk = self.load_weight(es, "wup", w_up[li], D, DFF)
            wdn, wdnk = self.load_weight(es, "wdn", w_down[li], DFF, D)
            xts = self.rot(es, "xt", [128, D], F32, 3)
            hs = self.rot(es, "h", [128, D], BF16, 2)
            hTs = self.rot(es, "hT", [128, 8, 128], BF16, 2)
            sts = self.rot(es, "st", [128, 4], F32, 2)
            rls = self.rot(es, "rl", [128, 512], F32, 3)
            hids = self.rot(es, "hid", [128, DFF], BF16, 2)
            hidTs = self.rot(es, "hidT", [128, 32, 128], BF16, 2)
            xos = self.rot(es, "xo", [128, D], F32, 2)
            junk = self.sb(es, "junk", [128, D], BF16)

            def load(ti):
                xt, xk = xts.get(ti)
                self.dma_in(xt[:], x_src[ti * 128:(ti + 1) * 128, :], xk, r=(("xd", src_id, ti),))

            def front(ti):
                xt, xk = xts.get(ti)
                h, hk = hs.get(ti)
                hT, hTk = hTs.get(ti)
                st, sk = sts.get(ti)
                self.tile_norm(xt, xk, st, sk, 0, g, gk, h, hk, junk)
                self.transpose_full(h, hk, 8, hT, hTk)

            def body(ti):
                xt, xk = xts.get(ti)
                hT, hTk = hTs.get(ti)
                hid, hidk = hids.get(ti)
                hidT, hidTk = hidTs.get(ti)
                xo, xok = xos.get(ti)

                def evac_up(b, bk, n0, wd):
                    rl, rlk = rls.next()
                    ps = self.banks[b]
                    self.op("act", lambda e: e.activation(out=rl[:, 0:wd], in_=ps[:, 0:wd], func=AF.Relu), r=(bk,), w=(rlk,))
                    self.op("dve", lambda e: e.tensor_tensor(out=hid[:, n0:n0 + wd], in0=rl[:, 0:wd], in1=rl[:, 0:wd],
                                                             op=ALU.mult), r=(rlk,), w=(hidk,))

                self.linear(hT, hTk, 8, wup, wupk, DFF, evac_up)
                self.transpose_full(hid, hidk, 32, hidT, hidTk)

                def evac_dn(b, bk, n0, wd):
                    ps = self.banks[b]
                    self.op("dve", lambda e: e.tensor_tensor(out=xo[:, n0:n0 + wd], in0=ps[:, 0:wd], in1=xt[:, n0:n0 + wd],
                                                             op=ALU.add), r=(bk, xk), w=(xok,))

                self.linear(hidT, hidTk, 32, wdn, wdnk, D, evac_dn)
                self.dma_out(x_dst[ti * 128:(ti + 1) * 128, :], xo[:], xok, w=(("xd", dst_id, ti),))

            load(0)
            if ntile > 1:
                load(1)
            front(0)
            for ti in range(ntile):
                if ti + 2 < ntile:
                    load(ti + 2)
                if ti + 1 < ntile:
                    front(ti + 1)
                body(ti)
        S.barrier()
        S.recycle()
        self.sb_off = mark


class Rot:
    def __init__(self, tiles):
        self.tiles = tiles
        self.i = 0

    def get(self, i):
        return self.tiles[i % len(self.tiles)]

    def next(self):
        t = self.tiles[self.i % len(self.tiles)]
        self.i += 1
        return t


def build_program(phases, SEQ, NSEQ, topk):
    B = Builder(phases, SEQ, NSEQ, topk)
    nc = B.nc
    TOK = B.TOK
    B.setup_psum(None)
    B.setup_consts(None)
    x_in = B.din("x", [TOK, D])
    out = nc.dram_tensor("out", [TOK, D], F32, kind="ExternalOutput").ap()
    scr = [nc.dram_tensor("xs%d" % i, [TOK, D], F32, kind="Internal").ap() for i in range(2)] if len(phases) > 1 else []
    cur, cur_id = x_in, "in"
    for pi, (kind, li) in enumerate(phases):
        last = pi == len(phases) - 1
        dst, dst_id = (out, "out") if last else (scr[pi % 2], "s%d_%d" % (pi % 2, pi))
        if kind == "mlp":
            B.phase_mlp(li, cur, cur_id, dst, dst_id)
        elif kind == "a":
            B.phase_a(li, li // 3, cur, cur_id, dst, dst_id)
        elif kind == "b":
            B.phase_b(li, cur, cur_id, dst, dst_id)
        elif kind == "c":
            B.phase_c(li, cur, cur_id, dst, dst_id)
        cur, cur_id = dst, dst_id
    B.S.barrier()
    B.op("sp", lambda e: e.nop())
    B.S.emit()
    return B


def const_inputs(B):
    hc = B.host_consts
    m = {}
    for name in B.dram_in:
        if name.startswith("k_"):
            m[name] = np.ascontiguousarray(hc[name[2:]])
    return m


def _setup_bias(self):
    if getattr(self, "bias_ready", False):
        return
    self.bias_ready = True
    nc = self.nc
    rel_bias = self.din("rel_bias", [32, 16])
    oh_d = self.din("k_t5oh", [2, 33, TW])
    rb = self.sb(None, "rbaug", [33, 16], F32)
    self.op("dve", lambda e: e.memset(rb[32:33, :], 1.0), w=("rbaug",))
    self.dma_in(rb[0:32, :], rel_bias[:, :], "rbaug")
    oh = self.sb(None, "t5oh", [33, 2, TW], F32)
    self.dma_in(oh[:], oh_d.rearrange("v b r -> b v r"), "t5oh")
    fsb = self.sb(None, "fsb", [16, 2, TW], F32)
    fd = nc.dram_tensor("fd_scr", [2, 16, TW], F32, kind="Internal").ap()
    for v in range(2):
        b = self.gbank()
        bk = "bank%d" % b
        ps = self.banks[b]
        self.op("pe", lambda e, v=v, ps=ps: e.matmul(ps[0:16, 0:TW], lhsT=rb[:, :], rhs=oh[:, v, :], start=True, stop=True),
                r=("rbaug", "t5oh"), w=(bk,))
        self.op("dve", lambda e, v=v, ps=ps: e.tensor_copy(out=fsb[:, v, :], in_=ps[0:16, 0:TW]), r=(bk,), w=("fsb",))
    self.dma_out(fd.rearrange("v h r -> h v r"), fsb[:], "fsb", w=("fd",))
    self.fd = fd


def _load_bias_tile(self, name, v, delta):
    fd = self.fd
    tile = self.sb(None, name, [128, 16, 128], F32)
    key = "%s_%d" % (name, self.uid)
    for s_ in range(128):
        src = bass.AP(tensor=fd.tensor, offset=v * 16 * TW + 127 - s_ + 128 * delta, ap=[[0, 1], [TW, 16], [1, 128]])
        self.dma_in(tile[s_:s_ + 1, :, :], src, key, r=("fd",))
    return tile, key


def _proj_front(self, ti, xts, hs, hTs, sts, prs, g, gk, win, wink, nin, x_src, src_id, junk):
    xt, xk = xts.get(ti)
    h, hk = hs.get(ti)
    hT, hTk = hTs.get(ti)
    st, sk = sts.get(ti)
    pr, prk = prs.get(ti)
    self.tile_norm(xt, xk, st, sk, 0, g, gk, h, hk, junk)
    self.transpose_full(h, hk, 8, hT, hTk)

    def evac(b, bk, n0, wd):
        ps = self.banks[b]
        self.op("act", lambda e: e.copy(out=pr[:, n0:n0 + wd], in_=ps[:, 0:wd]), r=(bk,), w=(prk,))

    self.linear(hT, hTk, 8, win, wink, nin, evac)
    return xt, xk, pr, prk, st, sk


def _head_norm(self, src3, srck, H, dh, gbc, gbck, st, sk, col, tmp, tmpk, out3, outk):
    t3 = tmp[:, 0:H * dh].rearrange("p (h d) -> p h d", d=dh)
    ssq = st[:, col:col + H]
    rstd = st[:, col + H:col + 2 * H]
    self.op("dve", lambda e: e.tensor_tensor(out=t3, in0=src3, in1=src3, op=ALU.mult), r=(srck,), w=(tmpk,))
    self.op("dve", lambda e: e.tensor_reduce(out=ssq, in_=t3, axis=AX.X, op=ALU.add), r=(tmpk,), w=(sk,))
    self.rms_scale(None, ssq, sk, rstd, sk, dh, H)
    self.op("dve", lambda e: e.tensor_tensor(out=t3, in0=src3, in1=rstd.unsqueeze(2).to_broadcast([128, H, dh]),
                                             op=ALU.mult), r=(srck, sk), w=(tmpk,))
    self.op("dve", lambda e: e.tensor_tensor(out=out3, in0=t3, in1=gbc[:, 0:dh].unsqueeze(1).to_broadcast([128, H, dh]),
                                             op=ALU.mult), r=(tmpk, gbck), w=(outk,))


PVB = 5


def _pv_slot(h):
    return PVB + h // 7, (h % 7) * 65


def _attend(self, QT, QTk, dk, scale, ktiles, get_kT, get_v, bias_for, mask_for, PTs, tmps):
    started = set()
    nkt = len(ktiles)
    state = {}

    def stage1(jj):
        j = ktiles[jj]
        groups = get_kT(j)
        PT, PTk = PTs.next()
        state[jj] = (PT, PTk)
        m = mask_for(j) if mask_for is not None else None
        for c in range(4):
            b = 3 + (c % 2)
            bk = "bank%d" % b
            ps = self.banks[b]
            for (kT, kk, h0, nh) in groups:
                lo, hi = max(h0, 4 * c), min(h0 + nh, 4 * c + 4)
                if lo >= hi:
                    continue
                self.op("pe", lambda e, kT=kT, lo=lo, hi=hi, ps=ps, c=c: e.matmul(
                    ps[:, (lo - 4 * c) * 128:(hi - 4 * c) * 128], lhsT=kT, rhs=QT[0:dk, lo * 128:hi * 128],
                    start=True, stop=(m is None), skip_group_check=True), r=(kk, QTk), w=(bk,))
            if m is not None:
                map_, mkey = m
                self.op("pe", lambda e, ps=ps, map_=map_: e.matmul(
                    ps[:, 0:512], lhsT=self.ident[:, :], rhs=map_, start=False, stop=True, skip_group_check=True),
                    r=(mkey, "ident"), w=(bk,))
            bias = bias_for(j, c)
            pt_c = PT[:, c * 512:(c + 1) * 512]
            if bias is None:
                self.op("act", lambda e, ps=ps, pt_c=pt_c: e.activation(out=pt_c, in_=ps[:, :], func=AF.Exp, scale=scale),
                        r=(bk,), w=(PTk,))
            else:
                bap, bkey = bias
                tmp, tmpk = tmps.next()
                self.op("dve", lambda e, ps=ps, tmp=tmp, bap=bap: e.scalar_tensor_tensor(
                    out=tmp[:, 0:512].rearrange("p (a b) -> p a b", b=128), in0=ps[:, :].rearrange("p (a b) -> p a b", b=128),
                    scalar=scale, in1=bap, op0=ALU.mult, op1=ALU.add), r=(bk, bkey), w=(tmpk,))
                self.op("act", lambda e, tmp=tmp, pt_c=pt_c: e.activation(out=pt_c, in_=tmp[:, 0:512], func=AF.Exp),
                        r=(tmpk,), w=(PTk,))

    def stage2(jj):
        j = ktiles[jj]
        PT, PTk = state.pop(jj)
        v3, vk, vh = get_v(j)
        for h in range(16):
            b, off = _pv_slot(h)
            bk = "bank%d" % b
            st_flag = b not in started
            started.add(b)
            self.op("pe", lambda e, h=h, b=b, off=off, PT=PT, v3=v3, st_flag=st_flag, last=(jj == nkt - 1): e.matmul(
                self.banks[b][:, off:off + 65], lhsT=PT[:, h * 128:(h + 1) * 128], rhs=v3[:, vh(h), :],
                start=st_flag, stop=last, skip_group_check=True), r=(PTk, vk), w=(bk,))

    stage1(0)
    for jj in range(nkt):
        if jj + 1 < nkt:
            stage1(jj + 1)
        stage2(jj)


def _attn_finish(self, st, sk, col, ao, aok, sinkexp=None):
    den = st[:, col:col + 16]
    rden = st[:, col + 16:col + 32]
    for b in range(PVB, PVB + 3):
        h0 = (b - PVB) * 7
        nh = min(7, 16 - h0)
        bk = "bank%d" % b
        o3 = self.banks[b][:, 0:nh * 65].rearrange("p (h d) -> p h d", d=65)
        self.op("dve", lambda e, o3=o3, h0=h0, nh=nh: e.tensor_copy(out=den[:, h0:h0 + nh].unsqueeze(2), in_=o3[:, :, 64:65]),
                r=(bk,), w=(sk,))
    if sinkexp is not None:
        se, sek = sinkexp
        self.op("dve", lambda e: e.tensor_tensor(out=den, in0=den, in1=se[:, 0:16], op=ALU.add), r=(sk, sek), w=(sk,))
    self.op("dve", lambda e: e.reciprocal(out=rden, in_=den), r=(sk,), w=(sk,))
    ao3 = ao[:, :].rearrange("p (h d) -> p h d", d=64)
    for b in range(PVB, PVB + 3):
        h0 = (b - PVB) * 7
        nh = min(7, 16 - h0)
        bk = "bank%d" % b
        o3 = self.banks[b][:, 0:nh * 65].rearrange("p (h d) -> p h d", d=65)
        self.op("dve", lambda e, o3=o3, h0=h0, nh=nh: e.tensor_tensor(
            out=ao3[:, h0:h0 + nh, :], in0=o3[:, :, 0:64], in1=rden[:, h0:h0 + nh].unsqueeze(2).to_broadcast([128, nh, 64]),
            op=ALU.mult), r=(bk, sk), w=(aok,))


def _out_proj(self, ao, aok, aT, aTk, wout, woutk, xt, xk, xo, xok, x_dst, dst_id, ti):
    self.transpose_full(ao, aok, 8, aT, aTk)

    def evac(b, bk, n0, wd):
        ps = self.banks[b]
        self.op("dve", lambda e: e.tensor_tensor(out=xo[:, n0:n0 + wd], in0=ps[:, 0:wd], in1=xt[:, n0:n0 + wd], op=ALU.add),
                r=(bk, xk), w=(xok,))

    self.linear(aT, aTk, 8, wout, woutk, D, evac)
    self.dma_out(x_dst[ti * 128:(ti + 1) * 128, :], xo[:], xok, w=(("xd", dst_id, ti),))


def _phase_b(self, li, x_src, src_id, x_dst, dst_id):
    S = self.S
    self.ng = 3
    _setup_bias(self)
    mark = self.sb_off
    NT = self.NT
    ib = 0
    biasD, biasDk = _load_bias_tile(self, "biasD", 0, 0)
    biasOB, biasOBk = _load_bias_tile(self, "biasOB", 1, 1)
    b_w_in = self.din("b_w_in", [1, D, 1536])
    b_w_out = self.din("b_w_out", [1, D, D])
    norm_mix = self.din("norm_mix", [4, D])
    g, gk = self.load_bcast_row(None, "gmix", norm_mix[li, :], D)
    gq, gqk = self.load_bcast_row(None, "gq", self.din("b_q_gain", [1, 64])[ib, :], 64)
    gkk_t, gkk = self.load_bcast_row(None, "gk", self.din("b_k_gain", [1, 64])[ib, :], 64)
    sk_t, skk = self.load_bcast_row(None, "sinks", self.din("b_sinks", [1, 16])[ib, :], 16)
    b31, b31k = self.load_bcast_row(None, "b31", self.din("rel_bias", [32, 16])[31, :], 16)
    self.op("dve", lambda e: e.tensor_tensor(out=sk_t[:], in0=sk_t[:], in1=b31[:], op=ALU.subtract), r=(skk, b31k), w=(skk,))
    self.op("act", lambda e: e.activation(out=sk_t[:], in_=sk_t[:], func=AF.Exp), r=(skk,), w=(skk,))
    win, wink = self.load_weight(None, "bwin", b_w_in[ib], D, 1536)
    wout, woutk = self.load_weight(None, "bwout", b_w_out[ib], D, D)
    xts = self.rot(None, "xt", [128, D], F32, 3)
    hs = self.rot(None, "h", [128, D], BF16, 2)
    hTs = self.rot(None, "hT", [128, 8, 128], BF16, 2)
    sts = self.rot(None, "st", [128, 128], F32, 2)
    prs = self.rot(None, "pr", [128, 1536], F32, 2)
    tmpn = self.rot(None, "tmpn", [128, D], F32, 2)
    qns = self.rot(None, "qn", [128, D], BF16, 2)
    kns = self.rot(None, "kn", [128, 256], BF16, 2)
    QTs = self.rot(None, "QT", [64, 16 * 128], BF16, 2)
    KTs = self.rot(None, "KT", [64, 4, 128], BF16, 3)
    VAs = self.rot(None, "VA", [128, 4, 65], BF16, 3)
    PTs = self.rot(None, "PT", [128, 16 * 128], BF16, 2)
    tmps = self.rot(None, "tmpb", [128, 512], F32, 2)
    aos = self.rot(None, "ao", [128, D], BF16, 2)
    aTs = self.rot(None, "aT", [128, 8, 128], BF16, 2)
    xos = self.rot(None, "xo", [128, D], F32, 2)
    junk = self.sb(None, "junk", [128, D], BF16)
    for (va, vak) in VAs.tiles:
        self.op("dve", lambda e, va=va: e.memset(va[:, :, 64:65], 1.0), w=(vak,))
    ntile = self.TOK // 128

    def load(ti):
        xt, xk = xts.get(ti)
        self.dma_in(xt[:], x_src[ti * 128:(ti + 1) * 128, :], xk, r=(("xd", src_id, ti),))

    def stage1(ti):
        i = ti % NT
        xt, xk, pr, prk, st, sk = _proj_front(self, ti, xts, hs, hTs, sts, prs, g, gk, win, wink, 1536, x_src, src_id, junk)
        tmp, tmpk = tmpn.get(ti)
        qn, qnk = qns.get(ti)
        kn, knk = kns.get(ti)
        QT, QTk = QTs.get(ti)
        KT, KTk = KTs.get(ti)
        VA, VAk = VAs.get(ti)
        _head_norm(self, pr[:, 0:1024].rearrange("p (h d) -> p h d", d=64), prk, 16, 64, gq, gqk, st, sk, 8, tmp, tmpk,
                   qn[:, :].rearrange("p (h d) -> p h d", d=64), qnk)
        _head_norm(self, pr[:, 1024:1280].rearrange("p (h d) -> p h d", d=64), prk, 4, 64, gkk_t, gkk, st, sk, 48, tmp, tmpk,
                   kn[:, :].rearrange("p (h d) -> p h d", d=64), knk)
        self.transpose_to(qn, qnk, [h * 64 for h in range(16)], 64, QT[:, :].rearrange("p (h t) -> p h t", t=128), QTk)
        self.transpose_to(kn, knk, [h * 64 for h in range(4)], 64, KT, KTk)
        self.op("act", lambda e: e.copy(out=VA[:, :, 0:64], in_=pr[:, 1280:1536].rearrange("p (h d) -> p h d", d=64)),
                r=(prk,), w=(VAk,))

    def stage2(ti):
        i = ti % NT
        xt, xk = xts.get(ti)
        st, sk = sts.get(ti)
        QT, QTk = QTs.get(ti)
        ktiles = ([i - 1] if i > 0 else []) + [i]

        def get_kT(j):
            KTj, KTjk = KTs.get(ti - (i - j))
            return [(KTj[:, kv, :], KTjk, kv * 4, 4) for kv in range(4)]

        def get_v(j):
            VAj, VAjk = VAs.get(ti - (i - j))
            return VAj, VAjk, (lambda h: h // 4)

        def bias_for(j, c):
            if j == i:
                return biasD[:, 4 * c:4 * c + 4, :], biasDk
            return biasOB[:, 4 * c:4 * c + 4, :], biasOBk

        _attend(self, QT, QTk, 64, 0.125, ktiles, get_kT, get_v, bias_for, None, PTs, tmps)
        ao, aok = aos.get(ti)
        aT, aTk = aTs.get(ti)
        xo, xok = xos.get(ti)
        _attn_finish(self, st, sk, 64, ao, aok, sinkexp=(sk_t, skk))
        _out_proj(self, ao, aok, aT, aTk, wout, woutk, xt, xk, xo, xok, x_dst, dst_id, ti)

    load(0)
    if ntile > 1:
        load(1)
    stage1(0)
    for ti in range(ntile):
        if ti + 2 < ntile:
            load(ti + 2)
        if ti + 1 < ntile:
            stage1(ti + 1)
        stage2(ti)
    S.barrier()
    S.recycle()
    self.sb_off = mark


Builder.phase_b = _phase_b


def _rope(self, src3, srck, sc, sck, r1, r1k, r2, r2k, dst3, dstk, H):
    t1 = src3[:, :, 64:80]
    t2 = src3[:, :, 80:96]
    sin_b = sc[:, 0:16].unsqueeze(1).to_broadcast([128, H, 16])
    cos_b = sc[:, 16:32].unsqueeze(1).to_broadcast([128, H, 16])
    a = r1[:, 0:H * 16].rearrange("p (h d) -> p h d", d=16)
    b = r2[:, 0:H * 16].rearrange("p (h d) -> p h d", d=16)
    self.op("dve", lambda e: e.tensor_tensor(out=a, in0=t1, in1=cos_b, op=ALU.mult), r=(srck, sck), w=(r1k,))
    self.op("dve", lambda e: e.tensor_tensor(out=b, in0=t2, in1=sin_b, op=ALU.mult), r=(srck, sck), w=(r2k,))
    self.op("dve", lambda e: e.tensor_tensor(out=dst3[:, :, 64:80], in0=a, in1=b, op=ALU.subtract), r=(r1k, r2k), w=(dstk,))
    self.op("dve", lambda e: e.tensor_tensor(out=a, in0=t2, in1=cos_b, op=ALU.mult), r=(srck, sck), w=(r1k,))
    self.op("dve", lambda e: e.tensor_tensor(out=b, in0=t1, in1=sin_b, op=ALU.mult), r=(srck, sck), w=(r2k,))
    self.op("dve", lambda e: e.tensor_tensor(out=dst3[:, :, 80:96], in0=a, in1=b, op=ALU.add), r=(r1k, r2k), w=(dstk,))


def _phase_c(self, li, x_src, src_id, x_dst, dst_id):
    S = self.S
    nc = self.nc
    self.ng = 3
    mark = self.sb_off
    NT = self.NT
    ic = 0
    ntile = self.TOK // 128
    TWO_PI = 2.0 * math.pi
    c_w_in = self.din("c_w_in", [1, D, 416])
    c_w_q_b = self.din("c_w_q_b", [1, 256, 1536])
    c_w_kv_b = self.din("c_w_kv_b", [1, 128, 2048])
    c_w_out = self.din("c_w_out", [1, D, D])
    norm_mix = self.din("norm_mix", [4, D])
    pos_d = self.din("positions", [self.TOK, 1], I32)
    ktd = nc.dram_tensor("ktd_scr", [ntile, 96, 16 * 128], BF16, kind="Internal").ap()
    vad = nc.dram_tensor("vad_scr", [ntile, 128, 16 * 65], BF16, kind="Internal").ap()
    g, gk = self.load_bcast_row(None, "gmix", norm_mix[li, :], D)
    gqa, gqak = self.load_bcast_row(None, "gqa", self.din("c_q_a_gain", [1, 256])[ic, :], 256)
    gkva, gkvak = self.load_bcast_row(None, "gkva", self.din("c_kv_a_gain", [1, 128])[ic, :], 128)
    gq, gqk = self.load_bcast_row(None, "gq", self.din("c_q_gain", [1, 96])[ic, :], 96)
    gkg, gkgk = self.load_bcast_row(None, "gk", self.din("c_k_gain", [1, 96])[ic, :], 96)
    invf = self.sb(None, "invf", [128, 16], F32)
    self.dma_in(invf[:], self.din("k_invf", [128, 16])[:, :], "invf")
    caus = self.sb(None, "causst", [128, 128], F32)
    self.dma_in(caus[:], self.din("k_caus_st", [128, 128])[:, :], "causst")
    negpi = self.sb(None, "negpi", [128, 1], F32)
    win, wink = self.load_weight(None, "cwin", c_w_in[ic], D, 416)
    wqb, wqbk = self.load_weight(None, "cwqb", c_w_q_b[ic], 256, 1536)
    wkvb, wkvbk = self.load_weight(None, "cwkvb", c_w_kv_b[ic], 128, 2048)
    wout, woutk = self.load_weight(None, "cwout", c_w_out[ic], D, D)
    xts = self.rot(None, "xt", [128, D], F32, 2)
    hs = self.rot(None, "h", [128, D], BF16, 2)
    hTs = self.rot(None, "hT", [128, 8, 128], BF16, 2)
    sts = self.rot(None, "st", [128, 256], F32, 2)
    prs = self.rot(None, "pr", [128, 416], F32, 2)
    tmpn = self.rot(None, "tmpn", [128, 1536], F32, 1)
    lat = self.rot(None, "lat", [128, 384], BF16, 2)
    qlTs = self.rot(None, "qlT", [128, 3, 128], BF16, 2)
    q32s = self.rot(None, "q32", [128, 1536], F32, 1)
    k32s = self.rot(None, "k32", [128, 1536], F32, 1)
    qfs = self.rot(None, "qf", [128, 1536], BF16, 2)
    kfs = self.rot(None, "kf", [128, 1536], BF16, 2)
    r1s = self.rot(None, "r1", [128, 256], F32, 2)
    r2s = self.rot(None, "r2", [128, 256], F32, 2)
    posi_s = self.rot(None, "posi", [128, 1], I32, 2)
    angs = self.rot(None, "ang", [128, 64], F32, 2)
    angi = self.rot(None, "angi", [128, 32], I32, 2)
    QTs = self.rot(None, "QT", [96, 16 * 128], BF16, 2)
    KTt = self.rot(None, "KTt", [96, 16, 128], BF16, 2)
    VAt = self.rot(None, "VAt", [128, 16, 65], BF16, 2)
    KTb = self.rot(None, "KTb", [96, 16, 128], BF16, 3)
    VAb = self.rot(None, "VAb", [128, 16, 65], BF16, 3)
    PTs = self.rot(None, "PT", [128, 16 * 128], BF16, 2)
    tmps = self.rot(None, "tmpb", [128, 512], F32, 2)
    aos = self.rot(None, "ao", [128, D], BF16, 2)
    aTs = self.rot(None, "aT", [128, 8, 128], BF16, 2)
    xos = self.rot(None, "xo", [128, D], F32, 2)
    junk = self.sb(None, "junk", [128, D], BF16)
    self.op("dve", lambda e: e.memset(negpi[:], -math.pi), w=("negpi",))
    for (va, vak) in VAt.tiles:
        self.op("dve", lambda e, va=va: e.memset(va[:, :, 64:65], 1.0), w=(vak,))

    def load(ti):
        xt, xk = xts.get(ti)
        self.dma_in(xt[:], x_src[ti * 128:(ti + 1) * 128, :], xk, r=(("xd", src_id, ti),))
        pi_, pik = posi_s.get(ti)
        self.dma_in(pi_[:], pos_d[ti * 128:(ti + 1) * 128, :], pik)

    def tile_body(ti):
        i = ti % NT
        xt, xk, pr, prk, st, sk = _proj_front(self, ti, xts, hs, hTs, sts, prs, g, gk, win, wink, 416, x_src, src_id, junk)
        tmp, tmpk = tmpn.get(ti)
        la, lak = lat.get(ti)
        qlT, qlTk = qlTs.get(ti)
        q32, q32k = q32s.get(ti)
        k32, k32k = k32s.get(ti)
        qf, qfk = qfs.get(ti)
        kf, kfk = kfs.get(ti)
        r1, r1k = r1s.get(ti)
        r2, r2k = r2s.get(ti)
        VA, VAk = VAt.get(ti)
        KT, KTk = KTt.get(ti)
        QT, QTk = QTs.get(ti)
        pi_, pik = posi_s.get(ti)
        ang, angk = angs.get(ti)
        ai, aik = angi.get(ti)
        posf = ang[:, 32:33]
        a32 = ang[:, 0:32]
        kf32 = ang[:, 33:33 + 31]
        self.op("dve", lambda e: e.tensor_copy(out=posf, in_=pi_[:]), r=(pik,), w=(angk,))
        self.op("dve", lambda e: e.tensor_scalar(out=ang[:, 0:16], in0=invf[:], scalar1=posf, scalar2=None, op0=ALU.mult),
                r=(angk, "invf"), w=(angk,))
        self.op("dve", lambda e: e.tensor_scalar(out=ang[:, 16:32], in0=ang[:, 0:16], scalar1=math.pi / 2, scalar2=None,
                                                 op0=ALU.add), r=(angk,), w=(angk,))
        self.op("dve", lambda e: e.tensor_scalar(out=ang[:, 32:64], in0=a32, scalar1=1.0 / TWO_PI, scalar2=None,
                                                 op0=ALU.mult), r=(angk,), w=(angk,))
        self.op("dve", lambda e: e.tensor_copy(out=ai[:], in_=ang[:, 32:64]), r=(angk,), w=(aik,))
        self.op("dve", lambda e: e.tensor_copy(out=ang[:, 32:64], in_=ai[:]), r=(aik,), w=(angk,))
        self.op("dve", lambda e: e.scalar_tensor_tensor(out=a32, in0=ang[:, 32:64], scalar=-TWO_PI, in1=a32, op0=ALU.mult,
                                                        op1=ALU.add), r=(angk,), w=(angk,))
        self.op("dve", lambda e: e.tensor_scalar(out=ang[:, 32:64], in0=a32, scalar1=math.pi, scalar2=-TWO_PI,
                                                 op0=ALU.is_gt, op1=ALU.mult), r=(angk,), w=(angk,))
        self.op("dve", lambda e: e.tensor_tensor(out=a32, in0=a32, in1=ang[:, 32:64], op=ALU.add), r=(angk,), w=(angk,))
        self.op("dve", lambda e: e.tensor_scalar(out=ang[:, 32:64], in0=a32, scalar1=-math.pi, scalar2=TWO_PI,
                                                 op0=ALU.is_lt, op1=ALU.mult), r=(angk,), w=(angk,))
        self.op("dve", lambda e: e.tensor_tensor(out=a32, in0=a32, in1=ang[:, 32:64], op=ALU.add), r=(angk,), w=(angk,))
        self.op("act", lambda e: e.activation(out=a32, in_=a32, func=AF.Sin), r=(angk,), w=(angk,))
        sc, sck = ang, angk
        _head_norm(self, pr[:, 0:256].unsqueeze(1), prk, 1, 256, gqa, gqak, st, sk, 8, tmp, tmpk, la[:, 0:256].unsqueeze(1), lak)
        _head_norm(self, pr[:, 256:384].unsqueeze(1), prk, 1, 128, gkva, gkvak, st, sk, 12, tmp, tmpk,
                   la[:, 256:384].unsqueeze(1), lak)
        self.transpose_full(la, lak, 3, qlT, qlTk)

        def evac_q(b, bk, n0, wd):
            ps = self.banks[b]
            self.op("act", lambda e: e.copy(out=q32[:, n0:n0 + wd], in_=ps[:, 0:wd]), r=(bk,), w=(q32k,))

        self.linear(qlT, qlTk, 2, wqb, wqbk, 1536, evac_q)
        k3 = k32[:, :].rearrange("p (h d) -> p h d", d=96)

        def evac_kv(b, bk, n0, wd):
            ps3 = self.banks[b][:, 0:512].rearrange("p (h d) -> p h d", d=128)
            h0 = n0 // 128
            self.op("act", lambda e: e.copy(out=k3[:, h0:h0 + 4, 0:64], in_=ps3[:, :, 0:64]), r=(bk,), w=(k32k,))
            self.op("dve", lambda e: e.tensor_copy(out=VA[:, h0:h0 + 4, 0:64], in_=ps3[:, :, 64:128]), r=(bk,), w=(VAk,))

        self.linear(qlT[:, 2:3, :], qlTk, 1, wkvb, wkvbk, 2048, evac_kv)
        self.op("dve", lambda e: e.tensor_copy(out=k3[:, :, 64:96], in_=pr[:, 384:416].unsqueeze(1).to_broadcast([128, 16, 32])),
                r=(prk,), w=(k32k,))
        q3 = q32[:, :].rearrange("p (h d) -> p h d", d=96)
        qf3 = qf[:, :].rearrange("p (h d) -> p h d", d=96)
        kf3 = kf[:, :].rearrange("p (h d) -> p h d", d=96)
        _head_norm(self, q3, q32k, 16, 96, gq, gqk, st, sk, 16, tmp, tmpk, q3, q32k)
        _head_norm(self, k3, k32k, 16, 96, gkg, gkgk, st, sk, 48, tmp, tmpk, k3, k32k)
        self.op("act", lambda e: e.copy(out=qf3[:, :, 0:64], in_=q3[:, :, 0:64]), r=(q32k,), w=(qfk,))
        self.op("act", lambda e: e.copy(out=kf3[:, :, 0:64], in_=k3[:, :, 0:64]), r=(k32k,), w=(kfk,))
        _rope(self, q3, q32k, sc, sck, r1, r1k, r2, r2k, qf3, qfk, 16)
        _rope(self, k3, k32k, sc, sck, r1, r1k, r2, r2k, kf3, kfk, 16)
        self.transpose_to(qf, qfk, [h * 96 for h in range(16)], 96, QT[:, :].rearrange("p (h t) -> p h t", t=128), QTk)
        self.transpose_to(kf, kfk, [h * 96 for h in range(16)], 96, KT, KTk)
        self.dma_out(ktd[ti].rearrange("p (h t) -> p h t", t=128), KT[:], KTk, w=(("ktd", ti),))
        self.dma_out(vad[ti].rearrange("p (h d) -> p h d", d=65), VA[:], VAk, w=(("vad", ti),))
        ktiles = list(range(i + 1))
        base = ti - i

        def get_kT(j):
            kb, kbk = KTb.next()
            self.dma_in(kb[:], ktd[base + j].rearrange("p (h t) -> p h t", t=128), kbk, r=(("ktd", base + j),))
            return [(kb[:, h, :], kbk, h, 1) for h in range(16)]

        def get_v(j):
            vb, vbk = VAb.next()
            self.dma_in(vb[:], vad[base + j].rearrange("p (h d) -> p h d", d=65), vbk, r=(("vad", base + j),))
            return vb, vbk, (lambda h: h)

        def bias_for(j, c, i=i):
            if j == i:
                return caus[:, :].unsqueeze(1).to_broadcast([128, 4, 128]), "causst"
            return None

        _attend(self, QT, QTk, 96, 96.0 ** -0.5, ktiles, get_kT, get_v, bias_for, None, PTs, tmps)
        ao, aok = aos.get(ti)
        aT, aTk = aTs.get(ti)
        xo, xok = xos.get(ti)
        _attn_finish(self, st, sk, 96, ao, aok)
        _out_proj(self, ao, aok, aT, aTk, wout, woutk, xt, xk, xo, xok, x_dst, dst_id, ti)

    load(0)
    for ti in range(ntile):
        if ti + 1 < ntile:
            load(ti + 1)
        tile_body(ti)
    S.barrier()
    S.recycle()
    self.sb_off = mark


Builder.phase_c = _phase_c


BIS = "dve"
NIT = 16
BIGM = 32768.0


def _phase_a(self, li, ia, x_src, src_id, x_dst, dst_id):
    S = self.S
    self.ng = 3
    _setup_bias(self)
    mark = self.sb_off
    NT = self.NT
    SEQ = self.SEQ
    topk = self.topk
    ntile = self.TOK // 128
    a_w_in = self.din("a_w_in", [2, D, 1736])
    a_w_out = self.din("a_w_out", [2, D, D])
    norm_mix = self.din("norm_mix", [4, D])
    biasD, biasDk = _load_bias_tile(self, "biasD", 0, 0)
    biasOA, biasOAk = _load_bias_tile(self, "biasOA", 0, 1)
    g, gk = self.load_bcast_row(None, "gmix", norm_mix[li, :], D)
    gq, gqk = self.load_bcast_row(None, "gq", self.din("a_q_gain", [2, 64])[ia, :], 64)
    gkt, gkk = self.load_bcast_row(None, "gk", self.din("a_k_gain", [2, 64])[ia, :], 64)
    causts = self.sb(None, "causts", [128, 128], F32)
    self.dma_in(causts[:], self.din("k_caus_ts", [128, 128])[:, :], "causts")
    negbig = self.sb(None, "negbig", [128, 1], F32)
    self.op("dve", lambda e: e.memset(negbig[:], -BIGM), w=("negbig",))
    ctab = self.sb(None, "ctab", [128, NIT], F32)
    cb = self.sb(None, "cb", [128, NT], F32)
    for k in range(NIT):
        self.op("dve", lambda e, k=k: e.memset(ctab[:, k:k + 1], -(2.0 ** -(k + 2))), w=("ctab",))
    for i_ in range(NT):
        self.op("dve", lambda e, i_=i_: e.memset(cb[:, i_:i_ + 1], float((i_ + 1) * 128 - 2 * topk) + 0.5), w=("cb",))
    win, wink = self.load_weight(None, "awin", a_w_in[ia], D, 1736)
    wout, woutk = self.load_weight(None, "awout", a_w_out[ia], D, D)
    KT = self.sb(None, "KT", [64, NT, 128], BF16)
    KIT = self.sb(None, "KIT", [64, NT * 128], BF16)
    VA = self.sb(None, "VA", [128, NT, 65], BF16)
    score = self.sb(None, "score", [128, SEQ], F32)
    Mts = self.rot(None, "Mt", [128, SEQ], BF16, 2)
    self.op("dve", lambda e: e.memset(VA[:, :, 64:65], 1.0), w=tuple(("VA", j) for j in range(NT)))
    xts = self.rot(None, "xt", [128, D], F32, 3)
    hs = self.rot(None, "h", [128, D], BF16, 1)
    hTs = self.rot(None, "hT", [128, 8, 128], BF16, 1)
    sts = self.rot(None, "st", [128, 160], F32, 2)
    prs = self.rot(None, "pr", [128, 1736], F32, 1)
    tmpn = self.rot(None, "tmpn", [128, D], F32, 1)
    qns = self.rot(None, "qn", [128, D], BF16, 1)
    kns = self.rot(None, "kn", [128, 64], BF16, 1)
    qkis = self.rot(None, "qki", [128, 576], BF16, 1)
    QTs = self.rot(None, "QT", [64, 16 * 128], BF16, 2)
    QITs = self.rot(None, "QIT", [64, 8 * 128], BF16, 1)
    rls = self.rot(None, "rl", [128, 512], F32, 3)
    MTs = self.rot(None, "MT", [128, 4, 128], BF16, 3)
    PTs = self.rot(None, "PT", [128, 16 * 128], BF16, 3)
    tmps = self.rot(None, "tmpb", [128, 512], F32, 2)
    aos = self.rot(None, "ao", [128, D], BF16, 1)
    aTs = self.rot(None, "aT", [128, 8, 128], BF16, 1)
    xos = self.rot(None, "xo", [128, D], F32, 1)
    junk = self.sb(None, "junk", [128, D], BF16)
    WS = 8.0 ** -0.5 / 8.0

    def load(ti):
        xt, xk = xts.get(ti)
        self.dma_in(xt[:], x_src[ti * 128:(ti + 1) * 128, :], xk, r=(("xd", src_id, ti),))

    def stage1(ti):
        i = ti % NT
        nk = (i + 1) * 128
        xt, xk, pr, prk, st, sk = _proj_front(self, ti, xts, hs, hTs, sts, prs, g, gk, win, wink, 1736, x_src, src_id, junk)
        tmp, tmpk = tmpn.get(ti)
        qn, qnk = qns.get(ti)
        kn, knk = kns.get(ti)
        qki, qkik = qkis.get(ti)
        QT, QTk = QTs.get(ti)
        QIT, QITk = QITs.get(ti)
        Mt, Mtk = Mts.get(ti)
        _head_norm(self, pr[:, 0:1024].rearrange("p (h d) -> p h d", d=64), prk, 16, 64, gq, gqk, st, sk, 8, tmp, tmpk,
                   qn[:, :].rearrange("p (h d) -> p h d", d=64), qnk)
        _head_norm(self, pr[:, 1024:1088].unsqueeze(1), prk, 1, 64, gkt, gkk, st, sk, 48, tmp, tmpk, kn[:, :].unsqueeze(1), knk)
        self.op("act", lambda e: e.copy(out=qki[:], in_=pr[:, 1152:1728]), r=(prk,), w=(qkik,))
        self.transpose_to(qn, qnk, [h * 64 for h in range(16)], 64, QT[:, :].rearrange("p (h t) -> p h t", t=128), QTk)
        self.transpose_to(kn, knk, [0], 64, KT, ("KT", i), dst_blk0=i)
        self.transpose_to(qki, qkik, [h * 64 for h in range(8)], 64, QIT[:, :].rearrange("p (h t) -> p h t", t=128), QITk)
        self.transpose_to(qki, qkik, [512], 64, KIT[:, :].rearrange("p (j t) -> p j t", t=128), ("KIT", i), dst_blk0=i)
        self.op("act", lambda e: e.copy(out=VA[:, i, 0:64], in_=pr[:, 1088:1152]), r=(prk,), w=(("VA", i),))
        if nk <= topk:
            return
        bk_ = sk + "_bis"
        wsc = st[:, 100:108]
        lo = st[:, 110:111]
        w0 = st[:, 111:112]
        hi = st[:, 112:113]
        acc = st[:, 113:114]
        sg = st[:, 114:115]
        negmid = st[:, 115:116]
        negW = st[:, 120:120 + NIT]
        self.op("dve", lambda e: e.tensor_scalar(out=wsc, in0=pr[:, 1728:1736], scalar1=WS, scalar2=None, op0=ALU.mult),
                r=(prk,), w=(bk_,))
        for c0 in range(0, nk, 512):
            wd = min(512, nk - c0)
            kkeys = tuple(("KIT", j) for j in range(c0 // 128, (c0 + wd) // 128))
            for h in range(8):
                b = self.gbank()
                bkb = "bank%d" % b
                ps = self.banks[b]
                self.op("pe", lambda e, h=h, ps=ps, c0=c0, wd=wd: e.matmul(
                    ps[:, 0:wd], lhsT=QIT[:, h * 128:(h + 1) * 128], rhs=KIT[:, c0:c0 + wd], start=True, stop=True),
                    r=(QITk,) + kkeys, w=(bkb,))
                rl, rlk = rls.next()
                self.op("act", lambda e, ps=ps, rl=rl, wd=wd: e.activation(out=rl[:, 0:wd], in_=ps[:, 0:wd], func=AF.Relu),
                        r=(bkb,), w=(rlk,))
                if h == 0:
                    self.op("dve", lambda e, rl=rl, c0=c0, wd=wd: e.tensor_scalar(
                        out=score[:, c0:c0 + wd], in0=rl[:, 0:wd], scalar1=wsc[:, 0:1], scalar2=None, op0=ALU.mult),
                        r=(rlk, bk_), w=("score",))
                else:
                    self.op("dve", lambda e, rl=rl, c0=c0, wd=wd, h=h: e.scalar_tensor_tensor(
                        out=score[:, c0:c0 + wd], in0=rl[:, 0:wd], scalar=wsc[:, h:h + 1], in1=score[:, c0:c0 + wd],
                        op0=ALU.mult, op1=ALU.add), r=(rlk, bk_, "score"), w=("score",))
        self.op("dve", lambda e: e.tensor_reduce(out=hi, in_=score[:, 0:nk], axis=AX.X, op=ALU.max), r=("score",), w=(bk_,))
        self.op("dve", lambda e: e.tensor_reduce(out=lo, in_=score[:, 0:nk], axis=AX.X, op=ALU.min), r=("score",), w=(bk_,))
        self.op("dve", lambda e: e.tensor_tensor(out=w0, in0=hi, in1=lo, op=ALU.subtract), r=(bk_,), w=(bk_,))
        self.op("dve", lambda e: e.tensor_tensor(out=score[:, nk - 128:nk], in0=score[:, nk - 128:nk], in1=causts[:],
                                                 op=ALU.add), r=("score", "causts"), w=("score",))
        self.op("dve", lambda e: e.tensor_scalar(out=negW, in0=ctab[:, 0:NIT], scalar1=w0, scalar2=None, op0=ALU.mult),
                r=(bk_, "ctab"), w=(bk_,))
        self.op("dve", lambda e: e.scalar_tensor_tensor(out=negmid, in0=w0, scalar=-0.5, in1=lo, op0=ALU.mult,
                                                        op1=ALU.subtract), r=(bk_,), w=(bk_,))
        if BIS == "act":
            for it in range(NIT):
                self.op("act", lambda e: e.activation(out=Mt[:, 0:nk], in_=score[:, 0:nk], func=AF.Sign, bias=negmid,
                                                      accum_out=acc), r=("score", bk_), w=(bk_, Mtk))
                self.op("act", lambda e: e.activation(out=sg, in_=acc, func=AF.Sign, bias=cb[:, i:i + 1]), r=(bk_, "cb"), w=(bk_,))
                self.op("act", lambda e, it=it: e.activation(out=negmid, in_=sg, func=AF.Identity, bias=negmid,
                                                             scale=negW[:, it:it + 1]), r=(bk_,), w=(bk_,))
            self.op("dve", lambda e: e.tensor_tensor(out=lo, in0=negW[:, NIT - 1:NIT], in1=negmid, op=ALU.subtract), r=(bk_,), w=(bk_,))

        else:
            self.op("dve", lambda e: e.tensor_scalar(out=negmid, in0=negmid, scalar1=-1.0, scalar2=None, op0=ALU.mult),
                    r=(bk_,), w=(bk_,))
            self.op("dve", lambda e: e.tensor_scalar(out=negW, in0=negW, scalar1=-2.0, scalar2=None, op0=ALU.mult),
                    r=(bk_,), w=(bk_,))
            for it in range(NIT):
                self.op("dve", lambda e: e.tensor_scalar(out=Mt[:, 0:nk], in0=score[:, 0:nk], scalar1=negmid, scalar2=0.0,
                                                         op0=ALU.is_ge, op1=ALU.add, accum_out=acc),
                        r=("score", bk_), w=(bk_, Mtk))
                self.op("dve", lambda e: e.tensor_scalar(out=sg, in0=acc, scalar1=float(topk) - 0.5, scalar2=0.5,
                                                         op0=ALU.is_ge, op1=ALU.subtract), r=(bk_,), w=(bk_,))
                self.op("dve", lambda e, it=it: e.scalar_tensor_tensor(out=negmid, in0=sg, scalar=negW[:, it:it + 1], in1=negmid,
                                                                       op0=ALU.mult, op1=ALU.add), r=(bk_,), w=(bk_,))
            self.op("dve", lambda e: e.scalar_tensor_tensor(out=lo, in0=negW[:, NIT - 1:NIT], scalar=-0.5, in1=negmid,
                                                            op0=ALU.mult, op1=ALU.add), r=(bk_,), w=(bk_,))
        self.op("dve", lambda e: e.tensor_scalar(out=Mt[:, 0:nk], in0=score[:, 0:nk], scalar1=lo, scalar2=None,
                                                 op0=ALU.is_ge), r=("score", bk_), w=(Mtk,))

    def stage2(ti):
        i = ti % NT
        nk = (i + 1) * 128
        select = nk > topk
        xt, xk = xts.get(ti)
        st, sk = sts.get(ti)
        QT, QTk = QTs.get(ti)
        Mt, Mtk = Mts.get(ti)

        def get_kT(j):
            return [(KT[:, j, :], ("KT", j), 0, 16)]

        def get_v(j):
            return VA[:, j:j + 1, :], ("VA", j), (lambda h: 0)

        def bias_for(j, c):
            if j == i:
                return biasD[:, 4 * c:4 * c + 4, :], biasDk
            if j == i - 1:
                return biasOA[:, 4 * c:4 * c + 4, :], biasOAk
            return None

        def mask_for(j):
            MT, MTk = MTs.next()
            b = self.gbank()
            bkb = "bank%d" % b
            pb = self.banks[b].bitcast(BF16)
            self.op("pe", lambda e: e.transpose(out=pb[:, 0:128], in_=Mt[:, j * 128:(j + 1) * 128], identity=self.ident[:]),
                    r=(Mtk, "ident"), w=(bkb,))
            self.op("act", lambda e: e.activation(out=MT[:, :, :], in_=pb[:, 0:128].unsqueeze(1).to_broadcast([128, 4, 128]),
                                                  func=AF.Identity, scale=BIGM, bias=negbig[:, 0:1]),
                    r=(bkb, "negbig"), w=(MTk,))
            return MT[:, :, :].rearrange("p a b -> p (a b)"), MTk

        _attend(self, QT, QTk, 64, 0.125, list(range(i + 1)), get_kT, get_v, bias_for, mask_for if select else None, PTs, tmps)
        ao, aok = aos.get(ti)
        aT, aTk = aTs.get(ti)
        xo, xok = xos.get(ti)
        _attn_finish(self, st, sk, 64, ao, aok)
        _out_proj(self, ao, aok, aT, aTk, wout, woutk, xt, xk, xo, xok, x_dst, dst_id, ti)

    load(0)
    if ntile > 1:
        load(1)
    stage1(0)
    for ti in range(ntile):
        if ti + 2 < ntile:
            load(ti + 2)
        nxt = ti + 1 < ntile
        if nxt and (ti + 1) % NT != 0:
            stage1(ti + 1)
            stage2(ti)
        else:
            stage2(ti)
            if nxt:
                stage1(ti + 1)
    S.barrier()
    S.recycle()
    self.sb_off = mark


Builder.phase_a = _phase_a


FULL_PHASES = [("a", 0), ("mlp", 0), ("b", 1), ("mlp", 1), ("c", 2), ("mlp", 2), ("a", 3), ("mlp", 3)]
LAUNCH_PLAN = [FULL_PHASES]


def _run_group(phases, xs, pos, inputs, SEQ, NSEQ, topk):
    B = build_program(phases, SEQ, NSEQ, topk)
    consts = const_inputs(B)
    in_maps = []
    for c in range(len(xs)):
        m = {}
        for name in B.dram_in:
            if name == "x":
                m[name] = xs[c]
            elif name == "positions":
                m[name] = pos[c]
            elif name in consts:
                m[name] = consts[name]
            else:
                m[name] = inputs[name]
        in_maps.append(m)
    res = run_bass_kernel_spmd(B.nc, in_maps, core_ids=list(range(len(xs))))
    return [np.asarray(r["out"]) for r in res.results]


def kernel(**inputs):
    inputs = {k: np.ascontiguousarray(np.asarray(v)) for k, v in inputs.items()}
    x = inputs["x"]
    Bsz, SEQ, _ = x.shape
    NSEQ = Bsz // NCORES
    topk = min(256, SEQ // 4)
    xs = [np.ascontiguousarray(x[c * NSEQ:(c + 1) * NSEQ].reshape(NSEQ * SEQ, D)) for c in range(NCORES)]
    pos = [np.ascontiguousarray(inputs["positions"][c * NSEQ:(c + 1) * NSEQ].reshape(NSEQ * SEQ, 1).astype(np.int32))
           for c in range(NCORES)]
    for group in LAUNCH_PLAN:
        xs = _run_group(group, xs, pos, inputs, SEQ, NSEQ, topk)
    out = np.stack([o.reshape(NSEQ, SEQ, D) for o in xs], axis=0).reshape(Bsz, SEQ, D)
    return out.astype(np.float32, copy=False)
```
